# Optimizing a Trainium2 kernel written in Bass

```python
import math
import jax
import jax.numpy as jnp
from jax import lax
import numpy as np

D_MODEL = 1024
BATCH = 8
SEQ = 4096
DEPTH = 2

HEAD_DIM = 64
Q_BLOCK = 128
NORM_EPS = 1e-6

REL_BUCKETS = 32
REL_MAX_DIST = 2048

A_HEADS = 8
A_IDX_HEADS = 4
A_IDX_DIM = 64
A_TOPK_MAX = 256

B_HEADS = 8
B_KV_HEADS = 2
B_GROUP = B_HEADS // B_KV_HEADS
B_CMP_LEN = 32
B_CMP_STRIDE = 16
B_SEL_LEN = 64
B_SEL_TOPK_MAX = 16
B_WINDOW = 512
B_Q_BLOCK = 64

C_GROUPS = ((128, 1), (512, 4), (2048, 16))
C_HEADS_PER_GROUP = 4
C_HEADS = C_HEADS_PER_GROUP * len(C_GROUPS)
C_PAD = 2048

REL_HEADS = A_HEADS + B_HEADS + C_HEADS

D_FF = ((8 * D_MODEL + 3 * 256 - 1) // (3 * 256)) * 256

IN_SIZES = (
    A_HEADS * HEAD_DIM,
    HEAD_DIM,
    HEAD_DIM,
    A_IDX_HEADS * A_IDX_DIM,
    A_IDX_DIM,
    A_IDX_HEADS,
    B_HEADS * HEAD_DIM,
    6 * B_KV_HEADS * HEAD_DIM,
    3 * B_HEADS,
    3 * C_HEADS * HEAD_DIM,
    3 * D_MODEL,
)
IN_TOTAL = sum(IN_SIZES)

kernel_name = "hybrid_dsa_nsa_dilated_gated_block"


def _rms(x, g):
    xf = x.astype(jnp.float32)
    y = xf * lax.rsqrt(jnp.mean(xf * xf, axis=-1, keepdims=True) + NORM_EPS)
    return (y * g.astype(jnp.float32)).astype(x.dtype)


def _split_last(y, sizes):
    out, off = [], 0
    for n in sizes:
        out.append(y[..., off:off + n])
        off += n
    return out


def _rel_bucket(dist):
    n = jnp.maximum(dist, 0)
    exact = REL_BUCKETS // 2
    nf = jnp.maximum(n, 1).astype(jnp.float32)
    large = exact + (jnp.log(nf / exact) / math.log(REL_MAX_DIST / exact)
                     * (REL_BUCKETS - exact)).astype(jnp.int32)
    return jnp.where(n < exact, n, jnp.minimum(large, REL_BUCKETS - 1))


def _masked_softmax(logits, mask):
    s = jnp.where(mask, logits, -jnp.inf)
    m = jnp.max(s, axis=-1, keepdims=True)
    m = jnp.where(jnp.isfinite(m), m, 0.0)
    e = jnp.exp(s - m)
    den = jnp.sum(e, axis=-1, keepdims=True)
    p = e / jnp.maximum(den, 1e-30)
    lse = (m + jnp.log(den))[..., 0]
    return p, lse


def _sweep(fn, seq, block):
    out = lax.map(fn, jnp.arange(seq // block))
    out = jnp.moveaxis(out, 0, 1)
    return out.reshape((out.shape[0], seq) + out.shape[3:])


_gather_rows = jax.vmap(lambda a, i: a[i])


def _dsa_attention(q, k, v, iq, ik, iw, rel_tab):
    seq = q.shape[1]
    topk = min(A_TOPK_MAX, seq // 4)
    scale = HEAD_DIM ** -0.5
    keys = jnp.arange(seq)

    def block(i):
        q0 = i * Q_BLOCK
        t = q0 + jnp.arange(Q_BLOCK)
        qb = lax.dynamic_slice_in_dim(q, q0, Q_BLOCK, axis=1)
        iqb = lax.dynamic_slice_in_dim(iq, q0, Q_BLOCK, axis=1)
        iwb = lax.dynamic_slice_in_dim(iw, q0, Q_BLOCK, axis=1).astype(jnp.float32)
        rel = jax.nn.relu(jnp.einsum("bqhd,bsd->bqhs", iqb, ik, preferred_element_type=jnp.float32))
        score = jnp.einsum("bqhs,bqh->bqs", rel, iwb)
        score = jnp.where(keys[None, None, :] <= t[None, :, None], score, -jnp.inf)
        _, idx = lax.top_k(score, topk)
        kg = _gather_rows(k, idx)
        vg = _gather_rows(v, idx)
        dist = t[None, :, None] - idx
        bias = jnp.moveaxis(rel_tab[_rel_bucket(dist)], -1, 1)
        logits = jnp.einsum("bqhd,bqkd->bhqk", qb, kg, preferred_element_type=jnp.float32) * scale + bias
        p, _ = _masked_softmax(logits, (dist >= 0)[:, None])
        return jnp.einsum("bhqk,bqkd->bqhd", p.astype(vg.dtype), vg).astype(q.dtype)

    o = _sweep(block, seq, Q_BLOCK)
    return o.reshape(o.shape[:2] + (A_HEADS * HEAD_DIM,))


def _nsa_attention(q, kc, vc, ks, vs, kw, vw, gates, cmp_pos, cmp_w, k_gain, rel_tab):
    bsz, seq = q.shape[:2]
    G, Hg, Dh = B_KV_HEADS, B_GROUP, HEAD_DIM
    scale = Dh ** -0.5
    n_cmp = (seq - B_CMP_LEN) // B_CMP_STRIDE + 1
    n_sel = seq // B_SEL_LEN
    n_top = min(B_SEL_TOPK_MAX, n_sel)

    cidx = jnp.arange(n_cmp)[:, None] * B_CMP_STRIDE + jnp.arange(B_CMP_LEN)[None, :]

    def compress(src, pos, w):
        blk = src[:, cidx] + pos[:, None, :]
        return jnp.einsum("bnlgd,lde->bnge", blk, w)

    kcmp = _rms(compress(kc, cmp_pos[0], cmp_w[0]), k_gain)
    vcmp = compress(vc, cmp_pos[1], cmp_w[1])
    cstart = cidx[:, 0]
    cend = cidx[:, -1]
    sstart = jnp.arange(n_sel) * B_SEL_LEN
    overlap = ((cstart[:, None] < sstart[None, :] + B_SEL_LEN)
               & (cstart[:, None] + B_CMP_LEN > sstart[None, :])).astype(jnp.float32)
    ksb = jnp.moveaxis(ks.reshape(bsz, n_sel, B_SEL_LEN, G, Dh), 3, 1)
    vsb = jnp.moveaxis(vs.reshape(bsz, n_sel, B_SEL_LEN, G, Dh), 3, 1)
    wpad = ((0, 0), (B_WINDOW, 0), (0, 0), (0, 0))
    kw_pad = jnp.pad(kw, wpad)
    vw_pad = jnp.pad(vw, wpad)
    tab = rel_tab.reshape(REL_BUCKETS, G, Hg)
    sel_blocks = jnp.arange(n_sel)
    gather_blocks = jax.vmap(jax.vmap(lambda a, i: a[i]))
    bias_per_group = jax.vmap(lambda tb, bk: tb[bk], in_axes=(1, 1), out_axes=1)

    def block(i):
        q0 = i * B_Q_BLOCK
        t = q0 + jnp.arange(B_Q_BLOCK)
        qb = lax.dynamic_slice_in_dim(q, q0, B_Q_BLOCK, axis=1).reshape(bsz, B_Q_BLOCK, G, Hg, Dh)
        gb = lax.dynamic_slice_in_dim(gates, q0, B_Q_BLOCK, axis=1).reshape(bsz, B_Q_BLOCK, G, Hg, 3)
        lc = jnp.einsum("bqghd,bngd->bghqn", qb, kcmp, preferred_element_type=jnp.float32) * scale
        pc, _ = _masked_softmax(lc, cend[None, :] <= t[:, None])
        oc = jnp.einsum("bghqn,bngd->bqghd", pc.astype(vcmp.dtype), vcmp)
        imp = jnp.einsum("bghqn,nm->bgqm", pc, overlap)
        jt = (t // B_SEL_LEN)[:, None]
        forced = (sel_blocks[None] == 0) | (sel_blocks[None] == jt) | (sel_blocks[None] == jt - 1)
        imp = jnp.where(forced, jnp.inf, jnp.where(sel_blocks[None] <= jt, imp, -jnp.inf))
        _, sel = lax.top_k(imp, n_top)
        kg = gather_blocks(ksb, sel).reshape(bsz, G, B_Q_BLOCK, n_top * B_SEL_LEN, Dh)
        vg = gather_blocks(vsb, sel).reshape(bsz, G, B_Q_BLOCK, n_top * B_SEL_LEN, Dh)
        pos = (sel[..., None] * B_SEL_LEN + jnp.arange(B_SEL_LEN)).reshape(bsz, G, B_Q_BLOCK, -1)
        dist = t[None, None, :, None] - pos
        bias_s = jnp.moveaxis(bias_per_group(tab, _rel_bucket(dist)), -1, 2)
        ls = jnp.einsum("bqghd,bgqkd->bghqk", qb, kg, preferred_element_type=jnp.float32) * scale + bias_s
        ps, _ = _masked_softmax(ls, (dist >= 0)[:, :, None])
        osel = jnp.einsum("bghqk,bgqkd->bqghd", ps.astype(vg.dtype), vg)
        kwb = lax.dynamic_slice_in_dim(kw_pad, q0, B_Q_BLOCK + B_WINDOW, axis=1)
        vwb = lax.dynamic_slice_in_dim(vw_pad, q0, B_Q_BLOCK + B_WINDOW, axis=1)
        kpos = q0 - B_WINDOW + jnp.arange(B_Q_BLOCK + B_WINDOW)
        dw = t[:, None] - kpos[None, :]
        mw = (dw >= 0) & (dw < B_WINDOW) & (kpos[None, :] >= 0)
        bias_w = jnp.transpose(tab[_rel_bucket(dw)], (2, 3, 0, 1))
        lw = jnp.einsum("bqghd,bkgd->bghqk", qb, kwb, preferred_element_type=jnp.float32) * scale + bias_w
        pw, _ = _masked_softmax(lw, mw)
        ow = jnp.einsum("bghqk,bkgd->bqghd", pw.astype(vwb.dtype), vwb)
        o = gb[..., 0:1] * oc + gb[..., 1:2] * osel + gb[..., 2:3] * ow
        return o.reshape(bsz, B_Q_BLOCK, G * Hg * Dh).astype(q.dtype)

    return _sweep(block, seq, B_Q_BLOCK)


def _dilated_attention(q, k, v, rel_tab):
    bsz, seq = q.shape[:2]
    scale = HEAD_DIM ** -0.5
    pad = ((0, 0), (C_PAD, 0), (0, 0), (0, 0))
    hsl = [slice(g * C_HEADS_PER_GROUP, (g + 1) * C_HEADS_PER_GROUP) for g in range(len(C_GROUPS))]
    kps = [jnp.pad(k[:, :, hs], pad) for hs in hsl]
    vps = [jnp.pad(v[:, :, hs], pad) for hs in hsl]

    def block(i):
        q0 = i * Q_BLOCK
        t = q0 + jnp.arange(Q_BLOCK)
        qb = lax.dynamic_slice_in_dim(q, q0, Q_BLOCK, axis=1)
        outs, lses = [], []
        for gi, (win, dil) in enumerate(C_GROUPS):
            steps = jnp.arange(win // dil + 1) * dil
            pos = t[:, None] - steps[None, :]
            kg = jnp.take(kps[gi], pos + C_PAD, axis=1)
            vg = jnp.take(vps[gi], pos + C_PAD, axis=1)
            bias = rel_tab[_rel_bucket(steps), hsl[gi]].T[None, :, None, :]
            logits = jnp.einsum("bqhd,bqkhd->bhqk", qb[:, :, hsl[gi]], kg,
                                preferred_element_type=jnp.float32) * scale + bias
            p, lse = _masked_softmax(logits, (pos >= 0)[None, None])
            outs.append(jnp.einsum("bhqk,bqkhd->bqhd", p.astype(vg.dtype), vg))
            lses.append(lse)
        w = jax.nn.softmax(jnp.stack(lses), axis=0)
        w = jnp.transpose(w, (0, 1, 3, 2))[..., None]
        o = w[0] * outs[0]
        for gi in range(1, len(C_GROUPS)):
            o = o + w[gi] * outs[gi]
        return o.reshape(bsz, Q_BLOCK, C_HEADS_PER_GROUP * HEAD_DIM).astype(q.dtype)

    return _sweep(block, seq, Q_BLOCK)


def setup_inputs(seed: int = 0) -> dict:
    key = jax.random.key(seed)
    ks = jax.random.split(key, 14)

    def nrm(k, shape, scale):
        return jax.random.normal(k, shape, jnp.float32) * scale

    return {
        "x": nrm(ks[0], (BATCH, SEQ, D_MODEL), 1.0),
        "norm1_g": 1.0 + nrm(ks[1], (DEPTH, D_MODEL), 0.1),
        "norm2_g": 1.0 + nrm(ks[2], (DEPTH, D_MODEL), 0.1),
        "w_in": nrm(ks[3], (DEPTH, D_MODEL, IN_TOTAL), D_MODEL ** -0.5),
        "qk_norm_g": 1.0 + nrm(ks[4], (DEPTH, 6, HEAD_DIM), 0.1),
        "nsa_cmp_pos": nrm(ks[5], (DEPTH, 2, B_CMP_LEN, HEAD_DIM), 0.5),
        "nsa_cmp_w": nrm(ks[6], (DEPTH, 2, B_CMP_LEN, HEAD_DIM, HEAD_DIM), (B_CMP_LEN * HEAD_DIM) ** -0.5),
        "w_branch_a": nrm(ks[7], (DEPTH, A_HEADS * HEAD_DIM, D_MODEL), (A_HEADS * HEAD_DIM) ** -0.5),
        "w_branch_b": nrm(ks[8], (DEPTH, B_HEADS * HEAD_DIM, D_MODEL), (B_HEADS * HEAD_DIM) ** -0.5),
        "w_branch_c": nrm(ks[9], (DEPTH, C_HEADS_PER_GROUP * HEAD_DIM, D_MODEL), (C_HEADS_PER_GROUP * HEAD_DIM) ** -0.5),
        "w_out": nrm(ks[10], (DEPTH, D_MODEL, D_MODEL), D_MODEL ** -0.5),
        "w_ffn_in": nrm(ks[11], (DEPTH, D_MODEL, 2 * D_FF), D_MODEL ** -0.5),
        "w_ffn_out": nrm(ks[12], (DEPTH, D_FF, D_MODEL), D_FF ** -0.5),
        "rel_bias": nrm(ks[13], (REL_BUCKETS, REL_HEADS), 0.5),
    }


def reference(x, norm1_g, norm2_g, w_in, qk_norm_g, nsa_cmp_pos, nsa_cmp_w, w_branch_a, w_branch_b,
              w_branch_c, w_out, w_ffn_in, w_ffn_out, rel_bias):
    bsz, seq, _ = x.shape
    rel_a = rel_bias[:, :A_HEADS]
    rel_b = rel_bias[:, A_HEADS:A_HEADS + B_HEADS]
    rel_c = rel_bias[:, A_HEADS + B_HEADS:]
    for layer in range(DEPTH):
        h = _rms(x, norm1_g[layer])
        (a_q, a_k, a_v, i_q, i_k, i_w, b_q, b_kv, b_g, c_qkv, mix_g) = _split_last(h @ w_in[layer], IN_SIZES)
        qk = qk_norm_g[layer]
        a_q = _rms(a_q.reshape(bsz, seq, A_HEADS, HEAD_DIM), qk[0])
        a_k = _rms(a_k, qk[1])
        y_a = _dsa_attention(a_q, a_k, a_v, i_q.reshape(bsz, seq, A_IDX_HEADS, A_IDX_DIM), i_k, i_w,
                             rel_a) @ w_branch_a[layer]
        b_q = _rms(b_q.reshape(bsz, seq, B_HEADS, HEAD_DIM), qk[2])
        b_kv = b_kv.reshape(bsz, seq, 6, B_KV_HEADS, HEAD_DIM)
        b_gates = jax.nn.sigmoid(b_g).reshape(bsz, seq, B_HEADS, 3)
        y_b = _nsa_attention(b_q, b_kv[:, :, 0], b_kv[:, :, 1], _rms(b_kv[:, :, 2], qk[3]), b_kv[:, :, 3],
                             _rms(b_kv[:, :, 4], qk[3]), b_kv[:, :, 5], b_gates, nsa_cmp_pos[layer],
                             nsa_cmp_w[layer], qk[3], rel_b) @ w_branch_b[layer]
        c_qkv = c_qkv.reshape(bsz, seq, 3, C_HEADS, HEAD_DIM)
        y_c = _dilated_attention(_rms(c_qkv[:, :, 0], qk[4]), _rms(c_qkv[:, :, 1], qk[5]), c_qkv[:, :, 2],
                                 rel_c) @ w_branch_c[layer]
        g = jax.nn.sigmoid(mix_g).reshape(bsz, seq, 3, D_MODEL)
        x = x + (g[:, :, 0] * y_a + g[:, :, 1] * y_b + g[:, :, 2] * y_c) @ w_out[layer]
        h = _rms(x, norm2_g[layer])
        gate, up = jnp.split(h @ w_ffn_in[layer], 2, axis=-1)
        x = x + (jax.nn.silu(gate) * up) @ w_ffn_out[layer]
    return x
```

```python
import math
from contextlib import ExitStack
import numpy as np
import concourse.bass as bass
import concourse.mybir as mybir
from concourse.bass_types import AP
from concourse.bass_utils import run_bass_kernel_spmd

F32 = mybir.dt.float32
BF16 = mybir.dt.bfloat16
AF = mybir.ActivationFunctionType
ALU = mybir.AluOpType
AX = mybir.AxisListType

BIG = 30000.0
BIGF = 1.0e30
EPS = 1e-6
NBIS = 14
N_CORES = 8


def _rel_bucket_np(dist):
    n = np.maximum(dist, 0)
    nf = np.maximum(n, 1).astype(np.float32)
    large = 16 + (np.log(nf / np.float32(16)) / np.float32(math.log(2048 / 16)) * np.float32(16)).astype(np.int32)
    return np.where(n < 16, n, np.minimum(large, 31))


def make_consts():
    c = {}
    c["ident"] = np.eye(128, dtype=np.float32)
    c["flipj"] = np.eye(128, dtype=np.float32)[::-1].copy()
    LX = 1920
    dist = np.arange(LX) - 127
    oh = np.zeros((33, LX), np.float32)
    bk = _rel_bucket_np(dist)
    for x in range(LX):
        if dist[x] >= 0:
            oh[bk[x], x] = 1
        else:
            oh[32, x] = 1
    c["oh_ab"] = oh
    LW = 768
    dist = np.arange(LW) - 127
    oh = np.zeros((33, LW), np.float32)
    bk = _rel_bucket_np(dist)
    for x in range(LW):
        if 0 <= dist[x] < 512:
            oh[bk[x], x] = 1
        else:
            oh[32, x] = 1
    c["oh_w"] = oh
    LC = 384
    ohc = np.zeros((3, 33, LC), np.float32)
    for gi, dil in enumerate((1, 4, 16)):
        e = np.arange(LC) - 127
        bk = _rel_bucket_np(e * dil)
        for x in range(LC):
            if 0 <= e[x] <= 128:
                ohc[gi, bk[x], x] = 1
            else:
                ohc[gi, 32, x] = 1
    c["oh_c"] = ohc
    return c


class KB:
    def __init__(self, nc, stack):
        self.nc = nc
        self.stack = stack
        self.E = {"pe": nc.tensor, "act": nc.scalar, "dve": nc.vector, "pool": nc.gpsimd, "sp": nc.sync}
        self.sems = []
        self.semval = []
        self.free = []
        self.esem = {}
        for e in ("pe", "act", "dve", "pool"):
            self.esem[e] = self._alloc()
        self.seen = {e: {} for e in self.E}
        self.lastw = {}
        self.lastr = {}
        self.dsem = {}

    def _alloc(self):
        if self.free:
            return self.free.pop()
        h = self.stack.enter_context(self.nc.semaphore(f"sm{len(self.sems)}"))
        self.sems.append(h)
        self.semval.append(0)
        return len(self.sems) - 1

    def _wait(self, eng, si, v):
        if v <= 0 or self.seen[eng].get(si, 0) >= v:
            return
        self.E[eng].wait_ge(self.sems[si], v)
        self.seen[eng][si] = v

    def _deps(self, eng, reads, writes):
        need = {}
        for k in reads:
            w = self.lastw.get(k)
            if w:
                need[w[0]] = max(need.get(w[0], 0), w[1])
        for k in writes:
            w = self.lastw.get(k)
            if w:
                need[w[0]] = max(need.get(w[0], 0), w[1])
            for si, v in self.lastr.get(k, {}).items():
                need[si] = max(need.get(si, 0), v)
        for si, v in need.items():
            if eng == "pe" and si == self.esem["pe"]:
                continue
            if si == self.esem.get(eng) and v > self.semval[si]:
                continue
            self._wait(eng, si, v)

    def _mark(self, si, v, reads, writes):
        for k in reads:
            d = self.lastr.setdefault(k, {})
            d[si] = max(d.get(si, 0), v)
        for k in writes:
            self.lastw[k] = (si, v)
            self.lastr[k] = {}

    def op(self, eng, reads, writes, fn, inc=True):
        self._deps(eng, reads, writes)
        ins = fn(self.E[eng])
        si = self.esem[eng]
        if inc:
            ins.then_inc(self.sems[si], 1)
            self.semval[si] += 1
            v = self.semval[si]
        else:
            v = self.semval[si] + 1
        self._mark(si, v, reads, writes)
        return ins

    def dma(self, q, out, in_, reads=(), writes=(), key=None, **kw):
        self._deps(q, reads, writes)
        k = key if key is not None else (writes[0] if writes else reads[0])
        si = self.dsem.get(k)
        if si is None:
            si = self._alloc()
            self.dsem[k] = si
        self._wait(q, si, self.semval[si])
        ins = self.E[q].dma_start(out=out, in_=in_, **kw)
        ins.then_inc(self.sems[si], 16)
        self.semval[si] += 16
        self._mark(si, self.semval[si], reads, writes)

    def barrier(self):
        used = list(self.esem.values()) + list(self.dsem.values())
        for eng in self.E:
            for si in used:
                self._wait(eng, si, self.semval[si])
        for si in self.dsem.values():
            self.free.append(si)
        self.dsem = {}
        self.lastw = {}
        self.lastr = {}


def build_nc(S, D, DFF, debug=False):
    NT = S // 128
    DC = D // 128
    FC = DFF // 128
    TOPK = min(256, S // 4)
    NCMP = (S - 32) // 16 + 1
    NCC = (NCMP + 127) // 128
    NSEL = S // 64
    assert NSEL > 16 and S % 512 == 0 and S // 16 >= 128
    ND = min(14, NT)
    sizes = [512, 64, 64, 256, 64, 4, 512, 768, 24, 2304, 3 * D]
    offs = np.concatenate([[0], np.cumsum(sizes)]).astype(int)
    o_aq, o_ak, o_av, o_iq, o_ik, o_iw, o_bq, o_bkv, o_bg, o_c, o_g = [int(v) for v in offs[:11]]
    INTOT = int(offs[11])
    GC = 3 * DC

    nc = bass.Bass("TRN2", target_bir_lowering=False)
    skind = "ExternalOutput" if debug else "Internal"

    def din(name, shape, dt=F32):
        return nc.dram_tensor(name, list(shape), dt, kind="ExternalInput").ap()

    def dscr(name, shape, dt=BF16):
        return nc.dram_tensor(name, list(shape), dt, kind=skind).ap()

    x_d = din("x", [S, D])
    n1_d = din("norm1_g", [2, D])
    n2_d = din("norm2_g", [2, D])
    win_d = din("w_in", [2, D, INTOT])
    qk_d = din("qk_norm_g", [2, 6, 64])
    cpos_d = din("nsa_cmp_pos", [2, 2, 32, 64])
    cw_d = din("nsa_cmp_w", [2, 2, 32, 64, 64])
    wba_d = din("w_branch_a", [2, 512, D])
    wbb_d = din("w_branch_b", [2, 512, D])
    wbc_d = din("w_branch_c", [2, 256, D])
    wout_d = din("w_out", [2, D, D])
    wfi_d = din("w_ffn_in", [2, D, 2 * DFF])
    wfo_d = din("w_ffn_out", [2, DFF, D])
    rel_d = din("rel_bias", [32, 28])
    ident_d = din("ident", [128, 128])
    flipj_d = din("flipj", [128, 128])
    ohab_d = din("oh_ab", [33, 1920])
    ohw_d = din("oh_w", [33, 768])
    ohc_d = din("oh_c", [3, 33, 384])
    y_d = nc.dram_tensor("y", [S, D], F32, kind="ExternalOutput").ap()

    x1_d = dscr("x1", [S, D], F32)
    t8ab_d = dscr("t8ab", [28, 1920], F32)
    t8w_d = dscr("t8w", [28, 768], F32)
    t8c_d = dscr("t8c", [3, 28, 384], F32)
    bA_d = dscr("bA", [128, ND, 8, 128])
    bBs_d = dscr("bBs", [128, ND, 8, 128])
    bBw_d = dscr("bBw", [128, 5, 8, 128])
    bC_d = dscr("bC", [128, 3, 2, 4, 128])
    QA_d = dscr("QA", [4, 128, S]); KA_d = dscr("KA", [1, 128, S])
    IQ_d = dscr("IQ", [2, 128, S]); IK_d = dscr("IK", [1, 128, S])
    QB_d = dscr("QB", [4, 128, S]); KCVC_d = dscr("KCVC", [2, 128, S])
    KS_d = dscr("KS", [2, 128, S]); KW_d = dscr("KW", [2, 128, S])
    QC_d = dscr("QC", [6, 128, S]); KCc_d = dscr("KCc", [6, 128, S])
    GT_d = dscr("GT", [GC, 128, S])
    VA_d = dscr("VA", [S, 1, 65]); VS_d = dscr("VS", [S, 2, 65]); VW_d = dscr("VW", [S, 2, 65])
    VC_d = dscr("VC", [3, S, 4, 65])
    IW_d = dscr("IW", [S, 4], F32); BG_d = dscr("BG", [S, 24], F32)
    OC_d = dscr("OCs", [3, S, 4, 65], F32)
    AT_d = dscr("ATT", [S, 1280])

    stack = ExitStack()
    kb = KB(nc, stack)
    uq = [0]
    reg_negbig = nc.gpsimd.to_reg(-BIG)
    reg_negbigf = nc.gpsimd.to_reg(-BIGF)
    reg_zero = nc.gpsimd.to_reg(0.0)

    def sbt(name, shape, dt):
        uq[0] += 1
        return nc.sbuf_tensor(f"{name}_{uq[0]}", shape, dt)

    def pst_(name, shape, dt):
        uq[0] += 1
        return nc.psum_tensor(f"{name}_{uq[0]}", shape, dt)

    def sb(name, shape, dt):
        return stack.enter_context(sbt(name, list(shape), dt))

    def ps(name, shape, dt=F32):
        return stack.enter_context(pst_(name, list(shape), dt))

    identf = sb("identf", [128, 128], F32)
    flipf = sb("flipf", [128, 128], F32)
    identb = sb("identb", [128, 128], BF16)
    negI4 = sb("negI4", [128, 4, 128], BF16)
    bdiag = sb("bdiag", [128, 128], BF16)
    epsc = sb("epsc", [128, 1], F32)
    kb.dma("sp", identf[:], ident_d, writes=["identf"])
    kb.dma("sp", flipf[:], flipj_d, writes=["flipf"])
    kb.op("dve", ["identf"], ["identb"], lambda e: e.tensor_copy(out=identb[:], in_=identf[:]))
    for hh in range(4):
        kb.op("dve", ["identf"], ["negI4"], lambda e, hh=hh: e.tensor_scalar(
            out=negI4[:, hh, :], in0=identf[:], scalar1=-BIG, scalar2=None, op0=ALU.mult))
    kb.op("dve", [], ["bdiag"], lambda e: e.memset(bdiag[:], 0.0))
    kb.op("dve", [], ["bdiag"], lambda e: e.memset(bdiag[0:64, 0:64], 1.0 / 64))
    kb.op("dve", [], ["bdiag"], lambda e: e.memset(bdiag[64:128, 64:128], 1.0 / 64))
    kb.op("dve", [], ["epsc"], lambda e: e.memset(epsc[:], EPS))

    def setup_bias():
        with ExitStack() as st:
            rel8 = st.enter_context(sbt("rel8", [33, 28], F32))
            ohs = st.enter_context(sbt("ohs", [33, 1920], F32))
            t8s = st.enter_context(sbt("t8s", [28, 1920], F32))
            hk = st.enter_context(sbt("hk", [128, 2, 4, 128], F32))
            bt = st.enter_context(sbt("bt", [128, 2, 4, 128], BF16))
            pst = st.enter_context(pst_("pst", [128, 2, 512], F32))
            kb.op("dve", [], ["rel8"], lambda e: e.memset(rel8[:], -BIG / 8))
            kb.dma("sp", rel8[0:32, :], rel_d, writes=["rel8"])
            kb.op("act", ["rel8"], ["rel8"], lambda e: e.mul(out=rel8[:], in_=rel8[:], mul=8.0))
            tabs = [(ohab_d, t8ab_d, 1920), (ohw_d, t8w_d, 768)] + [(ohc_d[gi], t8c_d[gi], 384) for gi in range(3)]
            for ti, (oh_d, t8_d, L) in enumerate(tabs):
                kb.dma("sp", ohs[:, 0:L], oh_d, writes=["ohs"])
                nb = (L + 383) // 384
                for b in range(nb):
                    sl = slice(b * 384, min(L, (b + 1) * 384))
                    w = sl.stop - sl.start
                    kb.op("pe", ["rel8", "ohs"], [("pst", b % 2)], lambda e, sl=sl, w=w, b=b: e.matmul(
                        pst[0:28, b % 2, 0:w], lhsT=rel8[:, :], rhs=ohs[:, sl], start=True, stop=True))
                    kb.op("act", [("pst", b % 2)], ["t8s"], lambda e, sl=sl, w=w, b=b: e.copy(
                        out=t8s[:, sl], in_=pst[0:28, b % 2, 0:w]))
                kb.dma("sp", t8_d, t8s[:, 0:L], reads=["t8s"], key=("t8st", ti))
            kb.barrier()
            jobs = []
            for d in range(ND):
                for hb in range(2):
                    jobs.append((t8ab_d, 1920, 128 * d, hb * 4, bA_d[:, d, hb * 4:hb * 4 + 4, :]))
                    jobs.append((t8ab_d, 1920, 128 * d, 8 + hb * 4, bBs_d[:, d, hb * 4:hb * 4 + 4, :]))
            for d in range(5):
                for hb in range(2):
                    jobs.append((t8w_d, 768, 128 * d, 8 + hb * 4, bBw_d[:, d, hb * 4:hb * 4 + 4, :]))
            for gi in range(3):
                for dj in range(2):
                    jobs.append((t8c_d[gi], 384, 128 * dj, 16 + 4 * gi, bC_d[:, gi, dj, :, :]))
            for n, (t8_d, L, c0, h0, dst) in enumerate(jobs):
                s = n % 2
                src = AP(tensor=t8_d.tensor, offset=t8_d.offset + h0 * L + c0, ap=[[1, 128], [L, 4], [1, 128]])
                kb.dma("sp", hk[:, s], src, writes=[("hk", s)])
                kb.op("pe", [("hk", s), "flipf"], [("pst", s)], lambda e, s=s: e.matmul(
                    pst[:, s, :], lhsT=flipf[:], rhs=hk[:, s].rearrange("p h f -> p (h f)"), start=True, stop=True))
                kb.op("act", [("pst", s)], [("bt", s)], lambda e, s=s: e.copy(
                    out=bt[:, s].rearrange("p h f -> p (h f)"), in_=pst[:, s, :]))
                kb.dma("act", dst, bt[:, s], reads=[("bt", s)])
            kb.barrier()

    if getattr(build_nc, "limit", "all") != "none":
        setup_bias()

    def phase1(layer, xin_d):
        with ExitStack() as st:
            def sbl(name, shape, dt):
                return st.enter_context(sbt(name, list(shape), dt))
            hT = sbl("hT", [128, DC, S], BF16)
            G1 = sbl("G1", [128, D], F32)
            g6 = sbl("g6", [128, 6], F32)
            xt = sbl("xt", [128, 2, D], F32)
            junk = sbl("junk", [128, D], BF16)
            hb_ = sbl("hb", [128, 2, D], BF16)
            ss = sbl("ss", [128, 2, 4], F32)
            wst = sbl("wst", [128, 2, DC, 512], F32)
            wbf = sbl("wbf", [128, 2, DC, 512], BF16)
            sq = sbl("sq", [128, 2, 512], BF16)
            sd = sbl("sd", [128, 2, 512], F32)
            ot = sbl("ot", [128, 3, 512], BF16)
            otf = sbl("otf", [128, 2, 32], F32)
            va = sbl("va", [128, 2, 4, 65], BF16)
            tp = st.enter_context(pst_("tp", [128, 2, DC, 128], BF16))
            p1 = st.enter_context(pst_("p1", [128, 3, 512], F32))
            p2 = st.enter_context(pst_("p2", [128, 2, 512], F32))

            kb.dma("sp", G1[:], n1_d[layer:layer + 1, :].to_broadcast([128, D]), writes=["G1"])
            for half in range(2):
                kb.dma("sp", g6[half * 64:(half + 1) * 64, :], qk_d[layer].rearrange("j d -> d j"),
                       writes=["g6"], key=("g6", half), allow_slow_non_contiguous=True)
            for s in range(2):
                kb.op("dve", [], [("va", s)], lambda e, s=s: e.memset(va[:, s], 1.0))
            for tt in range(NT):
                s = tt % 2
                kb.dma("sp", xt[:, s], xin_d[tt * 128:(tt + 1) * 128, :], writes=[("xt", s)])
                kb.op("act", [("xt", s)], ["junk", ("ss", s)], lambda e, s=s: e.activation(
                    out=junk[:], in_=xt[:, s], func=AF.Square, accum_out=ss[:, s, 0:1]))
                kb.op("act", [("ss", s)], [("ss", s)], lambda e, s=s: e.activation(
                    out=ss[:, s, 1:2], in_=ss[:, s, 0:1], func=AF.Sqrt, bias=epsc[:, 0:1], scale=1.0 / D))
                kb.op("dve", [("ss", s)], [("ss", s)], lambda e, s=s: e.reciprocal(out=ss[:, s, 2:3], in_=ss[:, s, 1:2]))
                kb.op("dve", [("xt", s), ("ss", s), "G1"], [("hb", s)], lambda e, s=s: e.scalar_tensor_tensor(
                    out=hb_[:, s], in0=xt[:, s], scalar=ss[:, s, 2:3], in1=G1[:], op0=ALU.mult, op1=ALU.mult))
                for c in range(DC):
                    kb.op("pe", [("hb", s), "identb"], [("tp", s)], lambda e, s=s, c=c: e.transpose(
                        out=tp[:, s, c, :], in_=hb_[:, s, c * 128:(c + 1) * 128], identity=identb[:]), inc=(c == DC - 1))
                kb.op("act", [("tp", s)], ["hT"], lambda e, s=s, tt=tt: e.copy(
                    out=hT[:, :, tt * 128:(tt + 1) * 128], in_=tp[:, s]))

            def perm_cols(gi, pp0, n):
                if gi is None or gi == 0:
                    return lambda c: hT[:, c, pp0:pp0 + n]
                dil = (1, 4, 16)[gi]
                U = S // dil
                r, u0 = pp0 // U, pp0 % U
                assert u0 + n <= U
                return lambda c: hT[:, c, r + dil * u0: r + dil * (u0 + n - 1) + 1: dil]

            wcount = [0]

            def load_w(segs):
                s = wcount[0] % 2
                wcount[0] += 1
                o = 0
                for (c0, n) in segs:
                    kb.dma("sp", wst[:, s, :, o:o + n], win_d[layer, :, c0:c0 + n].rearrange("(c p) n -> p c n", p=128),
                           writes=[("wst", s)], key=("wst", s, o))
                    o += n
                kb.op("pool", [("wst", s)], [("wbf", s)], lambda e, s=s, o=o: e.tensor_copy(
                    out=wbf[:, s, :, 0:o], in_=wst[:, s, :, 0:o]))
                return s, o

            it = [0]
            pend1 = [None]

            def fm_chunk(ws, wo, dst, post, gidx=None, gi=None):
                TB = 512 if gi in (None, 0) else min(512, S // (1, 4, 16)[gi])
                for tb in range(S // TB):
                    s = it[0] % 2
                    s3 = it[0] % 3
                    it[0] += 1
                    cols = perm_cols(gi, tb * TB, TB)
                    for c in range(DC):
                        kb.op("pe", [("wbf", ws), "hT"], [("p1", s3)], lambda e, c=c, s3=s3, cols=cols: e.matmul(
                            p1[:, s3, 0:TB], lhsT=wbf[:, ws, c, wo:wo + 128], rhs=cols(c), start=(c == 0), stop=(c == DC - 1)),
                            inc=(c == DC - 1))
                    store = lambda tb=tb, s3=s3: kb.dma("sp", dst[:, tb * TB:(tb + 1) * TB], ot[:, s3, 0:TB], reads=[("ot", s3)])
                    if post == "plain":
                        kb.op("act", [("p1", s3)], [("ot", s3)], lambda e, s3=s3: e.copy(out=ot[:, s3, 0:TB], in_=p1[:, s3, 0:TB]))
                        store()
                    elif post == "sigmoid":
                        kb.op("act", [("p1", s3)], [("ot", s3)], lambda e, s3=s3: e.activation(
                            out=ot[:, s3, 0:TB], in_=p1[:, s3, 0:TB], func=AF.Sigmoid))
                        store()
                    else:
                        kb.op("act", [("p1", s3)], [("sq", s)], lambda e, s=s, s3=s3: e.activation(
                            out=sq[:, s, 0:TB], in_=p1[:, s3, 0:TB], func=AF.Square))

                        def rest(s=s, s3=s3, store=store):
                            kb.op("pe", [("sq", s), "bdiag"], [("p2", s)], lambda e: e.matmul(
                                p2[:, s, 0:TB], lhsT=bdiag[:], rhs=sq[:, s, 0:TB], start=True, stop=True))
                            kb.op("act", [("p2", s)], [("sd", s)], lambda e: e.activation(
                                out=sd[:, s, 0:TB], in_=p2[:, s, 0:TB], func=AF.Sqrt, bias=epsc[:, 0:1], scale=1.0))
                            kb.op("dve", [("sd", s)], [("sd", s)], lambda e: e.reciprocal(out=sd[:, s, 0:TB], in_=sd[:, s, 0:TB]))
                            kb.op("dve", [("p1", s3), ("sd", s), "g6"], [("ot", s3)], lambda e: e.scalar_tensor_tensor(
                                out=ot[:, s3, 0:TB], in0=p1[:, s3, 0:TB], scalar=g6[:, gidx:gidx + 1], in1=sd[:, s, 0:TB],
                                op0=ALU.mult, op1=ALU.mult))
                            store()
                        prev = pend1[0]
                        pend1[0] = rest
                        if prev is not None:
                            prev()
                if pend1[0] is not None:
                    p_ = pend1[0]
                    pend1[0] = None
                    p_()

            def fm_group(chunks):
                def segs_of(b):
                    segs = []
                    for ch in chunks[b:b + 4]:
                        segs += ch[0]
                    return segs
                nxt = load_w(segs_of(0))
                for b in range(0, len(chunks), 4):
                    grp = chunks[b:b + 4]
                    ws, tot = nxt
                    if b + 4 < len(chunks):
                        nxt = load_w(segs_of(b + 4))
                    for k, ch in enumerate(grp):
                        fm_chunk(ws, 128 * k, ch[1], ch[2], ch[3], ch[4])

            def tm_group(c0, n, post, dst_fn, gi=None):
                ws, _ = load_w([(c0, n)])
                for t in range(NT):
                    s = it[0] % 2
                    it[0] += 1
                    cols = perm_cols(gi, t * 128, 128)
                    for c in range(DC):
                        kb.op("pe", [("wbf", ws), "hT"], [("p1", s)], lambda e, c=c, s=s, cols=cols: e.matmul(
                            p1[:, s, 0:n], lhsT=cols(c), rhs=wbf[:, ws, c, 0:n], start=(c == 0), stop=(c == DC - 1)),
                            inc=(c == DC - 1))
                    if post == "vaug":
                        nh = n // 64
                        kb.op("act", [("p1", s)], [("va", s)], lambda e, s=s, nh=nh: e.copy(
                            out=va[:, s, 0:nh, 0:64], in_=p1[:, s, 0:n].rearrange("p (h d) -> p h d", d=64)))
                        kb.dma("sp", dst_fn(t), va[:, s, 0:nh, :], reads=[("va", s)])
                    else:
                        if post == "sigf":
                            kb.op("act", [("p1", s)], [("otf", s)], lambda e, s=s: e.activation(
                                out=otf[:, s, 0:n], in_=p1[:, s, 0:n], func=AF.Sigmoid))
                        else:
                            kb.op("act", [("p1", s)], [("otf", s)], lambda e, s=s: e.copy(out=otf[:, s, 0:n], in_=p1[:, s, 0:n]))
                        kb.dma("sp", dst_fn(t), otf[:, s, 0:n], reads=[("otf", s)])

            chunks = []
            for c in range(4):
                chunks.append(([(o_aq + 128 * c, 128)], QA_d[c], "rms", 0, None))
            chunks.append(([(o_ak, 64), (o_ak, 64)], KA_d[0], "rms", 1, None))
            for c in range(2):
                chunks.append(([(o_iq + 128 * c, 128)], IQ_d[c], "plain", None, None))
            chunks.append(([(o_ik, 64), (o_ik, 64)], IK_d[0], "plain", None, None))
            for c in range(4):
                chunks.append(([(o_bq + 128 * c, 128)], QB_d[c], "rms", 2, None))
            for c in range(2):
                chunks.append(([(o_bkv + 128 * c, 128)], KCVC_d[c], "plain", None, None))
            for g in range(2):
                chunks.append(([(o_bkv + 256 + 64 * g, 64)] * 2, KS_d[g], "rms", 3, None))
            for g in range(2):
                chunks.append(([(o_bkv + 512 + 64 * g, 64)] * 2, KW_d[g], "rms", 3, None))
            for c in range(6):
                chunks.append(([(o_c + 128 * c, 128)], QC_d[c], "rms", 4, c // 2))
            for c in range(6):
                chunks.append(([(o_c + 768 + 128 * c, 128)], KCc_d[c], "rms", 5, c // 2))
            for c in range(GC):
                chunks.append(([(o_g + 128 * c, 128)], GT_d[c], "sigmoid", None, None))
            fm_group(chunks)
            tm_group(o_av, 64, "vaug", lambda t: VA_d[t * 128:(t + 1) * 128])
            tm_group(o_bkv + 384, 128, "vaug", lambda t: VS_d[t * 128:(t + 1) * 128])
            tm_group(o_bkv + 640, 128, "vaug", lambda t: VW_d[t * 128:(t + 1) * 128])
            for gi in range(3):
                tm_group(o_c + 1536 + 256 * gi, 256, "vaug", lambda t, gi=gi: VC_d[gi, t * 128:(t + 1) * 128], gi=gi)
            tm_group(o_iw, 4, "copyf", lambda t: IW_d[t * 128:(t + 1) * 128, :])
            tm_group(o_bg, 24, "sigf", lambda t: BG_d[t * 128:(t + 1) * 128, :])
            kb.barrier()

    def attn_tile(S_ps, s_slot, PT, pt_slot, O_ap_fn, bias_rhs, neg_lhsT, kq_list, v_rhs_fn, first, nk=128):
        sk = ("S_ps", s_slot)
        started = False
        if bias_rhs is not None:
            ap_, keys = bias_rhs
            kb.op("pe", ["identb"] + keys, [sk], lambda e: e.matmul(
                S_ps[0:nk, s_slot, :], lhsT=identb[0:nk, 0:nk], rhs=ap_, start=True, stop=False, skip_group_check=True), inc=False)
            started = True
        if neg_lhsT is not None:
            ap_, keys = neg_lhsT
            kb.op("pe", ["negI4"] + keys, [sk], lambda e: e.matmul(
                S_ps[0:nk, s_slot, :], lhsT=ap_, rhs=negI4[:].rearrange("p h f -> p (h f)"), start=not started, stop=False,
                skip_group_check=True), inc=False)
            started = True
        nq = len(kq_list)
        wq = 512 // nq
        for hh, (kT, qT, keys) in enumerate(kq_list):
            kb.op("pe", keys, [sk], lambda e, hh=hh, kT=kT, qT=qT, st_=(not started): e.matmul(
                S_ps[0:nk, s_slot, hh * wq:(hh + 1) * wq], lhsT=kT, rhs=qT, start=st_, stop=(hh == nq - 1),
                skip_group_check=True), inc=(hh == nq - 1))
            started = True
        perm = (0, 2, 1, 3) if nq == 2 else (0, 1, 2, 3)
        pk = ("PT", pt_slot)
        kb.op("act", [sk], [pk], lambda e: e.activation(
            out=PT[0:nk, pt_slot, :], in_=S_ps[0:nk, s_slot, :], func=AF.Exp, scale=0.125))

        def pv():
            for b_ in range(4):
                vap, oap, vkeys, okey, bfirst = v_rhs_fn(perm[b_])
                kb.op("pe", [pk] + vkeys, [okey], lambda e, b_=b_, vap=vap, oap=oap, bfirst=bfirst: e.matmul(
                    oap, lhsT=PT[0:nk, pt_slot, b_ * 128:(b_ + 1) * 128], rhs=vap, start=(first and bfirst), stop=False,
                    skip_group_check=True), inc=(b_ == 3))
        prev = pend[0]
        pend[0] = pv
        if prev is not None:
            prev()

    pend = [None]

    def attn_flush():
        if pend[0] is not None:
            p = pend[0]
            pend[0] = None
            p()

    def phase2A(layer):
        with ExitStack() as st:
            def sbl(name, shape, dt):
                return st.enter_context(sbt(name, list(shape), dt))
            QA = sbl("QA_s", [128, 4, S], BF16); KA = sbl("KA_s", [128, 2, S], BF16)
            IQ = sbl("IQ_s", [128, 2, S], BF16); IK = sbl("IK_s", [128, 2, S], BF16)
            VA = sbl("VA_s", [128, NT, 65], BF16); IW = sbl("IW_s", [128, NT, 4], F32)
            bA = sbl("bA_s", [128, ND, 8, 128], BF16)
            score = sbl("score", [128, 1, S], F32)
            negm = sbl("negm", [128, 2, S], BF16)
            junkb = sbl("junkb", [128, S], BF16)
            rt = sbl("rt", [128, 4, 512], F32)
            PT = sbl("PT", [128, 3, 512], BF16)
            cm = sbl("cm", [128, 128], F32)
            wv = sbl("wv", [128, 2, 16], F32)
            bis = sbl("bis", [128, 2, 8], F32)
            wk = sbl("wk", [128, 2, NBIS], F32)
            halves = sbl("halves", [128, NBIS], F32)
            rden = sbl("rden", [128, 2, 8], F32)
            oa = sbl("oa", [128, 2, 512], BF16)
            P_ps = st.enter_context(pst_("P_ps", [128, 2, 512], F32))
            S_ps = st.enter_context(pst_("S_ps", [128, 2, 512], F32))
            O_ps = st.enter_context(pst_("O_ps", [128, 2, 512], F32))

            kb.dma("sp", QA[:], QA_d.rearrange("c p s -> p c s"), writes=["QA"])
            kb.op("pool", [], ["KA"], lambda e: e.memset(KA[:], 0.0))
            kb.op("pool", [], ["IK"], lambda e: e.memset(IK[:], 0.0))
            for v in range(2):
                kb.dma("sp", KA[64 * v:64 * v + 64, v, :], KA_d[0, 64 * v:64 * v + 64, :], writes=["KA"], key=("KAl", v))
                kb.dma("sp", IK[64 * v:64 * v + 64, v, :], IK_d[0, 64 * v:64 * v + 64, :], writes=["IK"], key=("IKl", v))
            kb.dma("sp", IQ[:], IQ_d.rearrange("c p s -> p c s"), writes=["IQ"])
            kb.dma("sp", VA[:], VA_d.rearrange("(t p) o c -> p t (o c)", p=128), writes=["VA"])
            kb.dma("sp", IW[:], IW_d.rearrange("(t p) c -> p t c", p=128), writes=["IW"])
            kb.dma("sp", bA[:], bA_d, writes=["bA"])
            kb.op("pool", [], ["cm"], lambda e: e.memset(cm[:], 0.0))
            kb.op("pool", ["cm"], ["cm"], lambda e: e.affine_select(
                out=cm[:], in_=cm[:], pattern=[[-1, 128]], compare_op=ALU.is_ge, fill=reg_negbigf, base=0, channel_multiplier=1))
            for k in range(NBIS):
                kb.op("dve", [], ["halves"], lambda e, k=k: e.memset(halves[:, k:k + 1], 2.0 ** -(k + 1)))

            def idx_bis(i):
                L = 128 * (i + 1)
                s = i % 2
                sc = ("score", 0)
                qs = slice(i * 128, (i + 1) * 128)
                kb.op("act", ["IW"], [("wv", s)], lambda e, s=s, i=i: e.activation(
                    out=wv[:, s, 0:4], in_=IW[:, i, :], func=AF.Abs))
                kb.op("act", ["IW"], [("wv", s)], lambda e, s=s, i=i: e.activation(
                    out=wv[:, s, 4:8], in_=IW[:, i, :], func=AF.Sign))
                nkb = (L + 511) // 512
                for kbk in range(nkb):
                    k0 = kbk * 512
                    w = min(512, L - k0)
                    for h in range(4):
                        ps_ = (kbk * 4 + h) % 2
                        r4 = (kbk * 4 + h) % 4
                        kb.op("pe", ["IQ", "IK"], [("P_ps", ps_)], lambda e, h=h, ps_=ps_, k0=k0, w=w: e.matmul(
                            P_ps[:, ps_, 0:w], lhsT=IQ[:, h // 2, qs], rhs=IK[:, h % 2, k0:k0 + w], start=True, stop=True))
                        kb.op("act", [("P_ps", ps_), ("wv", s)], [("rt", r4)], lambda e, h=h, ps_=ps_, r4=r4, w=w, s=s: e.activation(
                            out=rt[:, r4, 0:w], in_=P_ps[:, ps_, 0:w], func=AF.Relu, scale=wv[:, s, h:h + 1]))
                        if h == 0:
                            kb.op("pool", [("rt", r4), ("wv", s)], [sc], lambda e, r4=r4, w=w, s=s, k0=k0: e.tensor_scalar(
                                out=score[:, 0, k0:k0 + w], in0=rt[:, r4, 0:w], scalar1=wv[:, s, 4:5], scalar2=None, op0=ALU.mult))
                        else:
                            kb.op("pool", [("rt", r4), ("wv", s)], [("rt", r4)], lambda e, r4=r4, w=w, s=s, h=h: e.tensor_scalar(
                                out=rt[:, r4, 0:w], in0=rt[:, r4, 0:w], scalar1=wv[:, s, 4 + h:5 + h], scalar2=None, op0=ALU.mult))
                            kb.op("pool", [("rt", r4), sc], [sc], lambda e, r4=r4, w=w, k0=k0: e.tensor_tensor(
                                out=score[:, 0, k0:k0 + w], in0=score[:, 0, k0:k0 + w], in1=rt[:, r4, 0:w], op=ALU.add))
                bk_ = ("bis", s)
                kb.op("dve", [sc], [bk_], lambda e, s=s, L=L: e.tensor_reduce(
                    out=bis[:, s, 0:1], in_=score[:, 0, 0:L], axis=AX.X, op=ALU.max, apply_absolute_value=True))
                kb.op("dve", [sc, "cm"], [sc], lambda e, s=s, i=i: e.tensor_tensor(
                    out=score[:, 0, qs], in0=score[:, 0, qs], in1=cm[:], op=ALU.add))
                kb.op("dve", [bk_], [bk_], lambda e, s=s: e.tensor_scalar(
                    out=bis[:, s, 1:2], in0=bis[:, s, 0:1], scalar1=2.002, scalar2=2e-6, op0=ALU.mult, op1=ALU.add))
                kb.op("dve", [bk_, "halves"], [("wk", s)], lambda e, s=s: e.tensor_scalar(
                    out=wk[:, s, :], in0=halves[:], scalar1=bis[:, s, 1:2], scalar2=None, op0=ALU.mult))
                kb.op("dve", [], [bk_], lambda e, s=s: e.memset(bis[:, s, 2:3], 0.0))
                for k in range(NBIS):
                    kb.op("dve", [sc, bk_], ["junkb", bk_], lambda e, s=s, L=L: e.tensor_scalar(
                        out=junkb[:, 0:L], in0=score[:, 0, 0:L], scalar1=bis[:, s, 2:3], scalar2=None,
                        op0=ALU.is_gt, op1=ALU.add, accum_out=bis[:, s, 3:4]))
                    kb.op("dve", [bk_], [bk_], lambda e, s=s: e.tensor_scalar(
                        out=bis[:, s, 4:5], in0=bis[:, s, 3:4], scalar1=float(TOPK), scalar2=0.5, op0=ALU.is_ge, op1=ALU.subtract))
                    kb.op("dve", [bk_, ("wk", s)], [bk_], lambda e, s=s, k=k: e.scalar_tensor_tensor(
                        out=bis[:, s, 2:3], in0=bis[:, s, 4:5], scalar=wk[:, s, k:k + 1], in1=bis[:, s, 2:3],
                        op0=ALU.mult, op1=ALU.add))
                nk_ = ("negm", s)
                kb.op("dve", [sc, bk_], [nk_], lambda e, s=s, L=L: e.tensor_scalar(
                    out=negm[:, s, 0:L], in0=score[:, 0, 0:L], scalar1=bis[:, s, 2:3], scalar2=None, op0=ALU.is_le))
            def att(i):
                L = 128 * (i + 1)
                s = i % 2
                qs = slice(i * 128, (i + 1) * 128)
                nk_ = ("negm", s)
                for j in range(i + 1):
                    d = min(i - j, ND - 1)
                    ks_ = slice(j * 128, (j + 1) * 128)
                    for hb in range(2):
                        ss_ = (i * 64 + j * 2 + hb) % 2
                        p3 = (i * 64 + j * 2 + hb) % 3
                        kq = []
                        for v in range(2):
                            kq.append((KA[:, v, ks_], QA[:, 2 * hb:2 * hb + 2, qs], ["KA", "QA"]))
                        attn_tile(S_ps, ss_, PT, p3,
                                  None,
                                  (bA[:, d, hb * 4:hb * 4 + 4, :].rearrange("p (c v) f -> p v c f", v=2), ["bA"]),
                                  (negm[:, s, ks_], [nk_]),
                                  kq,
                                  lambda hh, hb=hb, j=j: (VA[:, j, :], O_ps[:, hb, hh * 65:(hh + 1) * 65], ["VA"], ("O_ps", hb), hh == 0),
                                  first=(j == 0))
                attn_flush()
                okeys = [("O_ps", 0), ("O_ps", 1)]
                Ov = O_ps[:, :, 0:260].rearrange("p b (h c) -> p b h c", c=65)
                kb.op("dve", okeys, [("rden", s)], lambda e, s=s, Ov=Ov: e.reciprocal(
                    out=rden[:, s, :].rearrange("p (b h) -> p b h", b=2), in_=Ov[:, :, :, 64]))
                for hb in range(2):
                    kb.op("dve", [("O_ps", hb), ("rden", s)], [("oa", s)], lambda e, s=s, hb=hb, Ov=Ov: e.tensor_tensor(
                        out=oa[:, s, hb * 256:(hb + 1) * 256].rearrange("p (h c) -> p h c", c=64),
                        in0=Ov[:, hb, :, 0:64], in1=rden[:, s, hb * 4:hb * 4 + 4].unsqueeze(2).to_broadcast([128, 4, 64]),
                        op=ALU.mult))
                kb.dma("sp", AT_d[i * 128:(i + 1) * 128, 0:512], oa[:, s, :], reads=[("oa", s)])

            idx_bis(0)
            for i in range(NT):
                if i + 1 < NT:
                    idx_bis(i + 1)
                att(i)
            kb.barrier()


    def phase2B(layer):
        with ExitStack() as st:
            def sbl(name, shape, dt):
                return st.enter_context(sbt(name, list(shape), dt))
            NCP = NCC * 128
            QB = sbl("QB_s", [128, 2, S], BF16)
            KS = sbl("KS_s", [128, 2, S], BF16); KW = sbl("KW_s", [128, 2, S], BF16)
            VS = sbl("VS_s", [128, NT, 65], BF16); VW = sbl("VW_s", [128, NT, 65], BF16)
            BGs = sbl("BG_s", [128, NT, 8, 3], F32)
            bBs = sbl("bBs_s", [128, ND, 4, 128], BF16); bBw = sbl("bBw_s", [128, 5, 4, 128], BF16)
            kcT = sbl("kcT", [128, 2, 2, NCP], BF16)
            CVX = sbl("CVX", [128, 2, NCC, 129], BF16)
            Vw = sbl("Vw", [128, 2 * NSEL], F32); Fw = sbl("Fw", [128, 2 * NSEL], F32)
            zt = sbl("zt", [128, 4, 128], BF16)
            mct = sbl("mct", [128, 2, 4, 128], BF16)
            PT = sbl("PTb", [128, 3, 512], BF16)
            negmB = sbl("negmB", [128, 2, S], BF16)
            sm = sbl("smB", [128, 2, 32], F32)
            imp = sbl("imp", [128, 2, 64 * 4], F32)
            m8 = sbl("m8", [128, 2, 16], F32)
            t1 = sbl("t1", [128, 2, 256], F32); t2 = sbl("t2", [128, 2, 256], F32)
            ob = sbl("obB", [128, 2, 256], BF16)

            kb.dma("sp", BGs[:], BG_d.rearrange("(t p) (h b) -> p t h b", p=128, b=3), writes=["BGs"])
            kb.op("pool", [], ["KS"], lambda e: e.memset(KS[:], 0.0))
            kb.op("pool", [], ["KW"], lambda e: e.memset(KW[:], 0.0))
            kb.op("pool", [], ["zt"], lambda e: e.memset(zt[:], 0.0))
            kb.op("dve", [], ["Vw"], lambda e: e.memset(Vw[:], 0.0))
            kb.op("dve", [], ["Fw"], lambda e: e.memset(Fw[:], 0.0))
            for b in range(2):
                rows = slice(64 * b, 64 * b + 64)
                kb.op("dve", [], ["Vw"], lambda e, rows=rows, b=b: e.memset(Vw[rows, 0:NSEL + b - 1], 1.0))
                kb.op("dve", [], ["Fw"], lambda e, rows=rows, b=b: e.memset(Fw[rows, NSEL + b - 1:NSEL + b + 1], BIGF))
                kb.op("dve", [], ["Fw"], lambda e, rows=rows, b=b: e.memset(Fw[rows, NSEL + b + 1:2 * NSEL], -BIGF))

            with ExitStack() as st2:
                def sb2(name, shape, dt):
                    return st2.enter_context(sbt(name, list(shape), dt))
                KCVC = sb2("KCVC_s", [128, 2, S], BF16)
                wc32 = sb2("wc32", [128, 32, 64], F32)
                wc = sb2("wc", [128, 2, 2, 32, 64], BF16)
                pos32 = sb2("pos32", [128, 2, 32], F32)
                posB = sb2("posB", [128, 2, 32, 128], BF16)
                Gk = sb2("Gk", [128, 64], F32)
                ktm = sb2("ktm", [128, 2, 128], BF16)
                ov = sb2("ov", [128, NCC, 64], F32)
                cs = sb2("cs", [128, 2, 4], F32)
                cj = sb2("cj", [128, 64], F32)
                pc = st2.enter_context(pst_("pc", [128, 2, 512], F32))
                ptr = st2.enter_context(pst_("ptr", [128, 2, 1024], BF16))
                kb.dma("sp", KCVC[:], KCVC_d.rearrange("c p s -> p c s"), writes=["KCVC"])
                kb.dma("sp", Gk[:], qk_d[layer, 3:4, :].to_broadcast([128, 64]), writes=["Gk"])
                for half in range(2):
                    kb.dma("sp", pos32[64 * half:64 * half + 64], cpos_d[layer].rearrange("k l d -> d k l"),
                           writes=["pos32"], key=("pos32", half), allow_slow_non_contiguous=True)
                kb.op("pool", [], ["wc"], lambda e: e.memset(wc[:], 0.0))
                for kv in range(2):
                    for half in range(2):
                        kb.dma("sp", wc32[64 * half:64 * half + 64], cw_d[layer, kv].rearrange("l d e -> d l e"),
                               writes=["wc32"], key=("wc32", half))
                    for half in range(2):
                        kb.op("dve", ["wc32"], ["wc"], lambda e, kv=kv, half=half: e.tensor_copy(
                            out=wc[64 * half:64 * half + 64, half, kv], in_=wc32[64 * half:64 * half + 64]))
                kb.op("dve", ["pos32"], ["posB"], lambda e: e.tensor_copy(
                    out=posB[:].rearrange("p k l n -> p (k l) n"),
                    in_=pos32[:].rearrange("p k l -> p (k l)").unsqueeze(2).to_broadcast([128, 64, 128])))
                kb.op("pool", [], ["ov"], lambda e: e.memset(ov[:], 1.0))
                for c in range(NCC):
                    kb.op("pool", ["ov"], ["ov"], lambda e, c=c: e.affine_select(
                        out=ov[:, c, :], in_=ov[:, c, :], pattern=[[64, 64]], compare_op=ALU.is_gt, fill=reg_zero,
                        base=64 - 16 * 128 * c, channel_multiplier=-16))
                    kb.op("pool", ["ov"], ["ov"], lambda e, c=c: e.affine_select(
                        out=ov[:, c, :], in_=ov[:, c, :], pattern=[[-64, 64]], compare_op=ALU.is_gt, fill=reg_zero,
                        base=16 * 128 * c + 32, channel_multiplier=16))
                kb.op("dve", [], ["CVX"], lambda e: e.memset(CVX[:], 1.0))
                for g in range(2):
                    kb.op("dve", ["ov", "CVX"], ["CVX"], lambda e, g=g: e.tensor_copy(out=CVX[:, g, :, 65:129], in_=ov[:]))
                kb.op("dve", [], ["kcT"], lambda e: e.memset(kcT[:], 0.0))
                n_it = 0
                for kv in range(2):
                    for g in range(2):
                        rows = slice(64 * g, 64 * g + 64)
                        for c in range(NCC):
                            nv = min(128, NCMP - 128 * c)
                            s = n_it % 2
                            n_it += 1
                            for l in range(32):
                                t0 = 16 * 128 * c + l
                                kb.op("pe", ["KCVC", "wc"], [("pc", s)], lambda e, l=l, t0=t0, nv=nv, s=s, g=g, kv=kv: e.matmul(
                                    pc[0:nv, s, 0:64], lhsT=KCVC[:, kv, t0:t0 + 16 * (nv - 1) + 1:16], rhs=wc[:, g, kv, l, :],
                                    start=(l == 0), stop=False), inc=False)
                            for l in range(32):
                                kb.op("pe", ["posB", "wc"], [("pc", s)], lambda e, l=l, nv=nv, s=s, g=g, kv=kv: e.matmul(
                                    pc[0:nv, s, 0:64], lhsT=posB[:, kv, l, 0:nv], rhs=wc[:, g, kv, l, :],
                                    start=False, stop=(l == 31)), inc=(l == 31))
                            if kv == 1:
                                kb.op("act", [("pc", s)], ["CVX"], lambda e, nv=nv, s=s, g=g, c=c: e.copy(
                                    out=CVX[0:nv, g, c, 0:64], in_=pc[0:nv, s, 0:64]))
                            else:
                                kb.op("act", [("pc", s)], ["cj", ("cs", s)], lambda e, nv=nv, s=s: e.activation(
                                    out=cj[0:nv, :], in_=pc[0:nv, s, 0:64], func=AF.Square, accum_out=cs[0:nv, s, 0:1]))
                                kb.op("act", [("cs", s)], [("cs", s)], lambda e, nv=nv, s=s: e.activation(
                                    out=cs[0:nv, s, 1:2], in_=cs[0:nv, s, 0:1], func=AF.Sqrt, bias=epsc[0:nv, 0:1], scale=1.0 / 64))
                                kb.op("dve", [("cs", s)], [("cs", s)], lambda e, nv=nv, s=s: e.reciprocal(
                                    out=cs[0:nv, s, 2:3], in_=cs[0:nv, s, 1:2]))
                                kb.op("dve", [], [("ktm", s)], lambda e, s=s: e.memset(ktm[:, s, :], 0.0))
                                for dup in range(2):
                                    kb.op("dve", [("pc", s), ("cs", s), "Gk"], [("ktm", s)], lambda e, nv=nv, s=s, dup=dup: e.scalar_tensor_tensor(
                                        out=ktm[0:nv, s, 64 * dup:64 * dup + 64], in0=pc[0:nv, s, 0:64], scalar=cs[0:nv, s, 2:3],
                                        in1=Gk[0:nv, :], op0=ALU.mult, op1=ALU.mult))
                                kb.op("pe", [("ktm", s), "identb"], [("ptr", s)], lambda e, s=s: e.transpose(
                                    out=ptr[:, s, 0:128], in_=ktm[:, s, :], identity=identb[:]))
                                for v in range(2):
                                    kb.op("act", [("ptr", s)], ["kcT"], lambda e, s=s, g=g, c=c, v=v: e.copy(
                                        out=kcT[64 * v:64 * v + 64, g, v, 128 * c:128 * c + 128], in_=ptr[64 * v:64 * v + 64, s, 0:128]))
                kb.barrier()

            S_ps = st.enter_context(pst_("S_psB", [128, 2, 512], F32))
            OCU = st.enter_context(pst_("OCU", [128, 2, 512], F32))
            OS = st.enter_context(pst_("OS", [128, 512], F32))
            OW = st.enter_context(pst_("OW", [128, 512], F32))
            cnt = [0]

            def slots():
                cnt[0] += 1
                return cnt[0] % 2, cnt[0] % 3

            for g in range(2):
                kb.dma("sp", QB[:], QB_d[2 * g:2 * g + 2].rearrange("c p s -> p c s"), writes=["QB"])
                for v in range(2):
                    kb.dma("sp", KS[64 * v:64 * v + 64, v, :], KS_d[g, 64 * v:64 * v + 64, :], writes=["KS"], key=("KSl", v))
                    kb.dma("sp", KW[64 * v:64 * v + 64, v, :], KW_d[g, 64 * v:64 * v + 64, :], writes=["KW"], key=("KWl", v))
                kb.dma("sp", VS[:], VS_d[:, g, :].rearrange("(t p) c -> p t c", p=128), writes=["VS"])
                kb.dma("sp", VW[:], VW_d[:, g, :].rearrange("(t p) c -> p t c", p=128), writes=["VW"])
                kb.dma("sp", bBs[:], bBs_d[:, :, 4 * g:4 * g + 4, :], writes=["bBs"])
                kb.dma("sp", bBw[:], bBw_d[:, :, 4 * g:4 * g + 4, :], writes=["bBw"])
                for i in range(NT):
                    L = 128 * (i + 1)
                    qs = slice(i * 128, (i + 1) * 128)
                    so = i % 2
                    sg = i % 2
                    def kq_for(Ksrc, cols, nk=128, g=g):
                        out = []
                        for v in range(2):
                            out.append((Ksrc(v, cols), QB[:, 0:2, qs], ["QB", "KS", "KW", "kcT"]))
                        return out
                    cmax = min(NCC - 1, (128 * i + 96) // 2048)
                    for c in range(cmax + 1):
                        nv = min(128, NCMP - 128 * c)
                        o = 128 * i - 2048 * c - 31
                        s2, s3 = slots()
                        bias = None
                        if o < 16 * 127:
                            kb.op("pool", ["zt"], [("mct", s2)], lambda e, s2=s2, o=o: e.affine_select(
                                out=mct[:, s2], in_=zt[:], pattern=[[0, 4], [1, 128]], compare_op=ALU.is_ge, fill=reg_negbig,
                                base=o, channel_multiplier=-16))
                            bias = (mct[0:nv, s2].rearrange("p h f -> p (h f)"), [("mct", s2)])
                        attn_tile(S_ps, s2, PT, s3, None, bias, None,
                                  kq_for(lambda v, cols, g=g: kcT[:, g, v, cols], slice(128 * c, 128 * c + nv)),
                                  lambda hh, g=g, c=c, nv=nv: (CVX[0:nv, g, c, :], OCU[:, hh // 2, (hh % 2) * 129:(hh % 2) * 129 + 129],
                                                               ["CVX"], ("OCU", hh // 2), hh % 2 == 0),
                                  first=(c == 0), nk=nv)
                    attn_flush()
                    ock = [("OCU", 0), ("OCU", 1)]
                    OCv = OCU[:, :, 0:258].rearrange("p b (h c) -> p b h c", c=129)
                    smk = ("sm", sg)
                    kb.op("dve", ock, [smk], lambda e, sg=sg, OCv=OCv: e.tensor_scalar(
                        out=sm[:, sg, 0:4].rearrange("p (b h) -> p b h", b=2), in0=OCv[:, :, :, 64], scalar1=1e-30, scalar2=None, op0=ALU.max))
                    kb.op("dve", [smk], [smk], lambda e, sg=sg: e.reciprocal(out=sm[:, sg, 0:4], in_=sm[:, sg, 0:4]))
                    ik_ = ("imp", sg)
                    for hh in range(4):
                        Uh = OCU[:, hh // 2, (hh % 2) * 129 + 65:(hh % 2) * 129 + 65 + NSEL]
                        if hh == 0:
                            kb.op("dve", ock + [smk], [ik_], lambda e, sg=sg, Uh=Uh: e.tensor_scalar(
                                out=imp[:, sg, 0:NSEL], in0=Uh, scalar1=sm[:, sg, 0:1], scalar2=None, op0=ALU.mult))
                        else:
                            kb.op("dve", ock + [smk, ik_], [ik_], lambda e, sg=sg, Uh=Uh, hh=hh: e.scalar_tensor_tensor(
                                out=imp[:, sg, 0:NSEL], in0=Uh, scalar=sm[:, sg, hh:hh + 1], in1=imp[:, sg, 0:NSEL],
                                op0=ALU.mult, op1=ALU.add))
                    x0 = NSEL - 2 * i
                    kb.op("dve", [ik_, "Vw"], [ik_], lambda e, sg=sg, x0=x0: e.tensor_tensor(
                        out=imp[:, sg, 64:64 + NSEL], in0=imp[:, sg, 0:NSEL], in1=Vw[:, x0:x0 + NSEL], op=ALU.mult))
                    kb.op("dve", [ik_, "Fw"], [ik_], lambda e, sg=sg, x0=x0: e.tensor_tensor(
                        out=imp[:, sg, 64:64 + NSEL], in0=imp[:, sg, 64:64 + NSEL], in1=Fw[:, x0:x0 + NSEL], op=ALU.add))
                    kb.op("dve", [ik_], [ik_], lambda e, sg=sg: e.memset(imp[:, sg, 64:65], BIGF))
                    mk = ("m8", sg)
                    kb.op("dve", [ik_], [mk], lambda e, sg=sg: e.max(out=m8[:, sg, 0:8], in_=imp[:, sg, 64:64 + NSEL]))
                    kb.op("dve", [ik_, mk], [ik_], lambda e, sg=sg: e.match_replace(
                        out=imp[:, sg, 128:128 + NSEL], in_to_replace=m8[:, sg, 0:8], in_values=imp[:, sg, 64:64 + NSEL], imm_value=-3.0e38))
                    kb.op("dve", [ik_], [mk], lambda e, sg=sg: e.max(out=m8[:, sg, 8:16], in_=imp[:, sg, 128:128 + NSEL]))
                    kb.op("dve", [ik_, mk], [ik_], lambda e, sg=sg: e.tensor_scalar(
                        out=imp[:, sg, 192:192 + NSEL], in0=imp[:, sg, 64:64 + NSEL], scalar1=m8[:, sg, 15:16], scalar2=None, op0=ALU.is_lt))
                    nbk = ("negmB", sg)
                    nb_ = 2 * (i + 1)
                    kb.op("dve", [ik_], [nbk], lambda e, sg=sg, nb_=nb_, L=L: e.tensor_copy(
                        out=negmB[:, sg, 0:L].rearrange("p (m k) -> p m k", k=64),
                        in_=imp[:, sg, 192:192 + nb_].unsqueeze(2).to_broadcast([128, nb_, 64])))
                    for j in range(i + 1):
                        d = min(i - j, ND - 1)
                        ks_ = slice(j * 128, (j + 1) * 128)
                        s2, s3 = slots()
                        attn_tile(S_ps, s2, PT, s3, None,
                                  (bBs[:, d].rearrange("p (c v) f -> p v c f", v=2), ["bBs"]),
                                  (negmB[:, sg, ks_], [nbk]),
                                  kq_for(lambda v, cols: KS[:, v, cols], ks_),
                                  lambda hh, j=j: (VS[:, j, :], OS[:, hh * 65:(hh + 1) * 65], ["VS"], "OS", hh == 0),
                                  first=(j == 0))
                    j0 = max(0, i - 4)
                    for j in range(j0, i + 1):
                        ks_ = slice(j * 128, (j + 1) * 128)
                        s2, s3 = slots()
                        attn_tile(S_ps, s2, PT, s3, None,
                                  (bBw[:, i - j].rearrange("p (c v) f -> p v c f", v=2), ["bBw"]),
                                  None,
                                  kq_for(lambda v, cols: KW[:, v, cols], ks_),
                                  lambda hh, j=j: (VW[:, j, :], OW[:, hh * 65:(hh + 1) * 65], ["VW"], "OW", hh == 0),
                                  first=(j == j0))
                    attn_flush()
                    OSv = OS[:, 0:260].rearrange("p (h c) -> p h c", c=65)
                    OWv = OW[:, 0:260].rearrange("p (h c) -> p h c", c=65)
                    kb.op("dve", ["OS"], [smk], lambda e, sg=sg, OSv=OSv: e.reciprocal(out=sm[:, sg, 4:8], in_=OSv[:, :, 64]))
                    kb.op("dve", ["OW"], [smk], lambda e, sg=sg, OWv=OWv: e.reciprocal(out=sm[:, sg, 8:12], in_=OWv[:, :, 64]))
                    for br in range(3):
                        kb.op("dve", [smk, "BGs"], [smk], lambda e, sg=sg, br=br, g=g, i=i: e.tensor_tensor(
                            out=sm[:, sg, 12 + 4 * br:16 + 4 * br], in0=sm[:, sg, 4 * br:4 * br + 4], in1=BGs[:, i, 4 * g:4 * g + 4, br], op=ALU.mult))
                    def cf(br, sg=sg):
                        return sm[:, sg, 12 + 4 * br:16 + 4 * br].unsqueeze(2).to_broadcast([128, 4, 64])
                    t1v = t1[:, sg, :].rearrange("p (h c) -> p h c", c=64)
                    t2v = t2[:, sg, :].rearrange("p (h c) -> p h c", c=64)
                    kb.op("dve", ock + [smk], [("t1", sg)], lambda e, t1v=t1v, OCv=OCv, cf=cf: e.tensor_tensor(
                        out=t1v.rearrange("p (b h) c -> p b h c", b=2), in0=OCv[:, :, :, 0:64],
                        in1=cf(0).rearrange("p (b h) c -> p b h c", b=2), op=ALU.mult))
                    kb.op("dve", ["OS", smk], [("t2", sg)], lambda e, t2v=t2v, OSv=OSv, cf=cf: e.tensor_tensor(
                        out=t2v, in0=OSv[:, :, 0:64], in1=cf(1), op=ALU.mult))
                    kb.op("pool", [("t1", sg), ("t2", sg)], [("t1", sg)], lambda e, sg=sg: e.tensor_tensor(
                        out=t1[:, sg, :], in0=t1[:, sg, :], in1=t2[:, sg, :], op=ALU.add))
                    kb.op("dve", ["OW", smk], [("t2", sg)], lambda e, t2v=t2v, OWv=OWv, cf=cf: e.tensor_tensor(
                        out=t2v, in0=OWv[:, :, 0:64], in1=cf(2), op=ALU.mult))
                    kb.op("pool", [("t1", sg), ("t2", sg)], [("ob", so)], lambda e, sg=sg, so=so, g=g: e.tensor_tensor(
                        out=ob[:, so, :], in0=t1[:, sg, :], in1=t2[:, sg, :], op=ALU.add))
                    kb.dma("sp", AT_d[i * 128:(i + 1) * 128, 512 + 256 * g:768 + 256 * g], ob[:, so, :], reads=[("ob", so)])
            kb.barrier()

    def phase2C(layer):
        with ExitStack() as st:
            def sbl(name, shape, dt):
                return st.enter_context(sbt(name, list(shape), dt))
            QC = sbl("QC_s", [128, 2, S], BF16); KC = sbl("KC_s", [128, 2, 2, S], BF16)
            VC = sbl("VC_s", [128, NT, 4, 65], BF16)
            bC = sbl("bC_s", [128, 3, 2, 4, 128], BF16)
            PT = sbl("PTc", [128, 3, 512], BF16)
            osb = sbl("osb", [128, 2, 260], F32)
            S_ps = st.enter_context(pst_("S_psC", [128, 2, 512], F32))
            O_ps = st.enter_context(pst_("O_psC", [128, 2, 512], F32))
            kb.dma("sp", bC[:], bC_d, writes=["bC"])
            kb.op("pool", [], ["KC"], lambda e: e.memset(KC[:], 0.0))
            n = 0
            for gi, dil in enumerate((1, 4, 16)):
                U = S // dil
                UT = U // 128
                kb.dma("sp", QC[:], QC_d[2 * gi:2 * gi + 2].rearrange("c p s -> p c s"), writes=["QC"])
                for v in range(2):
                    kb.dma("sp", KC[64 * v:64 * v + 64, :, v, :], KCc_d[2 * gi:2 * gi + 2, 64 * v:64 * v + 64, :].rearrange("c p s -> p c s"),
                           writes=["KC"], key=("KCl", v))
                kb.dma("sp", VC[:], VC_d[gi].rearrange("(t p) h c -> p t h c", p=128), writes=["VC"])
                ocv = OC_d[gi].rearrange("(u r) h c -> r u (h c)", r=dil)
                for pt in range(NT):
                    r, ui = pt // UT, pt % UT
                    so = pt % 2
                    qs = slice(pt * 128, (pt + 1) * 128)
                    first = True
                    for dj in (1, 0):
                        if ui - dj < 0:
                            continue
                        kt = pt - dj
                        ks_ = slice(kt * 128, (kt + 1) * 128)
                        n += 1
                        kq = []
                        for hh in range(4):
                            kq.append((KC[:, hh // 2, hh % 2, ks_], QC[:, hh // 2, qs], ["KC", "QC"]))
                        attn_tile(S_ps, n % 2, PT, n % 3, None,
                                  (bC[:, gi, dj].rearrange("p h f -> p (h f)"), ["bC"]), None, kq,
                                  lambda hh, kt=kt, so=so: (VC[:, kt, hh, :], O_ps[:, so, hh * 65:(hh + 1) * 65], ["VC"], ("O_psC", so), hh == 0),
                                  first=first)
                        first = False
                    attn_flush()
                    kb.op("act", [("O_psC", so)], [("osb", so)], lambda e, so=so: e.copy(out=osb[:, so, :], in_=O_ps[:, so, 0:260]))
                    kb.dma("sp", ocv[r, ui * 128:(ui + 1) * 128, :], osb[:, so, :], reads=[("osb", so)])
            kb.barrier()
            oc3 = sbl("oc3", [128, 2, 3, 260], F32)
            rd = sbl("rdC", [128, 2, 4], F32)
            oc = sbl("ocC", [128, 2, 256], BF16)
            for tt in range(NT):
                s = tt % 2
                kb.dma("sp", oc3[:, s], OC_d[:, tt * 128:(tt + 1) * 128].rearrange("g p h c -> p g (h c)"), writes=[("oc3", s)])
                kb.op("dve", [("oc3", s)], [("oc3", s)], lambda e, s=s: e.tensor_tensor(
                    out=oc3[:, s, 0], in0=oc3[:, s, 0], in1=oc3[:, s, 1], op=ALU.add))
                kb.op("dve", [("oc3", s)], [("oc3", s)], lambda e, s=s: e.tensor_tensor(
                    out=oc3[:, s, 0], in0=oc3[:, s, 0], in1=oc3[:, s, 2], op=ALU.add))
                v0 = oc3[:, s, 0].rearrange("p (h c) -> p h c", c=65)
                kb.op("dve", [("oc3", s)], [("rdC", s)], lambda e, s=s, v0=v0: e.reciprocal(out=rd[:, s, :], in_=v0[:, :, 64]))
                kb.op("dve", [("oc3", s), ("rdC", s)], [("ocC", s)], lambda e, s=s, v0=v0: e.tensor_tensor(
                    out=oc[:, s, :].rearrange("p (h c) -> p h c", c=64), in0=v0[:, :, 0:64],
                    in1=rd[:, s, :].unsqueeze(2).to_broadcast([128, 4, 64]), op=ALU.mult))
                kb.dma("sp", AT_d[tt * 128:(tt + 1) * 128, 1024:1280], oc[:, s, :], reads=[("ocC", s)])
            kb.barrier()


    def load_cast(st_pool, dst, src_ap, key, nparts=128):
        stg, = st_pool
        shp = dst.shape
        A, Bn = shp[1], shp[2]
        per = max(1, 2048 // Bn)
        for a0 in range(0, A, per):
            a1 = min(A, a0 + per)
            sl = getattr(load_cast, "n", 0) % 2
            load_cast.n = getattr(load_cast, "n", 0) + 1
            kb.dma("sp", stg[0:nparts, sl, 0:(a1 - a0) * Bn].rearrange("p (a b) -> p a b", b=Bn), src_ap[:, a0:a1, :],
                   writes=[("stg", sl)])
            kb.op("pool", [("stg", sl)], [key], lambda e, a0=a0, a1=a1, sl=sl: e.tensor_copy(
                out=dst[0:nparts, a0:a1, :], in_=stg[0:nparts, sl, 0:(a1 - a0) * Bn].rearrange("p (a b) -> p a b", b=Bn)))

    def phase3(layer, xin_d):
        with ExitStack() as st:
            def sbl(name, shape, dt):
                return st.enter_context(sbt(name, list(shape), dt))
            stg = sbl("stg3", [128, 2, 2048], F32)
            Wb = sbl("Wb", [128, 10, D], BF16)
            Wo = sbl("Wo", [128, DC, D], BF16)
            att = sbl("att", [128, 2, 1280], BF16)
            attT = sbl("attT", [128, 10, 512], BF16)
            gt = sbl("gt", [128, GC, 512], BF16)
            m1 = sbl("m1", [128, 2, 512], F32); m2 = sbl("m2", [128, 2, 512], F32)
            mg = sbl("mg", [128, DC, 512], BF16)
            xt = sbl("xt3", [128, 2, D], F32)
            tpa = st.enter_context(pst_("tpa", [128, 2, 8, 128], BF16))
            yp = st.enter_context(pst_("yp", [128, 3, 512], F32))
            op_ = st.enter_context(pst_("op3", [128, 2, 512], F32))
            load_cast((stg,), Wb[:, 0:4, :], wba_d[layer].rearrange("(c p) n -> p c n", p=128), "Wb")
            load_cast((stg,), Wb[:, 4:8, :], wbb_d[layer].rearrange("(c p) n -> p c n", p=128), "Wb")
            load_cast((stg,), Wb[:, 8:10, :], wbc_d[layer].rearrange("(c p) n -> p c n", p=128), "Wb")
            load_cast((stg,), Wo[:], wout_d[layer].rearrange("(c p) n -> p c n", p=128), "Wo")
            nt4 = 0
            for tb in range(S // 512):
                ts_ = slice(tb * 512, (tb + 1) * 512)
                kb.dma("sp", gt[:], GT_d[:, :, ts_].rearrange("c p s -> p c s"), writes=["gt"])
                for t4 in range(4):
                    tt = tb * 4 + t4
                    s = tt % 2
                    kb.dma("sp", att[:, s, :], AT_d[tt * 128:(tt + 1) * 128, :], writes=[("att", s)])
                    for hf in range(2):
                        for c in range(5):
                            kb.op("pe", [("att", s), "identb"], [("tpa", hf)], lambda e, s=s, c=c, hf=hf: e.transpose(
                                out=tpa[:, hf, c, :], in_=att[:, s, (hf * 5 + c) * 128:(hf * 5 + c + 1) * 128], identity=identb[:]),
                                inc=(c == 4))
                        kb.op("act", [("tpa", hf)], ["attT"], lambda e, hf=hf, t4=t4: e.copy(
                            out=attT[:, hf * 5:hf * 5 + 5, t4 * 128:(t4 + 1) * 128], in_=tpa[:, hf, 0:5, :]))
                for fo in range(DC):
                    fs = slice(fo * 128, (fo + 1) * 128)
                    s = fo % 2
                    for bi, (c0, c1) in enumerate(((0, 4), (4, 8), (8, 10))):
                        for c in range(c0, c1):
                            kb.op("pe", ["Wb", "attT"], [("yp", bi)], lambda e, c=c, bi=bi, c0=c0, c1=c1, fs=fs: e.matmul(
                                yp[:, bi, :], lhsT=Wb[:, c, fs], rhs=attT[:, c, :], start=(c == c0), stop=(c == c1 - 1)),
                                inc=(c == c1 - 1))
                    kb.op("dve", [("yp", 0), "gt"], [("m1", s)], lambda e, s=s, fo=fo: e.tensor_tensor(
                        out=m1[:, s, :], in0=yp[:, 0, :], in1=gt[:, fo, :], op=ALU.mult))
                    kb.op("dve", [("yp", 1), "gt"], [("m2", s)], lambda e, s=s, fo=fo: e.tensor_tensor(
                        out=m2[:, s, :], in0=yp[:, 1, :], in1=gt[:, DC + fo, :], op=ALU.mult))
                    kb.op("pool", [("m1", s), ("m2", s)], [("m1", s)], lambda e, s=s: e.tensor_tensor(
                        out=m1[:, s, :], in0=m1[:, s, :], in1=m2[:, s, :], op=ALU.add))
                    kb.op("dve", [("yp", 2), "gt"], [("m2", s)], lambda e, s=s, fo=fo: e.tensor_tensor(
                        out=m2[:, s, :], in0=yp[:, 2, :], in1=gt[:, 2 * DC + fo, :], op=ALU.mult))
                    kb.op("pool", [("m1", s), ("m2", s)], ["mg"], lambda e, s=s, fo=fo: e.tensor_tensor(
                        out=mg[:, fo, :], in0=m1[:, s, :], in1=m2[:, s, :], op=ALU.add))
                for t4 in range(4):
                    tt = tb * 4 + t4
                    s = tt % 2
                    kb.dma("sp", xt[:, s, :], xin_d[tt * 128:(tt + 1) * 128, :], writes=[("xt3", s)])
                    for cb in range(0, D, 512):
                        cw_ = min(512, D - cb)
                        nt4 += 1
                        so = nt4 % 2
                        for fo in range(DC):
                            kb.op("pe", ["mg", "Wo"], [("op3", so)], lambda e, fo=fo, so=so, t4=t4, cb=cb, cw_=cw_: e.matmul(
                                op_[:, so, 0:cw_], lhsT=mg[:, fo, t4 * 128:(t4 + 1) * 128], rhs=Wo[:, fo, cb:cb + cw_],
                                start=(fo == 0), stop=(fo == DC - 1)), inc=(fo == DC - 1))
                        kb.op("dve", [("op3", so), ("xt3", s)], [("xt3", s)], lambda e, so=so, s=s, cb=cb, cw_=cw_: e.tensor_tensor(
                            out=xt[:, s, cb:cb + cw_], in0=op_[:, so, 0:cw_], in1=xt[:, s, cb:cb + cw_], op=ALU.add))
                    kb.dma("sp", x1_d[tt * 128:(tt + 1) * 128, :], xt[:, s, :], reads=[("xt3", s)], key=("x1st", s))
            kb.barrier()

    def phase4(layer, xout_d):
        with ExitStack() as st:
            def sbl(name, shape, dt):
                return st.enter_context(sbt(name, list(shape), dt))
            TB = 256
            stg = sbl("stg4", [128, 2, 2048], F32)
            Wfi = sbl("Wfi", [128, DC, 2 * DFF], BF16)
            Wfo = sbl("Wfo", [128, FC, D], BF16)
            G2 = sbl("G2", [128, D], F32)
            xt = sbl("xt4", [128, 2, D], F32)
            junk = sbl("junk4", [128, D], BF16)
            hb_ = sbl("hb4", [128, 2, D], BF16)
            ss = sbl("ss4", [128, 2, 4], F32)
            h2T = sbl("h2T", [128, DC, TB], BF16)
            sg = sbl("sg", [128, 2, TB], F32)
            actT = sbl("actT", [128, FC, TB], BF16)
            tp = st.enter_context(pst_("tp4", [128, 2, DC, 128], BF16))
            gp = st.enter_context(pst_("gp", [128, 2, 512], F32))
            up = st.enter_context(pst_("up", [128, 2, 512], F32))
            op_ = st.enter_context(pst_("op4", [128, 2, 512], F32))
            kb.dma("sp", G2[:], n2_d[layer:layer + 1, :].to_broadcast([128, D]), writes=["G2"])
            wfi_v = wfi_d[layer].rearrange("(c p) n -> p c n", p=128)
            for n0 in range(0, DFF, 256):
                n1 = min(DFF, n0 + 256)
                for half in range(2):
                    o_ = half * DFF
                    load_cast((stg,), Wfi[:, :, o_ + n0:o_ + n1], wfi_v[:, :, o_ + n0:o_ + n1], ("Wfi", half, n0 // 256))
            wfo_v = wfo_d[layer].rearrange("(c p) n -> p c n", p=128)
            for c0 in range(0, FC, 2):
                c1 = min(FC, c0 + 2)
                load_cast((stg,), Wfo[:, c0:c1, :], wfo_v[:, c0:c1, :], ("Wfo", c0 // 2))
            nt4 = 0
            for tb in range(S // TB):
                for t4 in range(TB // 128):
                    tt = tb * (TB // 128) + t4
                    s = t4 % 2
                    kb.dma("act", xt[:, s], x1_d[tt * 128:(tt + 1) * 128, :], writes=[("xt4", s)])
                    kb.op("act", [("xt4", s)], ["junk4", ("ss4", s)], lambda e, s=s: e.activation(
                        out=junk[:], in_=xt[:, s], func=AF.Square, accum_out=ss[:, s, 0:1]))
                    kb.op("act", [("ss4", s)], [("ss4", s)], lambda e, s=s: e.activation(
                        out=ss[:, s, 1:2], in_=ss[:, s, 0:1], func=AF.Sqrt, bias=epsc[:, 0:1], scale=1.0 / D))
                    kb.op("dve", [("ss4", s)], [("ss4", s)], lambda e, s=s: e.reciprocal(out=ss[:, s, 2:3], in_=ss[:, s, 1:2]))
                    kb.op("dve", [("xt4", s), ("ss4", s), "G2"], [("hb4", s)], lambda e, s=s: e.scalar_tensor_tensor(
                        out=hb_[:, s], in0=xt[:, s], scalar=ss[:, s, 2:3], in1=G2[:], op0=ALU.mult, op1=ALU.mult))
                    for c in range(DC):
                        kb.op("pe", [("hb4", s), "identb"], [("tp4", s)], lambda e, s=s, c=c: e.transpose(
                            out=tp[:, s, c, :], in_=hb_[:, s, c * 128:(c + 1) * 128], identity=identb[:]), inc=(c == DC - 1))
                    kb.op("act", [("tp4", s)], ["h2T"], lambda e, s=s, t4=t4: e.copy(
                        out=h2T[:, :, t4 * 128:(t4 + 1) * 128], in_=tp[:, s]))
                for fc in range(FC):
                    s = fc % 2
                    for c in range(DC):
                        kb.op("pe", [("Wfi", 0, fc // 2), "h2T"], [("gp", s)], lambda e, c=c, s=s, fc=fc: e.matmul(
                            gp[:, s, 0:TB], lhsT=Wfi[:, c, fc * 128:(fc + 1) * 128], rhs=h2T[:, c, :], start=(c == 0), stop=(c == DC - 1)),
                            inc=(c == DC - 1))
                    for c in range(DC):
                        kb.op("pe", [("Wfi", 1, fc // 2), "h2T"], [("up", s)], lambda e, c=c, s=s, fc=fc: e.matmul(
                            up[:, s, 0:TB], lhsT=Wfi[:, c, DFF + fc * 128:DFF + (fc + 1) * 128], rhs=h2T[:, c, :], start=(c == 0),
                            stop=(c == DC - 1)), inc=(c == DC - 1))
                    kb.op("act", [("gp", s)], [("sg", s)], lambda e, s=s: e.activation(
                        out=sg[:, s, :], in_=gp[:, s, 0:TB], func=AF.Silu))
                    kb.op("dve", [("up", s), ("sg", s)], ["actT"], lambda e, s=s, fc=fc: e.tensor_tensor(
                        out=actT[:, fc, :], in0=up[:, s, 0:TB], in1=sg[:, s, :], op=ALU.mult))
                for t4 in range(TB // 128):
                    tt = tb * (TB // 128) + t4
                    s = t4 % 2
                    for cb in range(0, D, 512):
                        cw_ = min(512, D - cb)
                        nt4 += 1
                        so = nt4 % 2
                        for fc in range(FC):
                            kb.op("pe", ["actT", ("Wfo", fc // 2)], [("op4", so)], lambda e, fc=fc, so=so, t4=t4, cb=cb, cw_=cw_: e.matmul(
                                op_[:, so, 0:cw_], lhsT=actT[:, fc, t4 * 128:(t4 + 1) * 128], rhs=Wfo[:, fc, cb:cb + cw_],
                                start=(fc == 0), stop=(fc == FC - 1)), inc=(fc == FC - 1))
                        kb.op("dve", [("op4", so), ("xt4", s)], [("xt4", s)], lambda e, so=so, s=s, cb=cb, cw_=cw_: e.tensor_tensor(
                            out=xt[:, s, cb:cb + cw_], in0=op_[:, so, 0:cw_], in1=xt[:, s, cb:cb + cw_], op=ALU.add))
                    kb.dma("sp", xout_d[tt * 128:(tt + 1) * 128, :], xt[:, s, :], reads=[("xt4", s)], key=("x2st", s))
            kb.barrier()

    LIMIT = getattr(build_nc, "limit", "all")
    NL = getattr(build_nc, "nlayers", 2)
    x2_d = dscr("x2", [S, D], F32)
    if LIMIT == "all":
        for layer in range(NL):
            xin = x_d if layer == 0 else x2_d
            xout = y_d if layer == NL - 1 else x2_d
            phase1(layer, xin)
            phase2A(layer)
            phase2B(layer)
            phase2C(layer)
            phase3(layer, xin)
            phase4(layer, xout)
    else:
        if LIMIT not in ("none", "setup"):
            phase1(0, x_d)
        if LIMIT == "A":
            phase2A(0)
        if LIMIT == "B":
            phase2B(0)
        if LIMIT == "C":
            phase2C(0)
        with ExitStack() as st:
            yt = st.enter_context(sbt("yt", [128, D], F32))
            for tt in range(NT):
                kb.dma("sp", yt[:], x_d[tt * 128:(tt + 1) * 128, :], writes=["yt"])
                kb.dma("sp", y_d[tt * 128:(tt + 1) * 128, :], yt[:], reads=["yt"], key="ystore")
            kb.barrier()
    stack.close()
    return nc


_NC_CACHE = {}


def kernel(**inputs):
    x = np.asarray(inputs["x"], dtype=np.float32)
    B, S, D = x.shape
    DFF = int(np.asarray(inputs["w_ffn_out"]).shape[1])
    key = (S, D, DFF)
    if key not in _NC_CACHE:
        _NC_CACHE[key] = build_nc(S, D, DFF)
    nc = _NC_CACHE[key]
    consts = make_consts()
    shared = {k: np.ascontiguousarray(np.asarray(v, dtype=np.float32)) for k, v in inputs.items() if k != "x"}
    shared.update(consts)
    in_maps = []
    for b in range(B):
        m = dict(shared)
        m["x"] = np.ascontiguousarray(x[b])
        in_maps.append(m)
    res = run_bass_kernel_spmd(nc, in_maps, core_ids=list(range(B)))
    return np.stack([np.asarray(r["y"], dtype=np.float32) for r in res.results], axis=0)
```

```python
import math
from contextlib import ExitStack
import numpy as np
import concourse.bass as bass
import concourse.mybir as mybir
from concourse.bass_types import AP
from concourse.bass_utils import run_bass_kernel_spmd

F32 = mybir.dt.float32
BF16 = mybir.dt.bfloat16
AF = mybir.ActivationFunctionType
ALU = mybir.AluOpType
AX = mybir.AxisListType

BIG = 30000.0
BIGF = 1.0e30
EPS = 1e-6
NBIS = 14
N_CORES = 8


def _rel_bucket_np(dist):
    n = np.maximum(dist, 0)
    nf = np.maximum(n, 1).astype(np.float32)
    large = 16 + (np.log(nf / np.float32(16)) / np.float32(math.log(2048 / 16)) * np.float32(16)).astype(np.int32)
    return np.where(n < 16, n, np.minimum(large, 31))


def make_consts():
    c = {}
    c["ident"] = np.eye(128, dtype=np.float32)
    c["flipj"] = np.eye(128, dtype=np.float32)[::-1].copy()
    LX = 1920
    dist = np.arange(LX) - 127
    oh = np.zeros((33, LX), np.float32)
    bk = _rel_bucket_np(dist)
    for x in range(LX):
        if dist[x] >= 0:
            oh[bk[x], x] = 1
        else:
            oh[32, x] = 1
    c["oh_ab"] = oh
    LW = 768
    dist = np.arange(LW) - 127
    oh = np.zeros((33, LW), np.float32)
    bk = _rel_bucket_np(dist)
    for x in range(LW):
        if 0 <= dist[x] < 512:
            oh[bk[x], x] = 1
        else:
            oh[32, x] = 1
    c["oh_w"] = oh
    LC = 384
    ohc = np.zeros((3, 33, LC), np.float32)
    for gi, dil in enumerate((1, 4, 16)):
        e = np.arange(LC) - 127
        bk = _rel_bucket_np(e * dil)
        for x in range(LC):
            if 0 <= e[x] <= 128:
                ohc[gi, bk[x], x] = 1
            else:
                ohc[gi, 32, x] = 1
    c["oh_c"] = ohc
    return c


class KB:
    def __init__(self, nc, stack):
        self.nc = nc
        self.stack = stack
        self.E = {"pe": nc.tensor, "act": nc.scalar, "dve": nc.vector, "pool": nc.gpsimd, "sp": nc.sync}
        self.sems = []
        self.semval = []
        self.free = []
        self.esem = {}
        for e in ("pe", "act", "dve", "pool"):
            self.esem[e] = self._alloc()
        self.seen = {e: {} for e in self.E}
        self.lastw = {}
        self.lastr = {}
        self.dsem = {}

    def _alloc(self):
        if self.free:
            return self.free.pop()
        h = self.stack.enter_context(self.nc.semaphore(f"sm{len(self.sems)}"))
        self.sems.append(h)
        self.semval.append(0)
        return len(self.sems) - 1

    def _wait(self, eng, si, v):
        if v <= 0 or self.seen[eng].get(si, 0) >= v:
            return
        self.E[eng].wait_ge(self.sems[si], v)
        self.seen[eng][si] = v

    def _deps(self, eng, reads, writes):
        need = {}
        for k in reads:
            w = self.lastw.get(k)
            if w:
                need[w[0]] = max(need.get(w[0], 0), w[1])
        for k in writes:
            w = self.lastw.get(k)
            if w:
                need[w[0]] = max(need.get(w[0], 0), w[1])
            for si, v in self.lastr.get(k, {}).items():
                need[si] = max(need.get(si, 0), v)
        for si, v in need.items():
            if eng == "pe" and si == self.esem["pe"]:
                continue
            if si == self.esem.get(eng) and v > self.semval[si]:
                continue
            self._wait(eng, si, v)

    def _mark(self, si, v, reads, writes):
        for k in reads:
            d = self.lastr.setdefault(k, {})
            d[si] = max(d.get(si, 0), v)
        for k in writes:
            self.lastw[k] = (si, v)
            self.lastr[k] = {}

    def op(self, eng, reads, writes, fn, inc=True):
        self._deps(eng, reads, writes)
        ins = fn(self.E[eng])
        si = self.esem[eng]
        if inc:
            ins.then_inc(self.sems[si], 1)
            self.semval[si] += 1
            v = self.semval[si]
        else:
            v = self.semval[si] + 1
        self._mark(si, v, reads, writes)
        return ins

    def dma(self, q, out, in_, reads=(), writes=(), key=None, **kw):
        self._deps(q, reads, writes)
        k = key if key is not None else (writes[0] if writes else reads[0])
        si = self.dsem.get(k)
        if si is None:
            si = self._alloc()
            self.dsem[k] = si
        self._wait(q, si, self.semval[si])
        ins = self.E[q].dma_start(out=out, in_=in_, **kw)
        ins.then_inc(self.sems[si], 16)
        self.semval[si] += 16
        self._mark(si, self.semval[si], reads, writes)

    def barrier(self):
        used = list(self.esem.values()) + list(self.dsem.values())
        for eng in self.E:
            for si in used:
                self._wait(eng, si, self.semval[si])
        for si in self.dsem.values():
            self.free.append(si)
        self.dsem = {}
        self.lastw = {}
        self.lastr = {}


def build_nc(S, D, DFF, debug=False):
    NT = S // 128
    DC = D // 128
    FC = DFF // 128
    TOPK = min(256, S // 4)
    NCMP = (S - 32) // 16 + 1
    NCC = (NCMP + 127) // 128
    NSEL = S // 64
    assert NSEL > 16 and S % 512 == 0 and S // 16 >= 128
    ND = min(14, NT)
    sizes = [512, 64, 64, 256, 64, 4, 512, 768, 24, 2304, 3 * D]
    offs = np.concatenate([[0], np.cumsum(sizes)]).astype(int)
    o_aq, o_ak, o_av, o_iq, o_ik, o_iw, o_bq, o_bkv, o_bg, o_c, o_g = [int(v) for v in offs[:11]]
    INTOT = int(offs[11])
    GC = 3 * DC

    nc = bass.Bass("TRN2", target_bir_lowering=False)
    skind = "ExternalOutput" if debug else "Internal"

    def din(name, shape, dt=F32):
        return nc.dram_tensor(name, list(shape), dt, kind="ExternalInput").ap()

    def dscr(name, shape, dt=BF16):
        return nc.dram_tensor(name, list(shape), dt, kind=skind).ap()

    x_d = din("x", [S, D])
    n1_d = din("norm1_g", [2, D])
    n2_d = din("norm2_g", [2, D])
    win_d = din("w_in", [2, D, INTOT])
    qk_d = din("qk_norm_g", [2, 6, 64])
    cpos_d = din("nsa_cmp_pos", [2, 2, 32, 64])
    cw_d = din("nsa_cmp_w", [2, 2, 32, 64, 64])
    wba_d = din("w_branch_a", [2, 512, D])
    wbb_d = din("w_branch_b", [2, 512, D])
    wbc_d = din("w_branch_c", [2, 256, D])
    wout_d = din("w_out", [2, D, D])
    wfi_d = din("w_ffn_in", [2, D, 2 * DFF])
    wfo_d = din("w_ffn_out", [2, DFF, D])
    rel_d = din("rel_bias", [32, 28])
    ident_d = din("ident", [128, 128])
    flipj_d = din("flipj", [128, 128])
    ohab_d = din("oh_ab", [33, 1920])
    ohw_d = din("oh_w", [33, 768])
    ohc_d = din("oh_c", [3, 33, 384])
    y_d = nc.dram_tensor("y", [S, D], F32, kind="ExternalOutput").ap()

    x1_d = dscr("x1", [S, D], F32)
    t8ab_d = dscr("t8ab", [28, 1920], F32)
    t8w_d = dscr("t8w", [28, 768], F32)
    t8c_d = dscr("t8c", [3, 28, 384], F32)
    bA_d = dscr("bA", [128, ND, 8, 128])
    bBs_d = dscr("bBs", [128, ND, 8, 128])
    bBw_d = dscr("bBw", [128, 5, 8, 128])
    bC_d = dscr("bC", [128, 3, 2, 4, 128])
    QA_d = dscr("QA", [4, 128, S]); KA_d = dscr("KA", [1, 128, S])
    IQ_d = dscr("IQ", [2, 128, S]); IK_d = dscr("IK", [1, 128, S])
    QB_d = dscr("QB", [4, 128, S]); KCVC_d = dscr("KCVC", [2, 128, S])
    KS_d = dscr("KS", [2, 128, S]); KW_d = dscr("KW", [2, 128, S])
    QC_d = dscr("QC", [6, 128, S]); KCc_d = dscr("KCc", [6, 128, S])
    GT_d = dscr("GT", [GC, 128, S])
    VA_d = dscr("VA", [S, 1, 65]); VS_d = dscr("VS", [S, 2, 65]); VW_d = dscr("VW", [S, 2, 65])
    VC_d = dscr("VC", [3, S, 4, 65])
    IW_d = dscr("IW", [S, 4], F32); BG_d = dscr("BG", [S, 24], F32)
    OC_d = dscr("OCs", [3, S, 4, 65], F32)
    AT_d = dscr("ATT", [S, 1280])

    stack = ExitStack()
    kb = KB(nc, stack)
    uq = [0]
    reg_negbig = nc.gpsimd.to_reg(-BIG)
    reg_negbigf = nc.gpsimd.to_reg(-BIGF)
    reg_zero = nc.gpsimd.to_reg(0.0)

    def sbt(name, shape, dt):
        uq[0] += 1
        return nc.sbuf_tensor(f"{name}_{uq[0]}", shape, dt)

    def pst_(name, shape, dt):
        uq[0] += 1
        return nc.psum_tensor(f"{name}_{uq[0]}", shape, dt)

    def sb(name, shape, dt):
        return stack.enter_context(sbt(name, list(shape), dt))

    def ps(name, shape, dt=F32):
        return stack.enter_context(pst_(name, list(shape), dt))

    identf = sb("identf", [128, 128], F32)
    flipf = sb("flipf", [128, 128], F32)
    identb = sb("identb", [128, 128], BF16)
    negI4 = sb("negI4", [128, 4, 128], BF16)
    bdiag = sb("bdiag", [128, 128], BF16)
    epsc = sb("epsc", [128, 1], F32)
    kb.dma("sp", identf[:], ident_d, writes=["identf"])
    kb.dma("sp", flipf[:], flipj_d, writes=["flipf"])
    kb.op("dve", ["identf"], ["identb"], lambda e: e.tensor_copy(out=identb[:], in_=identf[:]))
    for hh in range(4):
        kb.op("dve", ["identf"], ["negI4"], lambda e, hh=hh: e.tensor_scalar(
            out=negI4[:, hh, :], in0=identf[:], scalar1=-BIG, scalar2=None, op0=ALU.mult))
    kb.op("dve", [], ["bdiag"], lambda e: e.memset(bdiag[:], 0.0))
    kb.op("dve", [], ["bdiag"], lambda e: e.memset(bdiag[0:64, 0:64], 1.0 / 64))
    kb.op("dve", [], ["bdiag"], lambda e: e.memset(bdiag[64:128, 64:128], 1.0 / 64))
    kb.op("dve", [], ["epsc"], lambda e: e.memset(epsc[:], EPS))

    def setup_bias():
        with ExitStack() as st:
            rel8 = st.enter_context(sbt("rel8", [33, 28], F32))
            ohs = st.enter_context(sbt("ohs", [33, 1920], F32))
            t8s = st.enter_context(sbt("t8s", [28, 1920], F32))
            hk = st.enter_context(sbt("hk", [128, 2, 4, 128], F32))
            bt = st.enter_context(sbt("bt", [128, 2, 4, 128], BF16))
            pst = st.enter_context(pst_("pst", [128, 2, 512], F32))
            kb.op("dve", [], ["rel8"], lambda e: e.memset(rel8[:], -BIG / 8))
            kb.dma("sp", rel8[0:32, :], rel_d, writes=["rel8"])
            kb.op("act", ["rel8"], ["rel8"], lambda e: e.mul(out=rel8[:], in_=rel8[:], mul=8.0))
            tabs = [(ohab_d, t8ab_d, 1920), (ohw_d, t8w_d, 768)] + [(ohc_d[gi], t8c_d[gi], 384) for gi in range(3)]
            for ti, (oh_d, t8_d, L) in enumerate(tabs):
                kb.dma("sp", ohs[:, 0:L], oh_d, writes=["ohs"])
                nb = (L + 383) // 384
                for b in range(nb):
                    sl = slice(b * 384, min(L, (b + 1) * 384))
                    w = sl.stop - sl.start
                    kb.op("pe", ["rel8", "ohs"], [("pst", b % 2)], lambda e, sl=sl, w=w, b=b: e.matmul(
                        pst[0:28, b % 2, 0:w], lhsT=rel8[:, :], rhs=ohs[:, sl], start=True, stop=True))
                    kb.op("act", [("pst", b % 2)], ["t8s"], lambda e, sl=sl, w=w, b=b: e.copy(
                        out=t8s[:, sl], in_=pst[0:28, b % 2, 0:w]))
                kb.dma("sp", t8_d, t8s[:, 0:L], reads=["t8s"], key=("t8st", ti))
            kb.barrier()
            jobs = []
            for d in range(ND):
                for hb in range(2):
                    jobs.append((t8ab_d, 1920, 128 * d, hb * 4, bA_d[:, d, hb * 4:hb * 4 + 4, :]))
                    jobs.append((t8ab_d, 1920, 128 * d, 8 + hb * 4, bBs_d[:, d, hb * 4:hb * 4 + 4, :]))
            for d in range(5):
                for hb in range(2):
                    jobs.append((t8w_d, 768, 128 * d, 8 + hb * 4, bBw_d[:, d, hb * 4:hb * 4 + 4, :]))
            for gi in range(3):
                for dj in range(2):
                    jobs.append((t8c_d[gi], 384, 128 * dj, 16 + 4 * gi, bC_d[:, gi, dj, :, :]))
            for n, (t8_d, L, c0, h0, dst) in enumerate(jobs):
                s = n % 2
                src = AP(tensor=t8_d.tensor, offset=t8_d.offset + h0 * L + c0, ap=[[1, 128], [L, 4], [1, 128]])
                kb.dma("sp", hk[:, s], src, writes=[("hk", s)])
                kb.op("pe", [("hk", s), "flipf"], [("pst", s)], lambda e, s=s: e.matmul(
                    pst[:, s, :], lhsT=flipf[:], rhs=hk[:, s].rearrange("p h f -> p (h f)"), start=True, stop=True))
                kb.op("act", [("pst", s)], [("bt", s)], lambda e, s=s: e.copy(
                    out=bt[:, s].rearrange("p h f -> p (h f)"), in_=pst[:, s, :]))
                kb.dma("act", dst, bt[:, s], reads=[("bt", s)])
            kb.barrier()

    if getattr(build_nc, "limit", "all") != "none":
        setup_bias()

    def phase1(layer, xin_d):
        with ExitStack() as st:
            def sbl(name, shape, dt):
                return st.enter_context(sbt(name, list(shape), dt))
            hT = sbl("hT", [128, DC, S], BF16)
            G1 = sbl("G1", [128, D], F32)
            g6 = sbl("g6", [128, 6], F32)
            xt = sbl("xt", [128, 2, D], F32)
            junk = sbl("junk", [128, D], BF16)
            hb_ = sbl("hb", [128, 2, D], BF16)
            ss = sbl("ss", [128, 2, 4], F32)
            wst = sbl("wst", [128, 2, DC, 512], F32)
            wbf = sbl("wbf", [128, 2, DC, 512], BF16)
            sq = sbl("sq", [128, 2, 512], BF16)
            sd = sbl("sd", [128, 2, 512], F32)
            ot = sbl("ot", [128, 3, 512], BF16)
            otf = sbl("otf", [128, 2, 32], F32)
            va = sbl("va", [128, 2, 4, 65], BF16)
            tp = st.enter_context(pst_("tp", [128, 2, DC, 128], BF16))
            p1 = st.enter_context(pst_("p1", [128, 3, 512], F32))
            p2 = st.enter_context(pst_("p2", [128, 2, 512], F32))

            kb.dma("sp", G1[:], n1_d[layer:layer + 1, :].to_broadcast([128, D]), writes=["G1"])
            for half in range(2):
                kb.dma("sp", g6[half * 64:(half + 1) * 64, :], qk_d[layer].rearrange("j d -> d j"),
                       writes=["g6"], key=("g6", half), allow_slow_non_contiguous=True)
            for s in range(2):
                kb.op("dve", [], [("va", s)], lambda e, s=s: e.memset(va[:, s], 1.0))
            for tt in range(NT):
                s = tt % 2
                kb.dma("sp", xt[:, s], xin_d[tt * 128:(tt + 1) * 128, :], writes=[("xt", s)])
                kb.op("act", [("xt", s)], ["junk", ("ss", s)], lambda e, s=s: e.activation(
                    out=junk[:], in_=xt[:, s], func=AF.Square, accum_out=ss[:, s, 0:1]))
                kb.op("act", [("ss", s)], [("ss", s)], lambda e, s=s: e.activation(
                    out=ss[:, s, 1:2], in_=ss[:, s, 0:1], func=AF.Sqrt, bias=epsc[:, 0:1], scale=1.0 / D))
                kb.op("dve", [("ss", s)], [("ss", s)], lambda e, s=s: e.reciprocal(out=ss[:, s, 2:3], in_=ss[:, s, 1:2]))
                kb.op("dve", [("xt", s), ("ss", s), "G1"], [("hb", s)], lambda e, s=s: e.scalar_tensor_tensor(
                    out=hb_[:, s], in0=xt[:, s], scalar=ss[:, s, 2:3], in1=G1[:], op0=ALU.mult, op1=ALU.mult))
                for c in range(DC):
                    kb.op("pe", [("hb", s), "identb"], [("tp", s)], lambda e, s=s, c=c: e.transpose(
                        out=tp[:, s, c, :], in_=hb_[:, s, c * 128:(c + 1) * 128], identity=identb[:]), inc=(c == DC - 1))
                kb.op("act", [("tp", s)], ["hT"], lambda e, s=s, tt=tt: e.copy(
                    out=hT[:, :, tt * 128:(tt + 1) * 128], in_=tp[:, s]))

            def perm_cols(gi, pp0, n):
                if gi is None or gi == 0:
                    return lambda c: hT[:, c, pp0:pp0 + n]
                dil = (1, 4, 16)[gi]
                U = S // dil
                r, u0 = pp0 // U, pp0 % U
                assert u0 + n <= U
                return lambda c: hT[:, c, r + dil * u0: r + dil * (u0 + n - 1) + 1: dil]

            wcount = [0]

            def load_w(segs):
                s = wcount[0] % 2
                wcount[0] += 1
                o = 0
                for (c0, n) in segs:
                    kb.dma("sp", wst[:, s, :, o:o + n], win_d[layer, :, c0:c0 + n].rearrange("(c p) n -> p c n", p=128),
                           writes=[("wst", s)], key=("wst", s, o))
                    o += n
                kb.op("pool", [("wst", s)], [("wbf", s)], lambda e, s=s, o=o: e.tensor_copy(
                    out=wbf[:, s, :, 0:o], in_=wst[:, s, :, 0:o]))
                return s, o

            it = [0]
            pend1 = [None]

            def fm_chunk(ws, wo, dst, post, gidx=None, gi=None):
                TB = 512 if gi in (None, 0) else min(512, S // (1, 4, 16)[gi])
                for tb in range(S // TB):
                    s = it[0] % 2
                    s3 = it[0] % 3
                    it[0] += 1
                    cols = perm_cols(gi, tb * TB, TB)
                    for c in range(DC):
                        kb.op("pe", [("wbf", ws), "hT"], [("p1", s3)], lambda e, c=c, s3=s3, cols=cols: e.matmul(
                            p1[:, s3, 0:TB], lhsT=wbf[:, ws, c, wo:wo + 128], rhs=cols(c), start=(c == 0), stop=(c == DC - 1)),
                            inc=(c == DC - 1))
                    store = lambda tb=tb, s3=s3: kb.dma("sp", dst[:, tb * TB:(tb + 1) * TB], ot[:, s3, 0:TB], reads=[("ot", s3)])
                    if post == "plain":
                        kb.op("act", [("p1", s3)], [("ot", s3)], lambda e, s3=s3: e.copy(out=ot[:, s3, 0:TB], in_=p1[:, s3, 0:TB]))
                        store()
                    elif post == "sigmoid":
                        kb.op("act", [("p1", s3)], [("ot", s3)], lambda e, s3=s3: e.activation(
                            out=ot[:, s3, 0:TB], in_=p1[:, s3, 0:TB], func=AF.Sigmoid))
                        store()
                    else:
                        kb.op("act", [("p1", s3)], [("sq", s)], lambda e, s=s, s3=s3: e.activation(
                            out=sq[:, s, 0:TB], in_=p1[:, s3, 0:TB], func=AF.Square))

                        def rest(s=s, s3=s3, store=store):
                            kb.op("pe", [("sq", s), "bdiag"], [("p2", s)], lambda e: e.matmul(
                                p2[:, s, 0:TB], lhsT=bdiag[:], rhs=sq[:, s, 0:TB], start=True, stop=True))
                            kb.op("act", [("p2", s)], [("sd", s)], lambda e: e.activation(
                                out=sd[:, s, 0:TB], in_=p2[:, s, 0:TB], func=AF.Sqrt, bias=epsc[:, 0:1], scale=1.0))
                            kb.op("dve", [("sd", s)], [("sd", s)], lambda e: e.reciprocal(out=sd[:, s, 0:TB], in_=sd[:, s, 0:TB]))
                            kb.op("dve", [("p1", s3), ("sd", s), "g6"], [("ot", s3)], lambda e: e.scalar_tensor_tensor(
                                out=ot[:, s3, 0:TB], in0=p1[:, s3, 0:TB], scalar=g6[:, gidx:gidx + 1], in1=sd[:, s, 0:TB],
                                op0=ALU.mult, op1=ALU.mult))
                            store()
                        prev = pend1[0]
                        pend1[0] = rest
                        if prev is not None:
                            prev()
                if pend1[0] is not None:
                    p_ = pend1[0]
                    pend1[0] = None
                    p_()

            def fm_group(chunks):
                def segs_of(b):
                    segs = []
                    for ch in chunks[b:b + 4]:
                        segs += ch[0]
                    return segs
                nxt = load_w(segs_of(0))
                for b in range(0, len(chunks), 4):
                    grp = chunks[b:b + 4]
                    ws, tot = nxt
                    if b + 4 < len(chunks):
                        nxt = load_w(segs_of(b + 4))
                    for k, ch in enumerate(grp):
                        fm_chunk(ws, 128 * k, ch[1], ch[2], ch[3], ch[4])

            def tm_group(c0, n, post, dst_fn, gi=None):
                ws, _ = load_w([(c0, n)])
                for t in range(NT):
                    s = it[0] % 2
                    it[0] += 1
                    cols = perm_cols(gi, t * 128, 128)
                    for c in range(DC):
                        kb.op("pe", [("wbf", ws), "hT"], [("p1", s)], lambda e, c=c, s=s, cols=cols: e.matmul(
                            p1[:, s, 0:n], lhsT=cols(c), rhs=wbf[:, ws, c, 0:n], start=(c == 0), stop=(c == DC - 1)),
                            inc=(c == DC - 1))
                    if post == "vaug":
                        nh = n // 64
                        kb.op("act", [("p1", s)], [("va", s)], lambda e, s=s, nh=nh: e.copy(
                            out=va[:, s, 0:nh, 0:64], in_=p1[:, s, 0:n].rearrange("p (h d) -> p h d", d=64)))
                        kb.dma("sp", dst_fn(t), va[:, s, 0:nh, :], reads=[("va", s)])
                    else:
                        if post == "sigf":
                            kb.op("act", [("p1", s)], [("otf", s)], lambda e, s=s: e.activation(
                                out=otf[:, s, 0:n], in_=p1[:, s, 0:n], func=AF.Sigmoid))
                        else:
                            kb.op("act", [("p1", s)], [("otf", s)], lambda e, s=s: e.copy(out=otf[:, s, 0:n], in_=p1[:, s, 0:n]))
                        kb.dma("sp", dst_fn(t), otf[:, s, 0:n], reads=[("otf", s)])

            chunks = []
            for c in range(4):
                chunks.append(([(o_aq + 128 * c, 128)], QA_d[c], "rms", 0, None))
            chunks.append(([(o_ak, 64), (o_ak, 64)], KA_d[0], "rms", 1, None))
            for c in range(2):
                chunks.append(([(o_iq + 128 * c, 128)], IQ_d[c], "plain", None, None))
            chunks.append(([(o_ik, 64), (o_ik, 64)], IK_d[0], "plain", None, None))
            for c in range(4):
                chunks.append(([(o_bq + 128 * c, 128)], QB_d[c], "rms", 2, None))
            for c in range(2):
                chunks.append(([(o_bkv + 128 * c, 128)], KCVC_d[c], "plain", None, None))
            for g in range(2):
                chunks.append(([(o_bkv + 256 + 64 * g, 64)] * 2, KS_d[g], "rms", 3, None))
            for g in range(2):
                chunks.append(([(o_bkv + 512 + 64 * g, 64)] * 2, KW_d[g], "rms", 3, None))
            for c in range(6):
                chunks.append(([(o_c + 128 * c, 128)], QC_d[c], "rms", 4, c // 2))
            for c in range(6):
                chunks.append(([(o_c + 768 + 128 * c, 128)], KCc_d[c], "rms", 5, c // 2))
            for c in range(GC):
                chunks.append(([(o_g + 128 * c, 128)], GT_d[c], "sigmoid", None, None))
            fm_group(chunks)
            tm_group(o_av, 64, "vaug", lambda t: VA_d[t * 128:(t + 1) * 128])
            tm_group(o_bkv + 384, 128, "vaug", lambda t: VS_d[t * 128:(t + 1) * 128])
            tm_group(o_bkv + 640, 128, "vaug", lambda t: VW_d[t * 128:(t + 1) * 128])
            for gi in range(3):
                tm_group(o_c + 1536 + 256 * gi, 256, "vaug", lambda t, gi=gi: VC_d[gi, t * 128:(t + 1) * 128], gi=gi)
            tm_group(o_iw, 4, "copyf", lambda t: IW_d[t * 128:(t + 1) * 128, :])
            tm_group(o_bg, 24, "sigf", lambda t: BG_d[t * 128:(t + 1) * 128, :])
            kb.barrier()

    def attn_tile(S_ps, s_slot, PT, pt_slot, O_ap_fn, bias_rhs, neg_lhsT, kq_list, v_rhs_fn, first, nk=128):
        sk = ("S_ps", s_slot)
        started = False
        if bias_rhs is not None:
            ap_, keys = bias_rhs
            kb.op("pe", ["identb"] + keys, [sk], lambda e: e.matmul(
                S_ps[0:nk, s_slot, :], lhsT=identb[0:nk, 0:nk], rhs=ap_, start=True, stop=False, skip_group_check=True), inc=False)
            started = True
        if neg_lhsT is not None:
            ap_, keys = neg_lhsT
            kb.op("pe", ["negI4"] + keys, [sk], lambda e: e.matmul(
                S_ps[0:nk, s_slot, :], lhsT=ap_, rhs=negI4[:].rearrange("p h f -> p (h f)"), start=not started, stop=False,
                skip_group_check=True), inc=False)
            started = True
        nq = len(kq_list)
        wq = 512 // nq
        for hh, (kT, qT, keys) in enumerate(kq_list):
            kb.op("pe", keys, [sk], lambda e, hh=hh, kT=kT, qT=qT, st_=(not started): e.matmul(
                S_ps[0:nk, s_slot, hh * wq:(hh + 1) * wq], lhsT=kT, rhs=qT, start=st_, stop=(hh == nq - 1),
                skip_group_check=True), inc=(hh == nq - 1))
            started = True
        perm = (0, 2, 1, 3) if nq == 2 else (0, 1, 2, 3)
        pk = ("PT", pt_slot)
        kb.op("act", [sk], [pk], lambda e: e.activation(
            out=PT[0:nk, pt_slot, :], in_=S_ps[0:nk, s_slot, :], func=AF.Exp, scale=0.125))

        def pv():
            for b_ in range(4):
                vap, oap, vkeys, okey, bfirst = v_rhs_fn(perm[b_])
                kb.op("pe", [pk] + vkeys, [okey], lambda e, b_=b_, vap=vap, oap=oap, bfirst=bfirst: e.matmul(
                    oap, lhsT=PT[0:nk, pt_slot, b_ * 128:(b_ + 1) * 128], rhs=vap, start=(first and bfirst), stop=False,
                    skip_group_check=True), inc=(b_ == 3))
        prev = pend[0]
        pend[0] = pv
        if prev is not None:
            prev()

    pend = [None]

    def attn_flush():
        if pend[0] is not None:
            p = pend[0]
            pend[0] = None
            p()

    def phase2A(layer):
        with ExitStack() as st:
            def sbl(name, shape, dt):
                return st.enter_context(sbt(name, list(shape), dt))
            QA = sbl("QA_s", [128, 4, S], BF16); KA = sbl("KA_s", [128, 2, S], BF16)
            IQ = sbl("IQ_s", [128, 2, S], BF16); IK = sbl("IK_s", [128, 2, S], BF16)
            VA = sbl("VA_s", [128, NT, 65], BF16); IW = sbl("IW_s", [128, NT, 4], F32)
            bA = sbl("bA_s", [128, ND, 8, 128], BF16)
            score = sbl("score", [128, 1, S], F32)
            negm = sbl("negm", [128, 2, S], BF16)
            junkb = sbl("junkb", [128, S], BF16)
            rt = sbl("rt", [128, 4, 512], F32)
            PT = sbl("PT", [128, 3, 512], BF16)
            cm = sbl("cm", [128, 128], F32)
            wv = sbl("wv", [128, 2, 16], F32)
            bis = sbl("bis", [128, 2, 8], F32)
            wk = sbl("wk", [128, 2, NBIS], F32)
            halves = sbl("halves", [128, NBIS], F32)
            rden = sbl("rden", [128, 2, 8], F32)
            oa = sbl("oa", [128, 2, 512], BF16)
            P_ps = st.enter_context(pst_("P_ps", [128, 2, 512], F32))
            S_ps = st.enter_context(pst_("S_ps", [128, 2, 512], F32))
            O_ps = st.enter_context(pst_("O_ps", [128, 2, 512], F32))

            kb.dma("sp", QA[:], QA_d.rearrange("c p s -> p c s"), writes=["QA"])
            kb.op("pool", [], ["KA"], lambda e: e.memset(KA[:], 0.0))
            kb.op("pool", [], ["IK"], lambda e: e.memset(IK[:], 0.0))
            for v in range(2):
                kb.dma("sp", KA[64 * v:64 * v + 64, v, :], KA_d[0, 64 * v:64 * v + 64, :], writes=["KA"], key=("KAl", v))
                kb.dma("sp", IK[64 * v:64 * v + 64, v, :], IK_d[0, 64 * v:64 * v + 64, :], writes=["IK"], key=("IKl", v))
            kb.dma("sp", IQ[:], IQ_d.rearrange("c p s -> p c s"), writes=["IQ"])
            kb.dma("sp", VA[:], VA_d.rearrange("(t p) o c -> p t (o c)", p=128), writes=["VA"])
            kb.dma("sp", IW[:], IW_d.rearrange("(t p) c -> p t c", p=128), writes=["IW"])
            kb.dma("sp", bA[:], bA_d, writes=["bA"])
            kb.op("pool", [], ["cm"], lambda e: e.memset(cm[:], 0.0))
            kb.op("pool", ["cm"], ["cm"], lambda e: e.affine_select(
                out=cm[:], in_=cm[:], pattern=[[-1, 128]], compare_op=ALU.is_ge, fill=reg_negbigf, base=0, channel_multiplier=1))
            for k in range(NBIS):
                kb.op("dve", [], ["halves"], lambda e, k=k: e.memset(halves[:, k:k + 1], 2.0 ** -(k + 1)))

            def idx_bis(i):
                L = 128 * (i + 1)
                s = i % 2
                sc = ("score", 0)
                qs = slice(i * 128, (i + 1) * 128)
                kb.op("act", ["IW"], [("wv", s)], lambda e, s=s, i=i: e.activation(
                    out=wv[:, s, 0:4], in_=IW[:, i, :], func=AF.Abs))
                kb.op("act", ["IW"], [("wv", s)], lambda e, s=s, i=i: e.activation(
                    out=wv[:, s, 4:8], in_=IW[:, i, :], func=AF.Sign))
                nkb = (L + 511) // 512
                for kbk in range(nkb):
                    k0 = kbk * 512
                    w = min(512, L - k0)
                    for h in range(4):
                        ps_ = (kbk * 4 + h) % 2
                        r4 = (kbk * 4 + h) % 4
                        kb.op("pe", ["IQ", "IK"], [("P_ps", ps_)], lambda e, h=h, ps_=ps_, k0=k0, w=w: e.matmul(
                            P_ps[:, ps_, 0:w], lhsT=IQ[:, h // 2, qs], rhs=IK[:, h % 2, k0:k0 + w], start=True, stop=True))
                        kb.op("act", [("P_ps", ps_), ("wv", s)], [("rt", r4)], lambda e, h=h, ps_=ps_, r4=r4, w=w, s=s: e.activation(
                            out=rt[:, r4, 0:w], in_=P_ps[:, ps_, 0:w], func=AF.Relu, scale=wv[:, s, h:h + 1]))
                        if h == 0:
                            kb.op("dve", [("rt", r4), ("wv", s)], [sc], lambda e, r4=r4, w=w, s=s, k0=k0: e.tensor_scalar(
                                out=score[:, 0, k0:k0 + w], in0=rt[:, r4, 0:w], scalar1=wv[:, s, 4:5], scalar2=None, op0=ALU.mult))
                        else:
                            kb.op("dve", [("rt", r4), ("wv", s), sc], [sc], lambda e, r4=r4, w=w, s=s, k0=k0, h=h: e.scalar_tensor_tensor(
                                out=score[:, 0, k0:k0 + w], in0=rt[:, r4, 0:w], scalar=wv[:, s, 4 + h:5 + h],
                                in1=score[:, 0, k0:k0 + w], op0=ALU.mult, op1=ALU.add))
                bk_ = ("bis", s)
                kb.op("dve", [sc], [bk_], lambda e, s=s, L=L: e.tensor_reduce(
                    out=bis[:, s, 0:1], in_=score[:, 0, 0:L], axis=AX.X, op=ALU.max, apply_absolute_value=True))
                kb.op("dve", [sc, "cm"], [sc], lambda e, s=s, i=i: e.tensor_tensor(
                    out=score[:, 0, qs], in0=score[:, 0, qs], in1=cm[:], op=ALU.add))
                kb.op("dve", [bk_], [bk_], lambda e, s=s: e.tensor_scalar(
                    out=bis[:, s, 1:2], in0=bis[:, s, 0:1], scalar1=2.002, scalar2=2e-6, op0=ALU.mult, op1=ALU.add))
                kb.op("dve", [bk_, "halves"], [("wk", s)], lambda e, s=s: e.tensor_scalar(
                    out=wk[:, s, :], in0=halves[:], scalar1=bis[:, s, 1:2], scalar2=None, op0=ALU.mult))
                kb.op("dve", [], [bk_], lambda e, s=s: e.memset(bis[:, s, 2:3], 0.0))
                for k in range(NBIS):
                    kb.op("dve", [sc, bk_], ["junkb", bk_], lambda e, s=s, L=L: e.tensor_scalar(
                        out=junkb[:, 0:L], in0=score[:, 0, 0:L], scalar1=bis[:, s, 2:3], scalar2=None,
                        op0=ALU.is_gt, op1=ALU.add, accum_out=bis[:, s, 3:4]))
                    kb.op("dve", [bk_], [bk_], lambda e, s=s: e.tensor_scalar(
                        out=bis[:, s, 4:5], in0=bis[:, s, 3:4], scalar1=float(TOPK), scalar2=0.5, op0=ALU.is_ge, op1=ALU.subtract))
                    kb.op("dve", [bk_, ("wk", s)], [bk_], lambda e, s=s, k=k: e.scalar_tensor_tensor(
                        out=bis[:, s, 2:3], in0=bis[:, s, 4:5], scalar=wk[:, s, k:k + 1], in1=bis[:, s, 2:3],
                        op0=ALU.mult, op1=ALU.add))
                nk_ = ("negm", s)
                kb.op("dve", [sc, bk_], [nk_], lambda e, s=s, L=L: e.tensor_scalar(
                    out=negm[:, s, 0:L], in0=score[:, 0, 0:L], scalar1=bis[:, s, 2:3], scalar2=None, op0=ALU.is_le))
            def att(i):
                L = 128 * (i + 1)
                s = i % 2
                qs = slice(i * 128, (i + 1) * 128)
                nk_ = ("negm", s)
                for j in range(i + 1):
                    d = min(i - j, ND - 1)
                    ks_ = slice(j * 128, (j + 1) * 128)
                    for hb in range(2):
                        ss_ = (i * 64 + j * 2 + hb) % 2
                        p3 = (i * 64 + j * 2 + hb) % 3
                        kq = []
                        for v in range(2):
                            kq.append((KA[:, v, ks_], QA[:, 2 * hb:2 * hb + 2, qs], ["KA", "QA"]))
                        attn_tile(S_ps, ss_, PT, p3,
                                  None,
                                  (bA[:, d, hb * 4:hb * 4 + 4, :].rearrange("p (c v) f -> p v c f", v=2), ["bA"]),
                                  (negm[:, s, ks_], [nk_]),
                                  kq,
                                  lambda hh, hb=hb, j=j: (VA[:, j, :], O_ps[:, hb, hh * 65:(hh + 1) * 65], ["VA"], ("O_ps", hb), hh == 0),
                                  first=(j == 0))
                attn_flush()
                okeys = [("O_ps", 0), ("O_ps", 1)]
                Ov = O_ps[:, :, 0:260].rearrange("p b (h c) -> p b h c", c=65)
                kb.op("dve", okeys, [("rden", s)], lambda e, s=s, Ov=Ov: e.reciprocal(
                    out=rden[:, s, :].rearrange("p (b h) -> p b h", b=2), in_=Ov[:, :, :, 64]))
                for hb in range(2):
                    kb.op("dve", [("O_ps", hb), ("rden", s)], [("oa", s)], lambda e, s=s, hb=hb, Ov=Ov: e.tensor_tensor(
                        out=oa[:, s, hb * 256:(hb + 1) * 256].rearrange("p (h c) -> p h c", c=64),
                        in0=Ov[:, hb, :, 0:64], in1=rden[:, s, hb * 4:hb * 4 + 4].unsqueeze(2).to_broadcast([128, 4, 64]),
                        op=ALU.mult))
                kb.dma("sp", AT_d[i * 128:(i + 1) * 128, 0:512], oa[:, s, :], reads=[("oa", s)])

            idx_bis(0)
            for i in range(NT):
                if i + 1 < NT:
                    idx_bis(i + 1)
                att(i)
            kb.barrier()


    def phase2B(layer):
        with ExitStack() as st:
            def sbl(name, shape, dt):
                return st.enter_context(sbt(name, list(shape), dt))
            NCP = NCC * 128
            QB = sbl("QB_s", [128, 2, S], BF16)
            KS = sbl("KS_s", [128, 2, S], BF16); KW = sbl("KW_s", [128, 2, S], BF16)
            VS = sbl("VS_s", [128, NT, 65], BF16); VW = sbl("VW_s", [128, NT, 65], BF16)
            BGs = sbl("BG_s", [128, NT, 8, 3], F32)
            bBs = sbl("bBs_s", [128, ND, 4, 128], BF16); bBw = sbl("bBw_s", [128, 5, 4, 128], BF16)
            kcT = sbl("kcT", [128, 2, 2, NCP], BF16)
            CVX = sbl("CVX", [128, 2, NCC, 129], BF16)
            Vw = sbl("Vw", [128, 2 * NSEL], F32); Fw = sbl("Fw", [128, 2 * NSEL], F32)
            zt = sbl("zt", [128, 4, 128], BF16)
            mct = sbl("mct", [128, 2, 4, 128], BF16)
            PT = sbl("PTb", [128, 3, 512], BF16)
            negmB = sbl("negmB", [128, 2, S], BF16)
            sm = sbl("smB", [128, 2, 32], F32)
            imp = sbl("imp", [128, 2, 64 * 4], F32)
            m8 = sbl("m8", [128, 2, 16], F32)
            t1 = sbl("t1", [128, 2, 256], F32); t2 = sbl("t2", [128, 2, 256], F32)
            ob = sbl("obB", [128, 2, 256], BF16)

            kb.dma("sp", BGs[:], BG_d.rearrange("(t p) (h b) -> p t h b", p=128, b=3), writes=["BGs"])
            kb.op("pool", [], ["KS"], lambda e: e.memset(KS[:], 0.0))
            kb.op("pool", [], ["KW"], lambda e: e.memset(KW[:], 0.0))
            kb.op("pool", [], ["zt"], lambda e: e.memset(zt[:], 0.0))
            kb.op("dve", [], ["Vw"], lambda e: e.memset(Vw[:], 0.0))
            kb.op("dve", [], ["Fw"], lambda e: e.memset(Fw[:], 0.0))
            for b in range(2):
                rows = slice(64 * b, 64 * b + 64)
                kb.op("dve", [], ["Vw"], lambda e, rows=rows, b=b: e.memset(Vw[rows, 0:NSEL + b - 1], 1.0))
                kb.op("dve", [], ["Fw"], lambda e, rows=rows, b=b: e.memset(Fw[rows, NSEL + b - 1:NSEL + b + 1], BIGF))
                kb.op("dve", [], ["Fw"], lambda e, rows=rows, b=b: e.memset(Fw[rows, NSEL + b + 1:2 * NSEL], -BIGF))

            with ExitStack() as st2:
                def sb2(name, shape, dt):
                    return st2.enter_context(sbt(name, list(shape), dt))
                KCVC = sb2("KCVC_s", [128, 2, S], BF16)
                wc32 = sb2("wc32", [128, 32, 64], F32)
                wc = sb2("wc", [128, 2, 2, 32, 64], BF16)
                pos32 = sb2("pos32", [128, 2, 32], F32)
                posB = sb2("posB", [128, 2, 32, 128], BF16)
                Gk = sb2("Gk", [128, 64], F32)
                ktm = sb2("ktm", [128, 2, 128], BF16)
                ov = sb2("ov", [128, NCC, 64], F32)
                cs = sb2("cs", [128, 2, 4], F32)
                cj = sb2("cj", [128, 64], F32)
                pc = st2.enter_context(pst_("pc", [128, 2, 512], F32))
                ptr = st2.enter_context(pst_("ptr", [128, 2, 1024], BF16))
                kb.dma("sp", KCVC[:], KCVC_d.rearrange("c p s -> p c s"), writes=["KCVC"])
                kb.dma("sp", Gk[:], qk_d[layer, 3:4, :].to_broadcast([128, 64]), writes=["Gk"])
                for half in range(2):
                    kb.dma("sp", pos32[64 * half:64 * half + 64], cpos_d[layer].rearrange("k l d -> d k l"),
                           writes=["pos32"], key=("pos32", half), allow_slow_non_contiguous=True)
                kb.op("pool", [], ["wc"], lambda e: e.memset(wc[:], 0.0))
                for kv in range(2):
                    for half in range(2):
                        kb.dma("sp", wc32[64 * half:64 * half + 64], cw_d[layer, kv].rearrange("l d e -> d l e"),
                               writes=["wc32"], key=("wc32", half))
                    for half in range(2):
                        kb.op("dve", ["wc32"], ["wc"], lambda e, kv=kv, half=half: e.tensor_copy(
                            out=wc[64 * half:64 * half + 64, half, kv], in_=wc32[64 * half:64 * half + 64]))
                kb.op("dve", ["pos32"], ["posB"], lambda e: e.tensor_copy(
                    out=posB[:].rearrange("p k l n -> p (k l) n"),
                    in_=pos32[:].rearrange("p k l -> p (k l)").unsqueeze(2).to_broadcast([128, 64, 128])))
                kb.op("pool", [], ["ov"], lambda e: e.memset(ov[:], 1.0))
                for c in range(NCC):
                    kb.op("pool", ["ov"], ["ov"], lambda e, c=c: e.affine_select(
                        out=ov[:, c, :], in_=ov[:, c, :], pattern=[[64, 64]], compare_op=ALU.is_gt, fill=reg_zero,
                        base=64 - 16 * 128 * c, channel_multiplier=-16))
                    kb.op("pool", ["ov"], ["ov"], lambda e, c=c: e.affine_select(
                        out=ov[:, c, :], in_=ov[:, c, :], pattern=[[-64, 64]], compare_op=ALU.is_gt, fill=reg_zero,
                        base=16 * 128 * c + 32, channel_multiplier=16))
                kb.op("dve", [], ["CVX"], lambda e: e.memset(CVX[:], 1.0))
                for g in range(2):
                    kb.op("dve", ["ov", "CVX"], ["CVX"], lambda e, g=g: e.tensor_copy(out=CVX[:, g, :, 65:129], in_=ov[:]))
                kb.op("dve", [], ["kcT"], lambda e: e.memset(kcT[:], 0.0))
                n_it = 0
                for kv in range(2):
                    for g in range(2):
                        rows = slice(64 * g, 64 * g + 64)
                        for c in range(NCC):
                            nv = min(128, NCMP - 128 * c)
                            s = n_it % 2
                            n_it += 1
                            for l in range(32):
                                t0 = 16 * 128 * c + l
                                kb.op("pe", ["KCVC", "wc"], [("pc", s)], lambda e, l=l, t0=t0, nv=nv, s=s, g=g, kv=kv: e.matmul(
                                    pc[0:nv, s, 0:64], lhsT=KCVC[:, kv, t0:t0 + 16 * (nv - 1) + 1:16], rhs=wc[:, g, kv, l, :],
                                    start=(l == 0), stop=False), inc=False)
                            for l in range(32):
                                kb.op("pe", ["posB", "wc"], [("pc", s)], lambda e, l=l, nv=nv, s=s, g=g, kv=kv: e.matmul(
                                    pc[0:nv, s, 0:64], lhsT=posB[:, kv, l, 0:nv], rhs=wc[:, g, kv, l, :],
                                    start=False, stop=(l == 31)), inc=(l == 31))
                            if kv == 1:
                                kb.op("act", [("pc", s)], ["CVX"], lambda e, nv=nv, s=s, g=g, c=c: e.copy(
                                    out=CVX[0:nv, g, c, 0:64], in_=pc[0:nv, s, 0:64]))
                            else:
                                kb.op("act", [("pc", s)], ["cj", ("cs", s)], lambda e, nv=nv, s=s: e.activation(
                                    out=cj[0:nv, :], in_=pc[0:nv, s, 0:64], func=AF.Square, accum_out=cs[0:nv, s, 0:1]))
                                kb.op("act", [("cs", s)], [("cs", s)], lambda e, nv=nv, s=s: e.activation(
                                    out=cs[0:nv, s, 1:2], in_=cs[0:nv, s, 0:1], func=AF.Sqrt, bias=epsc[0:nv, 0:1], scale=1.0 / 64))
                                kb.op("dve", [("cs", s)], [("cs", s)], lambda e, nv=nv, s=s: e.reciprocal(
                                    out=cs[0:nv, s, 2:3], in_=cs[0:nv, s, 1:2]))
                                kb.op("dve", [], [("ktm", s)], lambda e, s=s: e.memset(ktm[:, s, :], 0.0))
                                for dup in range(2):
                                    kb.op("dve", [("pc", s), ("cs", s), "Gk"], [("ktm", s)], lambda e, nv=nv, s=s, dup=dup: e.scalar_tensor_tensor(
                                        out=ktm[0:nv, s, 64 * dup:64 * dup + 64], in0=pc[0:nv, s, 0:64], scalar=cs[0:nv, s, 2:3],
                                        in1=Gk[0:nv, :], op0=ALU.mult, op1=ALU.mult))
                                kb.op("pe", [("ktm", s), "identb"], [("ptr", s)], lambda e, s=s: e.transpose(
                                    out=ptr[:, s, 0:128], in_=ktm[:, s, :], identity=identb[:]))
                                for v in range(2):
                                    kb.op("act", [("ptr", s)], ["kcT"], lambda e, s=s, g=g, c=c, v=v: e.copy(
                                        out=kcT[64 * v:64 * v + 64, g, v, 128 * c:128 * c + 128], in_=ptr[64 * v:64 * v + 64, s, 0:128]))
                kb.barrier()

            S_ps = st.enter_context(pst_("S_psB", [128, 2, 512], F32))
            OCU = st.enter_context(pst_("OCU", [128, 2, 512], F32))
            OS = st.enter_context(pst_("OS", [128, 512], F32))
            OW = st.enter_context(pst_("OW", [128, 512], F32))
            cnt = [0]

            def slots():
                cnt[0] += 1
                return cnt[0] % 2, cnt[0] % 3

            for g in range(2):
                kb.dma("sp", QB[:], QB_d[2 * g:2 * g + 2].rearrange("c p s -> p c s"), writes=["QB"])
                for v in range(2):
                    kb.dma("sp", KS[64 * v:64 * v + 64, v, :], KS_d[g, 64 * v:64 * v + 64, :], writes=["KS"], key=("KSl", v))
                    kb.dma("sp", KW[64 * v:64 * v + 64, v, :], KW_d[g, 64 * v:64 * v + 64, :], writes=["KW"], key=("KWl", v))
                kb.dma("sp", VS[:], VS_d[:, g, :].rearrange("(t p) c -> p t c", p=128), writes=["VS"])
                kb.dma("sp", VW[:], VW_d[:, g, :].rearrange("(t p) c -> p t c", p=128), writes=["VW"])
                kb.dma("sp", bBs[:], bBs_d[:, :, 4 * g:4 * g + 4, :], writes=["bBs"])
                kb.dma("sp", bBw[:], bBw_d[:, :, 4 * g:4 * g + 4, :], writes=["bBw"])
                for i in range(NT):
                    L = 128 * (i + 1)
                    qs = slice(i * 128, (i + 1) * 128)
                    so = i % 2
                    sg = i % 2
                    def kq_for(Ksrc, cols, nk=128, g=g):
                        out = []
                        for v in range(2):
                            out.append((Ksrc(v, cols), QB[:, 0:2, qs], ["QB", "KS", "KW", "kcT"]))
                        return out
                    cmax = min(NCC - 1, (128 * i + 96) // 2048)
                    for c in range(cmax + 1):
                        nv = min(128, NCMP - 128 * c)
                        o = 128 * i - 2048 * c - 31
                        s2, s3 = slots()
                        bias = None
                        if o < 16 * 127:
                            kb.op("pool", ["zt"], [("mct", s2)], lambda e, s2=s2, o=o: e.affine_select(
                                out=mct[:, s2], in_=zt[:], pattern=[[0, 4], [1, 128]], compare_op=ALU.is_ge, fill=reg_negbig,
                                base=o, channel_multiplier=-16))
                            bias = (mct[0:nv, s2].rearrange("p h f -> p (h f)"), [("mct", s2)])
                        attn_tile(S_ps, s2, PT, s3, None, bias, None,
                                  kq_for(lambda v, cols, g=g: kcT[:, g, v, cols], slice(128 * c, 128 * c + nv)),
                                  lambda hh, g=g, c=c, nv=nv: (CVX[0:nv, g, c, :], OCU[:, hh // 2, (hh % 2) * 129:(hh % 2) * 129 + 129],
                                                               ["CVX"], ("OCU", hh // 2), hh % 2 == 0),
                                  first=(c == 0), nk=nv)
                    attn_flush()
                    ock = [("OCU", 0), ("OCU", 1)]
                    OCv = OCU[:, :, 0:258].rearrange("p b (h c) -> p b h c", c=129)
                    smk = ("sm", sg)
                    kb.op("dve", ock, [smk], lambda e, sg=sg, OCv=OCv: e.tensor_scalar(
                        out=sm[:, sg, 0:4].rearrange("p (b h) -> p b h", b=2), in0=OCv[:, :, :, 64], scalar1=1e-30, scalar2=None, op0=ALU.max))
                    kb.op("dve", [smk], [smk], lambda e, sg=sg: e.reciprocal(out=sm[:, sg, 0:4], in_=sm[:, sg, 0:4]))
                    ik_ = ("imp", sg)
                    for hh in range(4):
                        Uh = OCU[:, hh // 2, (hh % 2) * 129 + 65:(hh % 2) * 129 + 65 + NSEL]
                        if hh == 0:
                            kb.op("dve", ock + [smk], [ik_], lambda e, sg=sg, Uh=Uh: e.tensor_scalar(
                                out=imp[:, sg, 0:NSEL], in0=Uh, scalar1=sm[:, sg, 0:1], scalar2=None, op0=ALU.mult))
                        else:
                            kb.op("dve", ock + [smk, ik_], [ik_], lambda e, sg=sg, Uh=Uh, hh=hh: e.scalar_tensor_tensor(
                                out=imp[:, sg, 0:NSEL], in0=Uh, scalar=sm[:, sg, hh:hh + 1], in1=imp[:, sg, 0:NSEL],
                                op0=ALU.mult, op1=ALU.add))
                    x0 = NSEL - 2 * i
                    kb.op("dve", [ik_, "Vw"], [ik_], lambda e, sg=sg, x0=x0: e.tensor_tensor(
                        out=imp[:, sg, 64:64 + NSEL], in0=imp[:, sg, 0:NSEL], in1=Vw[:, x0:x0 + NSEL], op=ALU.mult))
                    kb.op("dve", [ik_, "Fw"], [ik_], lambda e, sg=sg, x0=x0: e.tensor_tensor(
                        out=imp[:, sg, 64:64 + NSEL], in0=imp[:, sg, 64:64 + NSEL], in1=Fw[:, x0:x0 + NSEL], op=ALU.add))
                    kb.op("dve", [ik_], [ik_], lambda e, sg=sg: e.memset(imp[:, sg, 64:65], BIGF))
                    mk = ("m8", sg)
                    kb.op("dve", [ik_], [mk], lambda e, sg=sg: e.max(out=m8[:, sg, 0:8], in_=imp[:, sg, 64:64 + NSEL]))
                    kb.op("dve", [ik_, mk], [ik_], lambda e, sg=sg: e.match_replace(
                        out=imp[:, sg, 128:128 + NSEL], in_to_replace=m8[:, sg, 0:8], in_values=imp[:, sg, 64:64 + NSEL], imm_value=-3.0e38))
                    kb.op("dve", [ik_], [mk], lambda e, sg=sg: e.max(out=m8[:, sg, 8:16], in_=imp[:, sg, 128:128 + NSEL]))
                    kb.op("dve", [ik_, mk], [ik_], lambda e, sg=sg: e.tensor_scalar(
                        out=imp[:, sg, 192:192 + NSEL], in0=imp[:, sg, 64:64 + NSEL], scalar1=m8[:, sg, 15:16], scalar2=None, op0=ALU.is_lt))
                    nbk = ("negmB", sg)
                    nb_ = 2 * (i + 1)
                    kb.op("dve", [ik_], [nbk], lambda e, sg=sg, nb_=nb_, L=L: e.tensor_copy(
                        out=negmB[:, sg, 0:L].rearrange("p (m k) -> p m k", k=64),
                        in_=imp[:, sg, 192:192 + nb_].unsqueeze(2).to_broadcast([128, nb_, 64])))
                    for j in range(i + 1):
                        d = min(i - j, ND - 1)
                        ks_ = slice(j * 128, (j + 1) * 128)
                        s2, s3 = slots()
                        attn_tile(S_ps, s2, PT, s3, None,
                                  (bBs[:, d].rearrange("p (c v) f -> p v c f", v=2), ["bBs"]),
                                  (negmB[:, sg, ks_], [nbk]),
                                  kq_for(lambda v, cols: KS[:, v, cols], ks_),
                                  lambda hh, j=j: (VS[:, j, :], OS[:, hh * 65:(hh + 1) * 65], ["VS"], "OS", hh == 0),
                                  first=(j == 0))
                    j0 = max(0, i - 4)
                    for j in range(j0, i + 1):
                        ks_ = slice(j * 128, (j + 1) * 128)
                        s2, s3 = slots()
                        attn_tile(S_ps, s2, PT, s3, None,
                                  (bBw[:, i - j].rearrange("p (c v) f -> p v c f", v=2), ["bBw"]),
                                  None,
                                  kq_for(lambda v, cols: KW[:, v, cols], ks_),
                                  lambda hh, j=j: (VW[:, j, :], OW[:, hh * 65:(hh + 1) * 65], ["VW"], "OW", hh == 0),
                                  first=(j == j0))
                    attn_flush()
                    OSv = OS[:, 0:260].rearrange("p (h c) -> p h c", c=65)
                    OWv = OW[:, 0:260].rearrange("p (h c) -> p h c", c=65)
                    kb.op("dve", ["OS"], [smk], lambda e, sg=sg, OSv=OSv: e.reciprocal(out=sm[:, sg, 4:8], in_=OSv[:, :, 64]))
                    kb.op("dve", ["OW"], [smk], lambda e, sg=sg, OWv=OWv: e.reciprocal(out=sm[:, sg, 8:12], in_=OWv[:, :, 64]))
                    for br in range(3):
                        kb.op("dve", [smk, "BGs"], [smk], lambda e, sg=sg, br=br, g=g, i=i: e.tensor_tensor(
                            out=sm[:, sg, 12 + 4 * br:16 + 4 * br], in0=sm[:, sg, 4 * br:4 * br + 4], in1=BGs[:, i, 4 * g:4 * g + 4, br], op=ALU.mult))
                    def cf(br, sg=sg):
                        return sm[:, sg, 12 + 4 * br:16 + 4 * br].unsqueeze(2).to_broadcast([128, 4, 64])
                    t1v = t1[:, sg, :].rearrange("p (h c) -> p h c", c=64)
                    t2v = t2[:, sg, :].rearrange("p (h c) -> p h c", c=64)
                    kb.op("dve", ock + [smk], [("t1", sg)], lambda e, t1v=t1v, OCv=OCv, cf=cf: e.tensor_tensor(
                        out=t1v.rearrange("p (b h) c -> p b h c", b=2), in0=OCv[:, :, :, 0:64],
                        in1=cf(0).rearrange("p (b h) c -> p b h c", b=2), op=ALU.mult))
                    kb.op("dve", ["OS", smk], [("t2", sg)], lambda e, t2v=t2v, OSv=OSv, cf=cf: e.tensor_tensor(
                        out=t2v, in0=OSv[:, :, 0:64], in1=cf(1), op=ALU.mult))
                    kb.op("dve", [("t1", sg), ("t2", sg)], [("t1", sg)], lambda e, sg=sg: e.tensor_tensor(
                        out=t1[:, sg, :], in0=t1[:, sg, :], in1=t2[:, sg, :], op=ALU.add))
                    kb.op("dve", ["OW", smk], [("t2", sg)], lambda e, t2v=t2v, OWv=OWv, cf=cf: e.tensor_tensor(
                        out=t2v, in0=OWv[:, :, 0:64], in1=cf(2), op=ALU.mult))
                    kb.op("dve", [("t1", sg), ("t2", sg)], [("ob", so)], lambda e, sg=sg, so=so, g=g: e.tensor_tensor(
                        out=ob[:, so, :], in0=t1[:, sg, :], in1=t2[:, sg, :], op=ALU.add))
                    kb.dma("sp", AT_d[i * 128:(i + 1) * 128, 512 + 256 * g:768 + 256 * g], ob[:, so, :], reads=[("ob", so)])
            kb.barrier()

    def phase2C(layer):
        with ExitStack() as st:
            def sbl(name, shape, dt):
                return st.enter_context(sbt(name, list(shape), dt))
            QC = sbl("QC_s", [128, 2, S], BF16); KC = sbl("KC_s", [128, 2, 2, S], BF16)
            VC = sbl("VC_s", [128, NT, 4, 65], BF16)
            bC = sbl("bC_s", [128, 3, 2, 4, 128], BF16)
            PT = sbl("PTc", [128, 3, 512], BF16)
            osb = sbl("osb", [128, 2, 260], F32)
            S_ps = st.enter_context(pst_("S_psC", [128, 2, 512], F32))
            O_ps = st.enter_context(pst_("O_psC", [128, 2, 512], F32))
            kb.dma("sp", bC[:], bC_d, writes=["bC"])
            kb.op("pool", [], ["KC"], lambda e: e.memset(KC[:], 0.0))
            n = 0
            for gi, dil in enumerate((1, 4, 16)):
                U = S // dil
                UT = U // 128
                kb.dma("sp", QC[:], QC_d[2 * gi:2 * gi + 2].rearrange("c p s -> p c s"), writes=["QC"])
                for v in range(2):
                    kb.dma("sp", KC[64 * v:64 * v + 64, :, v, :], KCc_d[2 * gi:2 * gi + 2, 64 * v:64 * v + 64, :].rearrange("c p s -> p c s"),
                           writes=["KC"], key=("KCl", v))
                kb.dma("sp", VC[:], VC_d[gi].rearrange("(t p) h c -> p t h c", p=128), writes=["VC"])
                ocv = OC_d[gi].rearrange("(u r) h c -> r u (h c)", r=dil)
                for pt in range(NT):
                    r, ui = pt // UT, pt % UT
                    so = pt % 2
                    qs = slice(pt * 128, (pt + 1) * 128)
                    first = True
                    for dj in (1, 0):
                        if ui - dj < 0:
                            continue
                        kt = pt - dj
                        ks_ = slice(kt * 128, (kt + 1) * 128)
                        n += 1
                        kq = []
                        for hh in range(4):
                            kq.append((KC[:, hh // 2, hh % 2, ks_], QC[:, hh // 2, qs], ["KC", "QC"]))
                        attn_tile(S_ps, n % 2, PT, n % 3, None,
                                  (bC[:, gi, dj].rearrange("p h f -> p (h f)"), ["bC"]), None, kq,
                                  lambda hh, kt=kt, so=so: (VC[:, kt, hh, :], O_ps[:, so, hh * 65:(hh + 1) * 65], ["VC"], ("O_psC", so), hh == 0),
                                  first=first)
                        first = False
                    attn_flush()
                    kb.op("act", [("O_psC", so)], [("osb", so)], lambda e, so=so: e.copy(out=osb[:, so, :], in_=O_ps[:, so, 0:260]))
                    kb.dma("sp", ocv[r, ui * 128:(ui + 1) * 128, :], osb[:, so, :], reads=[("osb", so)])
            kb.barrier()
            oc3 = sbl("oc3", [128, 2, 3, 260], F32)
            rd = sbl("rdC", [128, 2, 4], F32)
            oc = sbl("ocC", [128, 2, 256], BF16)
            for tt in range(NT):
                s = tt % 2
                kb.dma("sp", oc3[:, s], OC_d[:, tt * 128:(tt + 1) * 128].rearrange("g p h c -> p g (h c)"), writes=[("oc3", s)])
                kb.op("dve", [("oc3", s)], [("oc3", s)], lambda e, s=s: e.tensor_tensor(
                    out=oc3[:, s, 0], in0=oc3[:, s, 0], in1=oc3[:, s, 1], op=ALU.add))
                kb.op("dve", [("oc3", s)], [("oc3", s)], lambda e, s=s: e.tensor_tensor(
                    out=oc3[:, s, 0], in0=oc3[:, s, 0], in1=oc3[:, s, 2], op=ALU.add))
                v0 = oc3[:, s, 0].rearrange("p (h c) -> p h c", c=65)
                kb.op("dve", [("oc3", s)], [("rdC", s)], lambda e, s=s, v0=v0: e.reciprocal(out=rd[:, s, :], in_=v0[:, :, 64]))
                kb.op("dve", [("oc3", s), ("rdC", s)], [("ocC", s)], lambda e, s=s, v0=v0: e.tensor_tensor(
                    out=oc[:, s, :].rearrange("p (h c) -> p h c", c=64), in0=v0[:, :, 0:64],
                    in1=rd[:, s, :].unsqueeze(2).to_broadcast([128, 4, 64]), op=ALU.mult))
                kb.dma("sp", AT_d[tt * 128:(tt + 1) * 128, 1024:1280], oc[:, s, :], reads=[("ocC", s)])
            kb.barrier()


    def load_cast(st_pool, dst, src_ap, key, nparts=128):
        stg, = st_pool
        shp = dst.shape
        A, Bn = shp[1], shp[2]
        per = max(1, 2048 // Bn)
        for a0 in range(0, A, per):
            a1 = min(A, a0 + per)
            sl = getattr(load_cast, "n", 0) % 2
            load_cast.n = getattr(load_cast, "n", 0) + 1
            kb.dma("sp", stg[0:nparts, sl, 0:(a1 - a0) * Bn].rearrange("p (a b) -> p a b", b=Bn), src_ap[:, a0:a1, :],
                   writes=[("stg", sl)])
            kb.op("pool", [("stg", sl)], [key], lambda e, a0=a0, a1=a1, sl=sl: e.tensor_copy(
                out=dst[0:nparts, a0:a1, :], in_=stg[0:nparts, sl, 0:(a1 - a0) * Bn].rearrange("p (a b) -> p a b", b=Bn)))

    def phase3(layer, xin_d):
        with ExitStack() as st:
            def sbl(name, shape, dt):
                return st.enter_context(sbt(name, list(shape), dt))
            stg = sbl("stg3", [128, 2, 2048], F32)
            Wb = sbl("Wb", [128, 10, D], BF16)
            Wo = sbl("Wo", [128, DC, D], BF16)
            att = sbl("att", [128, 2, 1280], BF16)
            attT = sbl("attT", [128, 10, 512], BF16)
            gt = sbl("gt", [128, GC, 512], BF16)
            m1 = sbl("m1", [128, 2, 512], F32); m2 = sbl("m2", [128, 2, 512], F32)
            mg = sbl("mg", [128, DC, 512], BF16)
            xt = sbl("xt3", [128, 2, D], F32)
            tpa = st.enter_context(pst_("tpa", [128, 2, 8, 128], BF16))
            yp = st.enter_context(pst_("yp", [128, 3, 512], F32))
            op_ = st.enter_context(pst_("op3", [128, 2, 512], F32))
            load_cast((stg,), Wb[:, 0:4, :], wba_d[layer].rearrange("(c p) n -> p c n", p=128), "Wb")
            load_cast((stg,), Wb[:, 4:8, :], wbb_d[layer].rearrange("(c p) n -> p c n", p=128), "Wb")
            load_cast((stg,), Wb[:, 8:10, :], wbc_d[layer].rearrange("(c p) n -> p c n", p=128), "Wb")
            load_cast((stg,), Wo[:], wout_d[layer].rearrange("(c p) n -> p c n", p=128), "Wo")
            nt4 = 0
            for tb in range(S // 512):
                ts_ = slice(tb * 512, (tb + 1) * 512)
                kb.dma("sp", gt[:], GT_d[:, :, ts_].rearrange("c p s -> p c s"), writes=["gt"])
                for t4 in range(4):
                    tt = tb * 4 + t4
                    s = tt % 2
                    kb.dma("sp", att[:, s, :], AT_d[tt * 128:(tt + 1) * 128, :], writes=[("att", s)])
                    for hf in range(2):
                        for c in range(5):
                            kb.op("pe", [("att", s), "identb"], [("tpa", hf)], lambda e, s=s, c=c, hf=hf: e.transpose(
                                out=tpa[:, hf, c, :], in_=att[:, s, (hf * 5 + c) * 128:(hf * 5 + c + 1) * 128], identity=identb[:]),
                                inc=(c == 4))
                        kb.op("act", [("tpa", hf)], ["attT"], lambda e, hf=hf, t4=t4: e.copy(
                            out=attT[:, hf * 5:hf * 5 + 5, t4 * 128:(t4 + 1) * 128], in_=tpa[:, hf, 0:5, :]))
                for fo in range(DC):
                    fs = slice(fo * 128, (fo + 1) * 128)
                    s = fo % 2
                    for bi, (c0, c1) in enumerate(((0, 4), (4, 8), (8, 10))):
                        for c in range(c0, c1):
                            kb.op("pe", ["Wb", "attT"], [("yp", bi)], lambda e, c=c, bi=bi, c0=c0, c1=c1, fs=fs: e.matmul(
                                yp[:, bi, :], lhsT=Wb[:, c, fs], rhs=attT[:, c, :], start=(c == c0), stop=(c == c1 - 1)),
                                inc=(c == c1 - 1))
                    kb.op("dve", [("yp", 0), "gt"], [("m1", s)], lambda e, s=s, fo=fo: e.tensor_tensor(
                        out=m1[:, s, :], in0=yp[:, 0, :], in1=gt[:, fo, :], op=ALU.mult))
                    kb.op("dve", [("yp", 1), "gt"], [("m2", s)], lambda e, s=s, fo=fo: e.tensor_tensor(
                        out=m2[:, s, :], in0=yp[:, 1, :], in1=gt[:, DC + fo, :], op=ALU.mult))
                    kb.op("dve", [("m1", s), ("m2", s)], [("m1", s)], lambda e, s=s: e.tensor_tensor(
                        out=m1[:, s, :], in0=m1[:, s, :], in1=m2[:, s, :], op=ALU.add))
                    kb.op("dve", [("yp", 2), "gt"], [("m2", s)], lambda e, s=s, fo=fo: e.tensor_tensor(
                        out=m2[:, s, :], in0=yp[:, 2, :], in1=gt[:, 2 * DC + fo, :], op=ALU.mult))
                    kb.op("dve", [("m1", s), ("m2", s)], ["mg"], lambda e, s=s, fo=fo: e.tensor_tensor(
                        out=mg[:, fo, :], in0=m1[:, s, :], in1=m2[:, s, :], op=ALU.add))
                for t4 in range(4):
                    tt = tb * 4 + t4
                    s = tt % 2
                    kb.dma("sp", xt[:, s, :], xin_d[tt * 128:(tt + 1) * 128, :], writes=[("xt3", s)])
                    for cb in range(0, D, 512):
                        cw_ = min(512, D - cb)
                        nt4 += 1
                        so = nt4 % 2
                        for fo in range(DC):
                            kb.op("pe", ["mg", "Wo"], [("op3", so)], lambda e, fo=fo, so=so, t4=t4, cb=cb, cw_=cw_: e.matmul(
                                op_[:, so, 0:cw_], lhsT=mg[:, fo, t4 * 128:(t4 + 1) * 128], rhs=Wo[:, fo, cb:cb + cw_],
                                start=(fo == 0), stop=(fo == DC - 1)), inc=(fo == DC - 1))
                        kb.op("dve", [("op3", so), ("xt3", s)], [("xt3", s)], lambda e, so=so, s=s, cb=cb, cw_=cw_: e.tensor_tensor(
                            out=xt[:, s, cb:cb + cw_], in0=op_[:, so, 0:cw_], in1=xt[:, s, cb:cb + cw_], op=ALU.add))
                    kb.dma("sp", x1_d[tt * 128:(tt + 1) * 128, :], xt[:, s, :], reads=[("xt3", s)], key=("x1st", s))
            kb.barrier()

    def phase4(layer, xout_d):
        with ExitStack() as st:
            def sbl(name, shape, dt):
                return st.enter_context(sbt(name, list(shape), dt))
            TB = 256
            stg = sbl("stg4", [128, 2, 2048], F32)
            Wfi = sbl("Wfi", [128, DC, 2 * DFF], BF16)
            Wfo = sbl("Wfo", [128, FC, D], BF16)
            G2 = sbl("G2", [128, D], F32)
            xt = sbl("xt4", [128, 2, D], F32)
            junk = sbl("junk4", [128, D], BF16)
            hb_ = sbl("hb4", [128, 2, D], BF16)
            ss = sbl("ss4", [128, 2, 4], F32)
            h2T = sbl("h2T", [128, DC, TB], BF16)
            sg = sbl("sg", [128, 2, TB], F32)
            actT = sbl("actT", [128, FC, TB], BF16)
            tp = st.enter_context(pst_("tp4", [128, 2, DC, 128], BF16))
            gp = st.enter_context(pst_("gp", [128, 2, 512], F32))
            up = st.enter_context(pst_("up", [128, 2, 512], F32))
            op_ = st.enter_context(pst_("op4", [128, 2, 512], F32))
            kb.dma("sp", G2[:], n2_d[layer:layer + 1, :].to_broadcast([128, D]), writes=["G2"])
            wfi_v = wfi_d[layer].rearrange("(c p) n -> p c n", p=128)
            for n0 in range(0, DFF, 256):
                n1 = min(DFF, n0 + 256)
                for half in range(2):
                    o_ = half * DFF
                    load_cast((stg,), Wfi[:, :, o_ + n0:o_ + n1], wfi_v[:, :, o_ + n0:o_ + n1], ("Wfi", half, n0 // 256))
            wfo_v = wfo_d[layer].rearrange("(c p) n -> p c n", p=128)
            for c0 in range(0, FC, 2):
                c1 = min(FC, c0 + 2)
                load_cast((stg,), Wfo[:, c0:c1, :], wfo_v[:, c0:c1, :], ("Wfo", c0 // 2))
            nt4 = 0
            for tb in range(S // TB):
                for t4 in range(TB // 128):
                    tt = tb * (TB // 128) + t4
                    s = t4 % 2
                    kb.dma("act", xt[:, s], x1_d[tt * 128:(tt + 1) * 128, :], writes=[("xt4", s)])
                    kb.op("act", [("xt4", s)], ["junk4", ("ss4", s)], lambda e, s=s: e.activation(
                        out=junk[:], in_=xt[:, s], func=AF.Square, accum_out=ss[:, s, 0:1]))
                    kb.op("act", [("ss4", s)], [("ss4", s)], lambda e, s=s: e.activation(
                        out=ss[:, s, 1:2], in_=ss[:, s, 0:1], func=AF.Sqrt, bias=epsc[:, 0:1], scale=1.0 / D))
                    kb.op("dve", [("ss4", s)], [("ss4", s)], lambda e, s=s: e.reciprocal(out=ss[:, s, 2:3], in_=ss[:, s, 1:2]))
                    kb.op("dve", [("xt4", s), ("ss4", s), "G2"], [("hb4", s)], lambda e, s=s: e.scalar_tensor_tensor(
                        out=hb_[:, s], in0=xt[:, s], scalar=ss[:, s, 2:3], in1=G2[:], op0=ALU.mult, op1=ALU.mult))
                    for c in range(DC):
                        kb.op("pe", [("hb4", s), "identb"], [("tp4", s)], lambda e, s=s, c=c: e.transpose(
                            out=tp[:, s, c, :], in_=hb_[:, s, c * 128:(c + 1) * 128], identity=identb[:]), inc=(c == DC - 1))
                    kb.op("act", [("tp4", s)], ["h2T"], lambda e, s=s, t4=t4: e.copy(
                        out=h2T[:, :, t4 * 128:(t4 + 1) * 128], in_=tp[:, s]))
                for fc in range(FC):
                    s = fc % 2
                    for c in range(DC):
                        kb.op("pe", [("Wfi", 0, fc // 2), "h2T"], [("gp", s)], lambda e, c=c, s=s, fc=fc: e.matmul(
                            gp[:, s, 0:TB], lhsT=Wfi[:, c, fc * 128:(fc + 1) * 128], rhs=h2T[:, c, :], start=(c == 0), stop=(c == DC - 1)),
                            inc=(c == DC - 1))
                    for c in range(DC):
                        kb.op("pe", [("Wfi", 1, fc // 2), "h2T"], [("up", s)], lambda e, c=c, s=s, fc=fc: e.matmul(
                            up[:, s, 0:TB], lhsT=Wfi[:, c, DFF + fc * 128:DFF + (fc + 1) * 128], rhs=h2T[:, c, :], start=(c == 0),
                            stop=(c == DC - 1)), inc=(c == DC - 1))
                    kb.op("act", [("gp", s)], [("sg", s)], lambda e, s=s: e.activation(
                        out=sg[:, s, :], in_=gp[:, s, 0:TB], func=AF.Silu))
                    kb.op("dve", [("up", s), ("sg", s)], ["actT"], lambda e, s=s, fc=fc: e.tensor_tensor(
                        out=actT[:, fc, :], in0=up[:, s, 0:TB], in1=sg[:, s, :], op=ALU.mult))
                for t4 in range(TB // 128):
                    tt = tb * (TB // 128) + t4
                    s = t4 % 2
                    for cb in range(0, D, 512):
                        cw_ = min(512, D - cb)
                        nt4 += 1
                        so = nt4 % 2
                        for fc in range(FC):
                            kb.op("pe", ["actT", ("Wfo", fc // 2)], [("op4", so)], lambda e, fc=fc, so=so, t4=t4, cb=cb, cw_=cw_: e.matmul(
                                op_[:, so, 0:cw_], lhsT=actT[:, fc, t4 * 128:(t4 + 1) * 128], rhs=Wfo[:, fc, cb:cb + cw_],
                                start=(fc == 0), stop=(fc == FC - 1)), inc=(fc == FC - 1))
                        kb.op("dve", [("op4", so), ("xt4", s)], [("xt4", s)], lambda e, so=so, s=s, cb=cb, cw_=cw_: e.tensor_tensor(
                            out=xt[:, s, cb:cb + cw_], in0=op_[:, so, 0:cw_], in1=xt[:, s, cb:cb + cw_], op=ALU.add))
                    kb.dma("sp", xout_d[tt * 128:(tt + 1) * 128, :], xt[:, s, :], reads=[("xt4", s)], key=("x2st", s))
            kb.barrier()

    LIMIT = getattr(build_nc, "limit", "all")
    NL = getattr(build_nc, "nlayers", 2)
    x2_d = dscr("x2", [S, D], F32)
    if LIMIT == "all":
        for layer in range(NL):
            xin = x_d if layer == 0 else x2_d
            xout = y_d if layer == NL - 1 else x2_d
            phase1(layer, xin)
            phase2A(layer)
            phase2B(layer)
            phase2C(layer)
            phase3(layer, xin)
            phase4(layer, xout)
    else:
        if LIMIT not in ("none", "setup"):
            phase1(0, x_d)
        if LIMIT == "A":
            phase2A(0)
        if LIMIT == "B":
            phase2B(0)
        if LIMIT == "C":
            phase2C(0)
        with ExitStack() as st:
            yt = st.enter_context(sbt("yt", [128, D], F32))
            for tt in range(NT):
                kb.dma("sp", yt[:], x_d[tt * 128:(tt + 1) * 128, :], writes=["yt"])
                kb.dma("sp", y_d[tt * 128:(tt + 1) * 128, :], yt[:], reads=["yt"], key="ystore")
            kb.barrier()
    stack.close()
    return nc


_NC_CACHE = {}


def kernel(**inputs):
    x = np.asarray(inputs["x"], dtype=np.float32)
    B, S, D = x.shape
    DFF = int(np.asarray(inputs["w_ffn_out"]).shape[1])
    key = (S, D, DFF)
    if key not in _NC_CACHE:
        _NC_CACHE[key] = build_nc(S, D, DFF)
    nc = _NC_CACHE[key]
    consts = make_consts()
    shared = {k: np.ascontiguousarray(np.asarray(v, dtype=np.float32)) for k, v in inputs.items() if k != "x"}
    shared.update(consts)
    in_maps = []
    for b in range(B):
        m = dict(shared)
        m["x"] = np.ascontiguousarray(x[b])
        in_maps.append(m)
    res = run_bass_kernel_spmd(nc, in_maps, core_ids=list(range(B)))
    return np.stack([np.asarray(r["y"], dtype=np.float32) for r in res.results], axis=0)
```

```python
import math
from contextlib import ExitStack
import numpy as np
import concourse.bass as bass
import concourse.mybir as mybir
from concourse.bass_types import AP
from concourse.bass_utils import run_bass_kernel_spmd

F32 = mybir.dt.float32
BF16 = mybir.dt.bfloat16
AF = mybir.ActivationFunctionType
ALU = mybir.AluOpType
AX = mybir.AxisListType

BIG = 30000.0
BIGF = 1.0e30
EPS = 1e-6
NBIS = 14
N_CORES = 8


def _rel_bucket_np(dist):
    n = np.maximum(dist, 0)
    nf = np.maximum(n, 1).astype(np.float32)
    large = 16 + (np.log(nf / np.float32(16)) / np.float32(math.log(2048 / 16)) * np.float32(16)).astype(np.int32)
    return np.where(n < 16, n, np.minimum(large, 31))


def make_consts():
    c = {}
    c["ident"] = np.eye(128, dtype=np.float32)
    c["flipj"] = np.eye(128, dtype=np.float32)[::-1].copy()
    LX = 1920
    dist = np.arange(LX) - 127
    oh = np.zeros((33, LX), np.float32)
    bk = _rel_bucket_np(dist)
    for x in range(LX):
        if dist[x] >= 0:
            oh[bk[x], x] = 1
        else:
            oh[32, x] = 1
    c["oh_ab"] = oh
    LW = 768
    dist = np.arange(LW) - 127
    oh = np.zeros((33, LW), np.float32)
    bk = _rel_bucket_np(dist)
    for x in range(LW):
        if 0 <= dist[x] < 512:
            oh[bk[x], x] = 1
        else:
            oh[32, x] = 1
    c["oh_w"] = oh
    LC = 384
    ohc = np.zeros((3, 33, LC), np.float32)
    for gi, dil in enumerate((1, 4, 16)):
        e = np.arange(LC) - 127
        bk = _rel_bucket_np(e * dil)
        for x in range(LC):
            if 0 <= e[x] <= 128:
                ohc[gi, bk[x], x] = 1
            else:
                ohc[gi, 32, x] = 1
    c["oh_c"] = ohc
    return c


class KB:
    def __init__(self, nc, stack):
        self.nc = nc
        self.stack = stack
        self.E = {"pe": nc.tensor, "act": nc.scalar, "dve": nc.vector, "pool": nc.gpsimd, "sp": nc.sync}
        self.sems = []
        self.semval = []
        self.free = []
        self.esem = {}
        for e in ("pe", "act", "dve", "pool"):
            self.esem[e] = self._alloc()
        self.seen = {e: {} for e in self.E}
        self.lastw = {}
        self.lastr = {}
        self.dsem = {}

    def _alloc(self):
        if self.free:
            return self.free.pop()
        h = self.stack.enter_context(self.nc.semaphore(f"sm{len(self.sems)}"))
        self.sems.append(h)
        self.semval.append(0)
        return len(self.sems) - 1

    def _wait(self, eng, si, v):
        if v <= 0 or self.seen[eng].get(si, 0) >= v:
            return
        self.E[eng].wait_ge(self.sems[si], v)
        self.seen[eng][si] = v

    def _deps(self, eng, reads, writes):
        need = {}
        for k in reads:
            w = self.lastw.get(k)
            if w:
                need[w[0]] = max(need.get(w[0], 0), w[1])
        for k in writes:
            w = self.lastw.get(k)
            if w:
                need[w[0]] = max(need.get(w[0], 0), w[1])
            for si, v in self.lastr.get(k, {}).items():
                need[si] = max(need.get(si, 0), v)
        for si, v in need.items():
            if eng == "pe" and si == self.esem["pe"]:
                continue
            if si == self.esem.get(eng) and v > self.semval[si]:
                continue
            self._wait(eng, si, v)

    def _mark(self, si, v, reads, writes):
        for k in reads:
            d = self.lastr.setdefault(k, {})
            d[si] = max(d.get(si, 0), v)
        for k in writes:
            self.lastw[k] = (si, v)
            self.lastr[k] = {}

    def op(self, eng, reads, writes, fn, inc=True):
        self._deps(eng, reads, writes)
        ins = fn(self.E[eng])
        si = self.esem[eng]
        if inc:
            ins.then_inc(self.sems[si], 1)
            self.semval[si] += 1
            v = self.semval[si]
        else:
            v = self.semval[si] + 1
        self._mark(si, v, reads, writes)
        return ins

    def dma(self, q, out, in_, reads=(), writes=(), key=None, **kw):
        self._deps(q, reads, writes)
        k = key if key is not None else (writes[0] if writes else reads[0])
        si = self.dsem.get(k)
        if si is None:
            si = self._alloc()
            self.dsem[k] = si
        self._wait(q, si, self.semval[si])
        ins = self.E[q].dma_start(out=out, in_=in_, **kw)
        ins.then_inc(self.sems[si], 16)
        self.semval[si] += 16
        self._mark(si, self.semval[si], reads, writes)

    def barrier(self):
        used = list(self.esem.values()) + list(self.dsem.values())
        for eng in self.E:
            for si in used:
                self._wait(eng, si, self.semval[si])
        for si in self.dsem.values():
            self.free.append(si)
        self.dsem = {}
        self.lastw = {}
        self.lastr = {}


def build_nc(S, D, DFF, debug=False):
    NT = S // 128
    DC = D // 128
    FC = DFF // 128
    TOPK = min(256, S // 4)
    NCMP = (S - 32) // 16 + 1
    NCC = (NCMP + 127) // 128
    NSEL = S // 64
    assert NSEL > 16 and S % 512 == 0 and S // 16 >= 128
    ND = min(14, NT)
    sizes = [512, 64, 64, 256, 64, 4, 512, 768, 24, 2304, 3 * D]
    offs = np.concatenate([[0], np.cumsum(sizes)]).astype(int)
    o_aq, o_ak, o_av, o_iq, o_ik, o_iw, o_bq, o_bkv, o_bg, o_c, o_g = [int(v) for v in offs[:11]]
    INTOT = int(offs[11])
    GC = 3 * DC

    nc = bass.Bass("TRN2", target_bir_lowering=False)
    skind = "ExternalOutput" if debug else "Internal"

    def din(name, shape, dt=F32):
        return nc.dram_tensor(name, list(shape), dt, kind="ExternalInput").ap()

    def dscr(name, shape, dt=BF16):
        return nc.dram_tensor(name, list(shape), dt, kind=skind).ap()

    x_d = din("x", [S, D])
    n1_d = din("norm1_g", [2, D])
    n2_d = din("norm2_g", [2, D])
    win_d = din("w_in", [2, D, INTOT])
    qk_d = din("qk_norm_g", [2, 6, 64])
    cpos_d = din("nsa_cmp_pos", [2, 2, 32, 64])
    cw_d = din("nsa_cmp_w", [2, 2, 32, 64, 64])
    wba_d = din("w_branch_a", [2, 512, D])
    wbb_d = din("w_branch_b", [2, 512, D])
    wbc_d = din("w_branch_c", [2, 256, D])
    wout_d = din("w_out", [2, D, D])
    wfi_d = din("w_ffn_in", [2, D, 2 * DFF])
    wfo_d = din("w_ffn_out", [2, DFF, D])
    rel_d = din("rel_bias", [32, 28])
    ident_d = din("ident", [128, 128])
    flipj_d = din("flipj", [128, 128])
    ohab_d = din("oh_ab", [33, 1920])
    ohw_d = din("oh_w", [33, 768])
    ohc_d = din("oh_c", [3, 33, 384])
    y_d = nc.dram_tensor("y", [S, D], F32, kind="ExternalOutput").ap()

    x1_d = dscr("x1", [S, D], F32)
    t8ab_d = dscr("t8ab", [28, 1920], F32)
    t8w_d = dscr("t8w", [28, 768], F32)
    t8c_d = dscr("t8c", [3, 28, 384], F32)
    bA_d = dscr("bA", [128, ND, 8, 128])
    bBs_d = dscr("bBs", [128, ND, 8, 128])
    bBw_d = dscr("bBw", [128, 5, 8, 128])
    bC_d = dscr("bC", [128, 3, 2, 4, 128])
    QA_d = dscr("QA", [4, 128, S]); KA_d = dscr("KA", [1, 128, S])
    IQ_d = dscr("IQ", [2, 128, S]); IK_d = dscr("IK", [1, 128, S])
    QB_d = dscr("QB", [4, 128, S]); KCVC_d = dscr("KCVC", [2, 128, S])
    KS_d = dscr("KS", [2, 128, S]); KW_d = dscr("KW", [2, 128, S])
    QC_d = dscr("QC", [6, 128, S]); KCc_d = dscr("KCc", [6, 128, S])
    GT_d = dscr("GT", [GC, 128, S])
    VA_d = dscr("VA", [S, 1, 65]); VS_d = dscr("VS", [S, 2, 65]); VW_d = dscr("VW", [S, 2, 65])
    VC_d = dscr("VC", [3, S, 4, 65])
    IW_d = dscr("IW", [S, 4], F32); BG_d = dscr("BG", [S, 24], F32)
    OC_d = dscr("OCs", [3, S, 4, 65], F32)
    AT_d = dscr("ATT", [S, 1280])

    stack = ExitStack()
    kb = KB(nc, stack)
    uq = [0]
    reg_negbig = nc.gpsimd.to_reg(-BIG)
    reg_negbigf = nc.gpsimd.to_reg(-BIGF)
    reg_zero = nc.gpsimd.to_reg(0.0)

    def sbt(name, shape, dt):
        uq[0] += 1
        return nc.sbuf_tensor(f"{name}_{uq[0]}", shape, dt)

    def pst_(name, shape, dt):
        uq[0] += 1
        return nc.psum_tensor(f"{name}_{uq[0]}", shape, dt)

    def sb(name, shape, dt):
        return stack.enter_context(sbt(name, list(shape), dt))

    def ps(name, shape, dt=F32):
        return stack.enter_context(pst_(name, list(shape), dt))

    identf = sb("identf", [128, 128], F32)
    flipf = sb("flipf", [128, 128], F32)
    identb = sb("identb", [128, 128], BF16)
    negI4 = sb("negI4", [128, 4, 128], BF16)
    bdiag = sb("bdiag", [128, 128], BF16)
    epsc = sb("epsc", [128, 1], F32)
    kb.dma("sp", identf[:], ident_d, writes=["identf"])
    kb.dma("sp", flipf[:], flipj_d, writes=["flipf"])
    kb.op("dve", ["identf"], ["identb"], lambda e: e.tensor_copy(out=identb[:], in_=identf[:]))
    for hh in range(4):
        kb.op("dve", ["identf"], ["negI4"], lambda e, hh=hh: e.tensor_scalar(
            out=negI4[:, hh, :], in0=identf[:], scalar1=-BIG, scalar2=None, op0=ALU.mult))
    kb.op("dve", [], ["bdiag"], lambda e: e.memset(bdiag[:], 0.0))
    kb.op("dve", [], ["bdiag"], lambda e: e.memset(bdiag[0:64, 0:64], 1.0 / 64))
    kb.op("dve", [], ["bdiag"], lambda e: e.memset(bdiag[64:128, 64:128], 1.0 / 64))
    kb.op("dve", [], ["epsc"], lambda e: e.memset(epsc[:], EPS))

    def setup_bias():
        with ExitStack() as st:
            rel8 = st.enter_context(sbt("rel8", [33, 28], F32))
            ohs = st.enter_context(sbt("ohs", [33, 1920], F32))
            t8s = st.enter_context(sbt("t8s", [28, 1920], F32))
            hk = st.enter_context(sbt("hk", [128, 2, 4, 128], F32))
            bt = st.enter_context(sbt("bt", [128, 2, 4, 128], BF16))
            pst = st.enter_context(pst_("pst", [128, 2, 512], F32))
            kb.op("dve", [], ["rel8"], lambda e: e.memset(rel8[:], -BIG / 8))
            kb.dma("sp", rel8[0:32, :], rel_d, writes=["rel8"])
            kb.op("act", ["rel8"], ["rel8"], lambda e: e.mul(out=rel8[:], in_=rel8[:], mul=8.0))
            tabs = [(ohab_d, t8ab_d, 1920), (ohw_d, t8w_d, 768)] + [(ohc_d[gi], t8c_d[gi], 384) for gi in range(3)]
            for ti, (oh_d, t8_d, L) in enumerate(tabs):
                kb.dma("sp", ohs[:, 0:L], oh_d, writes=["ohs"])
                nb = (L + 383) // 384
                for b in range(nb):
                    sl = slice(b * 384, min(L, (b + 1) * 384))
                    w = sl.stop - sl.start
                    kb.op("pe", ["rel8", "ohs"], [("pst", b % 2)], lambda e, sl=sl, w=w, b=b: e.matmul(
                        pst[0:28, b % 2, 0:w], lhsT=rel8[:, :], rhs=ohs[:, sl], start=True, stop=True))
                    kb.op("act", [("pst", b % 2)], ["t8s"], lambda e, sl=sl, w=w, b=b: e.copy(
                        out=t8s[:, sl], in_=pst[0:28, b % 2, 0:w]))
                kb.dma("sp", t8_d, t8s[:, 0:L], reads=["t8s"], key=("t8st", ti))
            kb.barrier()
            jobs = []
            for d in range(ND):
                for hb in range(2):
                    jobs.append((t8ab_d, 1920, 128 * d, hb * 4, bA_d[:, d, hb * 4:hb * 4 + 4, :]))
                    jobs.append((t8ab_d, 1920, 128 * d, 8 + hb * 4, bBs_d[:, d, hb * 4:hb * 4 + 4, :]))
            for d in range(5):
                for hb in range(2):
                    jobs.append((t8w_d, 768, 128 * d, 8 + hb * 4, bBw_d[:, d, hb * 4:hb * 4 + 4, :]))
            for gi in range(3):
                for dj in range(2):
                    jobs.append((t8c_d[gi], 384, 128 * dj, 16 + 4 * gi, bC_d[:, gi, dj, :, :]))
            for n, (t8_d, L, c0, h0, dst) in enumerate(jobs):
                s = n % 2
                src = AP(tensor=t8_d.tensor, offset=t8_d.offset + h0 * L + c0, ap=[[1, 128], [L, 4], [1, 128]])
                kb.dma("sp", hk[:, s], src, writes=[("hk", s)])
                kb.op("pe", [("hk", s), "flipf"], [("pst", s)], lambda e, s=s: e.matmul(
                    pst[:, s, :], lhsT=flipf[:], rhs=hk[:, s].rearrange("p h f -> p (h f)"), start=True, stop=True))
                kb.op("act", [("pst", s)], [("bt", s)], lambda e, s=s: e.copy(
                    out=bt[:, s].rearrange("p h f -> p (h f)"), in_=pst[:, s, :]))
                kb.dma("act", dst, bt[:, s], reads=[("bt", s)])
            kb.barrier()

    if getattr(build_nc, "limit", "all") != "none":
        setup_bias()

    def phase1(layer, xin_d):
        with ExitStack() as st:
            def sbl(name, shape, dt):
                return st.enter_context(sbt(name, list(shape), dt))
            hT = sbl("hT", [128, DC, S], BF16)
            G1 = sbl("G1", [128, D], F32)
            g6 = sbl("g6", [128, 6], F32)
            xt = sbl("xt", [128, 2, D], F32)
            junk = sbl("junk", [128, D], BF16)
            hb_ = sbl("hb", [128, 2, D], BF16)
            ss = sbl("ss", [128, 2, 4], F32)
            wst = sbl("wst", [128, 2, DC, 512], F32)
            wbf = sbl("wbf", [128, 2, DC, 512], BF16)
            sq = sbl("sq", [128, 2, 512], BF16)
            sd = sbl("sd", [128, 2, 512], F32)
            ot = sbl("ot", [128, 3, 512], BF16)
            otf = sbl("otf", [128, 2, 32], F32)
            va = sbl("va", [128, 2, 4, 65], BF16)
            tp = st.enter_context(pst_("tp", [128, 2, DC, 128], BF16))
            p1 = st.enter_context(pst_("p1", [128, 3, 512], F32))
            p2 = st.enter_context(pst_("p2", [128, 2, 512], F32))

            kb.dma("sp", G1[:], n1_d[layer:layer + 1, :].to_broadcast([128, D]), writes=["G1"])
            for half in range(2):
                kb.dma("sp", g6[half * 64:(half + 1) * 64, :], qk_d[layer].rearrange("j d -> d j"),
                       writes=["g6"], key=("g6", half), allow_slow_non_contiguous=True)
            for s in range(2):
                kb.op("dve", [], [("va", s)], lambda e, s=s: e.memset(va[:, s], 1.0))
            for tt in range(NT):
                s = tt % 2
                kb.dma("sp", xt[:, s], xin_d[tt * 128:(tt + 1) * 128, :], writes=[("xt", s)])
                kb.op("act", [("xt", s)], ["junk", ("ss", s)], lambda e, s=s: e.activation(
                    out=junk[:], in_=xt[:, s], func=AF.Square, accum_out=ss[:, s, 0:1]))
                kb.op("act", [("ss", s)], [("ss", s)], lambda e, s=s: e.activation(
                    out=ss[:, s, 1:2], in_=ss[:, s, 0:1], func=AF.Sqrt, bias=epsc[:, 0:1], scale=1.0 / D))
                kb.op("dve", [("ss", s)], [("ss", s)], lambda e, s=s: e.reciprocal(out=ss[:, s, 2:3], in_=ss[:, s, 1:2]))
                kb.op("dve", [("xt", s), ("ss", s), "G1"], [("hb", s)], lambda e, s=s: e.scalar_tensor_tensor(
                    out=hb_[:, s], in0=xt[:, s], scalar=ss[:, s, 2:3], in1=G1[:], op0=ALU.mult, op1=ALU.mult))
                for c in range(DC):
                    kb.op("pe", [("hb", s), "identb"], [("tp", s)], lambda e, s=s, c=c: e.transpose(
                        out=tp[:, s, c, :], in_=hb_[:, s, c * 128:(c + 1) * 128], identity=identb[:]), inc=(c == DC - 1))
                kb.op("act", [("tp", s)], ["hT"], lambda e, s=s, tt=tt: e.copy(
                    out=hT[:, :, tt * 128:(tt + 1) * 128], in_=tp[:, s]))

            def perm_cols(gi, pp0, n):
                if gi is None or gi == 0:
                    return lambda c: hT[:, c, pp0:pp0 + n]
                dil = (1, 4, 16)[gi]
                U = S // dil
                r, u0 = pp0 // U, pp0 % U
                assert u0 + n <= U
                return lambda c: hT[:, c, r + dil * u0: r + dil * (u0 + n - 1) + 1: dil]

            wcount = [0]

            def load_w(segs):
                s = wcount[0] % 2
                wcount[0] += 1
                o = 0
                for (c0, n) in segs:
                    kb.dma("sp", wst[:, s, :, o:o + n], win_d[layer, :, c0:c0 + n].rearrange("(c p) n -> p c n", p=128),
                           writes=[("wst", s)], key=("wst", s, o))
                    o += n
                kb.op("pool", [("wst", s)], [("wbf", s)], lambda e, s=s, o=o: e.tensor_copy(
                    out=wbf[:, s, :, 0:o], in_=wst[:, s, :, 0:o]))
                return s, o

            it = [0]
            pend1 = [None]

            def fm_chunk(ws, wo, dst, post, gidx=None, gi=None):
                TB = 512 if gi in (None, 0) else min(512, S // (1, 4, 16)[gi])
                for tb in range(S // TB):
                    s = it[0] % 2
                    s3 = it[0] % 3
                    it[0] += 1
                    cols = perm_cols(gi, tb * TB, TB)
                    for c in range(DC):
                        kb.op("pe", [("wbf", ws), "hT"], [("p1", s3)], lambda e, c=c, s3=s3, cols=cols: e.matmul(
                            p1[:, s3, 0:TB], lhsT=wbf[:, ws, c, wo:wo + 128], rhs=cols(c), start=(c == 0), stop=(c == DC - 1)),
                            inc=(c == DC - 1))
                    store = lambda tb=tb, s3=s3: kb.dma("sp", dst[:, tb * TB:(tb + 1) * TB], ot[:, s3, 0:TB], reads=[("ot", s3)])
                    if post == "plain":
                        kb.op("act", [("p1", s3)], [("ot", s3)], lambda e, s3=s3: e.copy(out=ot[:, s3, 0:TB], in_=p1[:, s3, 0:TB]))
                        store()
                    elif post == "sigmoid":
                        kb.op("act", [("p1", s3)], [("ot", s3)], lambda e, s3=s3: e.activation(
                            out=ot[:, s3, 0:TB], in_=p1[:, s3, 0:TB], func=AF.Sigmoid))
                        store()
                    else:
                        kb.op("act", [("p1", s3)], [("sq", s)], lambda e, s=s, s3=s3: e.activation(
                            out=sq[:, s, 0:TB], in_=p1[:, s3, 0:TB], func=AF.Square))

                        def rest(s=s, s3=s3, store=store):
                            kb.op("pe", [("sq", s), "bdiag"], [("p2", s)], lambda e: e.matmul(
                                p2[:, s, 0:TB], lhsT=bdiag[:], rhs=sq[:, s, 0:TB], start=True, stop=True))
                            kb.op("act", [("p2", s)], [("sd", s)], lambda e: e.activation(
                                out=sd[:, s, 0:TB], in_=p2[:, s, 0:TB], func=AF.Sqrt, bias=epsc[:, 0:1], scale=1.0))
                            kb.op("dve", [("sd", s)], [("sd", s)], lambda e: e.reciprocal(out=sd[:, s, 0:TB], in_=sd[:, s, 0:TB]))
                            kb.op("dve", [("p1", s3), ("sd", s), "g6"], [("ot", s3)], lambda e: e.scalar_tensor_tensor(
                                out=ot[:, s3, 0:TB], in0=p1[:, s3, 0:TB], scalar=g6[:, gidx:gidx + 1], in1=sd[:, s, 0:TB],
                                op0=ALU.mult, op1=ALU.mult))
                            store()
                        prev = pend1[0]
                        pend1[0] = rest
                        if prev is not None:
                            prev()
                if pend1[0] is not None:
                    p_ = pend1[0]
                    pend1[0] = None
                    p_()

            def fm_group(chunks):
                def segs_of(b):
                    segs = []
                    for ch in chunks[b:b + 4]:
                        segs += ch[0]
                    return segs
                nxt = load_w(segs_of(0))
                for b in range(0, len(chunks), 4):
                    grp = chunks[b:b + 4]
                    ws, tot = nxt
                    if b + 4 < len(chunks):
                        nxt = load_w(segs_of(b + 4))
                    for k, ch in enumerate(grp):
                        fm_chunk(ws, 128 * k, ch[1], ch[2], ch[3], ch[4])

            def tm_group(c0, n, post, dst_fn, gi=None):
                ws, _ = load_w([(c0, n)])
                for t in range(NT):
                    s = it[0] % 2
                    it[0] += 1
                    cols = perm_cols(gi, t * 128, 128)
                    for c in range(DC):
                        kb.op("pe", [("wbf", ws), "hT"], [("p1", s)], lambda e, c=c, s=s, cols=cols: e.matmul(
                            p1[:, s, 0:n], lhsT=cols(c), rhs=wbf[:, ws, c, 0:n], start=(c == 0), stop=(c == DC - 1)),
                            inc=(c == DC - 1))
                    if post == "vaug":
                        nh = n // 64
                        kb.op("act", [("p1", s)], [("va", s)], lambda e, s=s, nh=nh: e.copy(
                            out=va[:, s, 0:nh, 0:64], in_=p1[:, s, 0:n].rearrange("p (h d) -> p h d", d=64)))
                        kb.dma("sp", dst_fn(t), va[:, s, 0:nh, :], reads=[("va", s)])
                    else:
                        if post == "sigf":
                            kb.op("act", [("p1", s)], [("otf", s)], lambda e, s=s: e.activation(
                                out=otf[:, s, 0:n], in_=p1[:, s, 0:n], func=AF.Sigmoid))
                        else:
                            kb.op("act", [("p1", s)], [("otf", s)], lambda e, s=s: e.copy(out=otf[:, s, 0:n], in_=p1[:, s, 0:n]))
                        kb.dma("sp", dst_fn(t), otf[:, s, 0:n], reads=[("otf", s)])

            chunks = []
            for c in range(4):
                chunks.append(([(o_aq + 128 * c, 128)], QA_d[c], "rms", 0, None))
            chunks.append(([(o_ak, 64), (o_ak, 64)], KA_d[0], "rms", 1, None))
            for c in range(2):
                chunks.append(([(o_iq + 128 * c, 128)], IQ_d[c], "plain", None, None))
            chunks.append(([(o_ik, 64), (o_ik, 64)], IK_d[0], "plain", None, None))
            for c in range(4):
                chunks.append(([(o_bq + 128 * c, 128)], QB_d[c], "rms", 2, None))
            for c in range(2):
                chunks.append(([(o_bkv + 128 * c, 128)], KCVC_d[c], "plain", None, None))
            for g in range(2):
                chunks.append(([(o_bkv + 256 + 64 * g, 64)] * 2, KS_d[g], "rms", 3, None))
            for g in range(2):
                chunks.append(([(o_bkv + 512 + 64 * g, 64)] * 2, KW_d[g], "rms", 3, None))
            for c in range(6):
                chunks.append(([(o_c + 128 * c, 128)], QC_d[c], "rms", 4, c // 2))
            for c in range(6):
                chunks.append(([(o_c + 768 + 128 * c, 128)], KCc_d[c], "rms", 5, c // 2))
            for c in range(GC):
                chunks.append(([(o_g + 128 * c, 128)], GT_d[c], "sigmoid", None, None))
            fm_group(chunks)
            tm_group(o_av, 64, "vaug", lambda t: VA_d[t * 128:(t + 1) * 128])
            tm_group(o_bkv + 384, 128, "vaug", lambda t: VS_d[t * 128:(t + 1) * 128])
            tm_group(o_bkv + 640, 128, "vaug", lambda t: VW_d[t * 128:(t + 1) * 128])
            for gi in range(3):
                tm_group(o_c + 1536 + 256 * gi, 256, "vaug", lambda t, gi=gi: VC_d[gi, t * 128:(t + 1) * 128], gi=gi)
            tm_group(o_iw, 4, "copyf", lambda t: IW_d[t * 128:(t + 1) * 128, :])
            tm_group(o_bg, 24, "sigf", lambda t: BG_d[t * 128:(t + 1) * 128, :])
            kb.barrier()

    def attn_tile(S_ps, s_slot, PT, pt_slot, O_ap_fn, bias_rhs, neg_lhsT, kq_list, v_rhs_fn, first, nk=128):
        sk = ("S_ps", s_slot)
        started = False
        if bias_rhs is not None:
            ap_, keys = bias_rhs
            kb.op("pe", ["identb"] + keys, [sk], lambda e: e.matmul(
                S_ps[0:nk, s_slot, :], lhsT=identb[0:nk, 0:nk], rhs=ap_, start=True, stop=False, skip_group_check=True), inc=False)
            started = True
        if neg_lhsT is not None:
            ap_, keys = neg_lhsT
            kb.op("pe", ["negI4"] + keys, [sk], lambda e: e.matmul(
                S_ps[0:nk, s_slot, :], lhsT=ap_, rhs=negI4[:].rearrange("p h f -> p (h f)"), start=not started, stop=False,
                skip_group_check=True), inc=False)
            started = True
        nq = len(kq_list)
        wq = 512 // nq
        for hh, (kT, qT, keys) in enumerate(kq_list):
            kb.op("pe", keys, [sk], lambda e, hh=hh, kT=kT, qT=qT, st_=(not started): e.matmul(
                S_ps[0:nk, s_slot, hh * wq:(hh + 1) * wq], lhsT=kT, rhs=qT, start=st_, stop=(hh == nq - 1),
                skip_group_check=True), inc=(hh == nq - 1))
            started = True
        perm = (0, 2, 1, 3) if nq == 2 else (0, 1, 2, 3)
        pk = ("PT", pt_slot)
        kb.op("act", [sk], [pk], lambda e: e.activation(
            out=PT[0:nk, pt_slot, :], in_=S_ps[0:nk, s_slot, :], func=AF.Exp, scale=0.125))

        def pv():
            for b_ in range(4):
                vap, oap, vkeys, okey, bfirst = v_rhs_fn(perm[b_])
                kb.op("pe", [pk] + vkeys, [okey], lambda e, b_=b_, vap=vap, oap=oap, bfirst=bfirst: e.matmul(
                    oap, lhsT=PT[0:nk, pt_slot, b_ * 128:(b_ + 1) * 128], rhs=vap, start=(first and bfirst), stop=False,
                    skip_group_check=True), inc=(b_ == 3))
        prev = pend[0]
        pend[0] = pv
        if prev is not None:
            prev()

    pend = [None]

    def attn_flush():
        if pend[0] is not None:
            p = pend[0]
            pend[0] = None
            p()

    def phase2A(layer):
        with ExitStack() as st:
            def sbl(name, shape, dt):
                return st.enter_context(sbt(name, list(shape), dt))
            QA = sbl("QA_s", [128, 4, S], BF16); KA = sbl("KA_s", [128, 2, S], BF16)
            IQ = sbl("IQ_s", [128, 2, S], BF16); IK = sbl("IK_s", [128, 2, S], BF16)
            VA = sbl("VA_s", [128, NT, 65], BF16); IW = sbl("IW_s", [128, NT, 4], F32)
            bA = sbl("bA_s", [128, ND, 8, 128], BF16)
            score = sbl("score", [128, 1, S], F32)
            negm = sbl("negm", [128, 2, S], BF16)
            junkb = sbl("junkb", [128, S], BF16)
            rt = sbl("rt", [128, 4, 512], F32)
            PT = sbl("PT", [128, 3, 512], BF16)
            cm = sbl("cm", [128, 128], F32)
            wv = sbl("wv", [128, 2, 16], F32)
            bis = sbl("bis", [128, 2, 8], F32)
            wk = sbl("wk", [128, 2, NBIS], F32)
            halves = sbl("halves", [128, NBIS], F32)
            rden = sbl("rden", [128, 2, 8], F32)
            oa = sbl("oa", [128, 2, 512], BF16)
            P_ps = st.enter_context(pst_("P_ps", [128, 2, 512], F32))
            S_ps = st.enter_context(pst_("S_ps", [128, 2, 512], F32))
            O_ps = st.enter_context(pst_("O_ps", [128, 2, 512], F32))

            kb.dma("sp", QA[:], QA_d.rearrange("c p s -> p c s"), writes=["QA"])
            kb.op("pool", [], ["KA"], lambda e: e.memset(KA[:], 0.0))
            kb.op("pool", [], ["IK"], lambda e: e.memset(IK[:], 0.0))
            for v in range(2):
                kb.dma("sp", KA[64 * v:64 * v + 64, v, :], KA_d[0, 64 * v:64 * v + 64, :], writes=["KA"], key=("KAl", v))
                kb.dma("sp", IK[64 * v:64 * v + 64, v, :], IK_d[0, 64 * v:64 * v + 64, :], writes=["IK"], key=("IKl", v))
            kb.dma("sp", IQ[:], IQ_d.rearrange("c p s -> p c s"), writes=["IQ"])
            kb.dma("sp", VA[:], VA_d.rearrange("(t p) o c -> p t (o c)", p=128), writes=["VA"])
            kb.dma("sp", IW[:], IW_d.rearrange("(t p) c -> p t c", p=128), writes=["IW"])
            kb.dma("sp", bA[:], bA_d, writes=["bA"])
            kb.op("pool", [], ["cm"], lambda e: e.memset(cm[:], 0.0))
            kb.op("pool", ["cm"], ["cm"], lambda e: e.affine_select(
                out=cm[:], in_=cm[:], pattern=[[-1, 128]], compare_op=ALU.is_ge, fill=reg_negbigf, base=0, channel_multiplier=1))
            for k in range(NBIS):
                kb.op("dve", [], ["halves"], lambda e, k=k: e.memset(halves[:, k:k + 1], 2.0 ** -(k + 1)))

            def idx_bis(i):
                L = 128 * (i + 1)
                s = i % 2
                sc = ("score", 0)
                qs = slice(i * 128, (i + 1) * 128)
                kb.op("act", ["IW"], [("wv", s)], lambda e, s=s, i=i: e.activation(
                    out=wv[:, s, 0:4], in_=IW[:, i, :], func=AF.Abs))
                kb.op("act", ["IW"], [("wv", s)], lambda e, s=s, i=i: e.activation(
                    out=wv[:, s, 4:8], in_=IW[:, i, :], func=AF.Sign))
                nkb = (L + 511) // 512
                for kbk in range(nkb):
                    k0 = kbk * 512
                    w = min(512, L - k0)
                    for h in range(4):
                        ps_ = (kbk * 4 + h) % 2
                        r4 = (kbk * 4 + h) % 4
                        kb.op("pe", ["IQ", "IK"], [("P_ps", ps_)], lambda e, h=h, ps_=ps_, k0=k0, w=w: e.matmul(
                            P_ps[:, ps_, 0:w], lhsT=IQ[:, h // 2, qs], rhs=IK[:, h % 2, k0:k0 + w], start=True, stop=True))
                        kb.op("act", [("P_ps", ps_), ("wv", s)], [("rt", r4)], lambda e, h=h, ps_=ps_, r4=r4, w=w, s=s: e.activation(
                            out=rt[:, r4, 0:w], in_=P_ps[:, ps_, 0:w], func=AF.Relu, scale=wv[:, s, h:h + 1]))
                        if h == 0:
                            kb.op("dve", [("rt", r4), ("wv", s)], [sc], lambda e, r4=r4, w=w, s=s, k0=k0: e.tensor_scalar(
                                out=score[:, 0, k0:k0 + w], in0=rt[:, r4, 0:w], scalar1=wv[:, s, 4:5], scalar2=None, op0=ALU.mult))
                        else:
                            kb.op("dve", [("rt", r4), ("wv", s), sc], [sc], lambda e, r4=r4, w=w, s=s, k0=k0, h=h: e.scalar_tensor_tensor(
                                out=score[:, 0, k0:k0 + w], in0=rt[:, r4, 0:w], scalar=wv[:, s, 4 + h:5 + h],
                                in1=score[:, 0, k0:k0 + w], op0=ALU.mult, op1=ALU.add))
                bk_ = ("bis", s)
                kb.op("dve", [sc], [bk_], lambda e, s=s, L=L: e.tensor_reduce(
                    out=bis[:, s, 0:1], in_=score[:, 0, 0:L], axis=AX.X, op=ALU.max, apply_absolute_value=True))
                kb.op("dve", [sc, "cm"], [sc], lambda e, s=s, i=i: e.tensor_tensor(
                    out=score[:, 0, qs], in0=score[:, 0, qs], in1=cm[:], op=ALU.add))
                kb.op("dve", [bk_], [bk_], lambda e, s=s: e.tensor_scalar(
                    out=bis[:, s, 1:2], in0=bis[:, s, 0:1], scalar1=2.002, scalar2=2e-6, op0=ALU.mult, op1=ALU.add))
                kb.op("dve", [bk_, "halves"], [("wk", s)], lambda e, s=s: e.tensor_scalar(
                    out=wk[:, s, :], in0=halves[:], scalar1=bis[:, s, 1:2], scalar2=None, op0=ALU.mult))
                kb.op("dve", [], [bk_], lambda e, s=s: e.memset(bis[:, s, 2:3], 0.0))
                for k in range(NBIS):
                    kb.op("dve", [sc, bk_], ["junkb", bk_], lambda e, s=s, L=L: e.tensor_scalar(
                        out=junkb[:, 0:L], in0=score[:, 0, 0:L], scalar1=bis[:, s, 2:3], scalar2=None,
                        op0=ALU.is_gt, op1=ALU.add, accum_out=bis[:, s, 3:4]))
                    kb.op("dve", [bk_], [bk_], lambda e, s=s: e.tensor_scalar(
                        out=bis[:, s, 4:5], in0=bis[:, s, 3:4], scalar1=float(TOPK), scalar2=0.5, op0=ALU.is_ge, op1=ALU.subtract))
                    kb.op("dve", [bk_, ("wk", s)], [bk_], lambda e, s=s, k=k: e.scalar_tensor_tensor(
                        out=bis[:, s, 2:3], in0=bis[:, s, 4:5], scalar=wk[:, s, k:k + 1], in1=bis[:, s, 2:3],
                        op0=ALU.mult, op1=ALU.add))
                nk_ = ("negm", s)
                kb.op("dve", [sc, bk_], [nk_], lambda e, s=s, L=L: e.tensor_scalar(
                    out=negm[:, s, 0:L], in0=score[:, 0, 0:L], scalar1=bis[:, s, 2:3], scalar2=None, op0=ALU.is_le))
            def att(i):
                L = 128 * (i + 1)
                s = i % 2
                qs = slice(i * 128, (i + 1) * 128)
                nk_ = ("negm", s)
                for j in range(i + 1):
                    d = min(i - j, ND - 1)
                    ks_ = slice(j * 128, (j + 1) * 128)
                    for hb in range(2):
                        ss_ = (i * 64 + j * 2 + hb) % 2
                        p3 = (i * 64 + j * 2 + hb) % 3
                        kq = []
                        for v in range(2):
                            kq.append((KA[:, v, ks_], QA[:, 2 * hb:2 * hb + 2, qs], ["KA", "QA"]))
                        attn_tile(S_ps, ss_, PT, p3,
                                  None,
                                  (bA[:, d, hb * 4:hb * 4 + 4, :].rearrange("p (c v) f -> p v c f", v=2), ["bA"]),
                                  (negm[:, s, ks_], [nk_]),
                                  kq,
                                  lambda hh, hb=hb, j=j: (VA[:, j, :], O_ps[:, hb, hh * 65:(hh + 1) * 65], ["VA"], ("O_ps", hb), hh == 0),
                                  first=(j == 0))
                attn_flush()
                okeys = [("O_ps", 0), ("O_ps", 1)]
                Ov = O_ps[:, :, 0:260].rearrange("p b (h c) -> p b h c", c=65)
                kb.op("dve", okeys, [("rden", s)], lambda e, s=s, Ov=Ov: e.reciprocal(
                    out=rden[:, s, :].rearrange("p (b h) -> p b h", b=2), in_=Ov[:, :, :, 64]))
                for hb in range(2):
                    kb.op("dve", [("O_ps", hb), ("rden", s)], [("oa", s)], lambda e, s=s, hb=hb, Ov=Ov: e.tensor_tensor(
                        out=oa[:, s, hb * 256:(hb + 1) * 256].rearrange("p (h c) -> p h c", c=64),
                        in0=Ov[:, hb, :, 0:64], in1=rden[:, s, hb * 4:hb * 4 + 4].unsqueeze(2).to_broadcast([128, 4, 64]),
                        op=ALU.mult))
                kb.dma("sp", AT_d[i * 128:(i + 1) * 128, 0:512], oa[:, s, :], reads=[("oa", s)])

            idx_bis(0)
            for i in range(NT):
                if i + 1 < NT:
                    idx_bis(i + 1)
                att(i)
            kb.barrier()


    def phase2B(layer):
        with ExitStack() as st:
            def sbl(name, shape, dt):
                return st.enter_context(sbt(name, list(shape), dt))
            NCP = NCC * 128
            QB = sbl("QB_s", [128, 2, S], BF16)
            KS = sbl("KS_s", [128, 2, S], BF16); KW = sbl("KW_s", [128, 2, S], BF16)
            VS = sbl("VS_s", [128, NT, 65], BF16); VW = sbl("VW_s", [128, NT, 65], BF16)
            BGs = sbl("BG_s", [128, NT, 8, 3], F32)
            bBs = sbl("bBs_s", [128, ND, 4, 128], BF16); bBw = sbl("bBw_s", [128, 5, 4, 128], BF16)
            kcT = sbl("kcT", [128, 2, 2, NCP], BF16)
            CVX = sbl("CVX", [128, 2, NCC, 129], BF16)
            Vw = sbl("Vw", [128, 2 * NSEL], F32); Fw = sbl("Fw", [128, 2 * NSEL], F32)
            zt = sbl("zt", [128, 4, 128], BF16)
            mct = sbl("mct", [128, 2, 4, 128], BF16)
            PT = sbl("PTb", [128, 3, 512], BF16)
            negmB = sbl("negmB", [128, 2, S], BF16)
            sm = sbl("smB", [128, 2, 32], F32)
            imp = sbl("imp", [128, 2, 64 * 4], F32)
            m8 = sbl("m8", [128, 2, 16], F32)
            t1 = sbl("t1", [128, 2, 256], F32); t2 = sbl("t2", [128, 2, 256], F32)
            ob = sbl("obB", [128, 2, 256], BF16)

            kb.dma("sp", BGs[:], BG_d.rearrange("(t p) (h b) -> p t h b", p=128, b=3), writes=["BGs"])
            kb.op("pool", [], ["KS"], lambda e: e.memset(KS[:], 0.0))
            kb.op("pool", [], ["KW"], lambda e: e.memset(KW[:], 0.0))
            kb.op("pool", [], ["zt"], lambda e: e.memset(zt[:], 0.0))
            kb.op("dve", [], ["Vw"], lambda e: e.memset(Vw[:], 0.0))
            kb.op("dve", [], ["Fw"], lambda e: e.memset(Fw[:], 0.0))
            for b in range(2):
                rows = slice(64 * b, 64 * b + 64)
                kb.op("dve", [], ["Vw"], lambda e, rows=rows, b=b: e.memset(Vw[rows, 0:NSEL + b - 1], 1.0))
                kb.op("dve", [], ["Fw"], lambda e, rows=rows, b=b: e.memset(Fw[rows, NSEL + b - 1:NSEL + b + 1], BIGF))
                kb.op("dve", [], ["Fw"], lambda e, rows=rows, b=b: e.memset(Fw[rows, NSEL + b + 1:2 * NSEL], -BIGF))

            with ExitStack() as st2:
                def sb2(name, shape, dt):
                    return st2.enter_context(sbt(name, list(shape), dt))
                KCVC = sb2("KCVC_s", [128, 2, S], BF16)
                wc32 = sb2("wc32", [128, 32, 64], F32)
                wc = sb2("wc", [128, 2, 2, 32, 64], BF16)
                pos32 = sb2("pos32", [128, 2, 32], F32)
                posB = sb2("posB", [128, 2, 32, 128], BF16)
                Gk = sb2("Gk", [128, 64], F32)
                ktm = sb2("ktm", [128, 2, 128], BF16)
                ov = sb2("ov", [128, NCC, 64], F32)
                cs = sb2("cs", [128, 2, 4], F32)
                cj = sb2("cj", [128, 64], F32)
                pc = st2.enter_context(pst_("pc", [128, 2, 512], F32))
                ptr = st2.enter_context(pst_("ptr", [128, 2, 1024], BF16))
                kb.dma("sp", KCVC[:], KCVC_d.rearrange("c p s -> p c s"), writes=["KCVC"])
                kb.dma("sp", Gk[:], qk_d[layer, 3:4, :].to_broadcast([128, 64]), writes=["Gk"])
                for half in range(2):
                    kb.dma("sp", pos32[64 * half:64 * half + 64], cpos_d[layer].rearrange("k l d -> d k l"),
                           writes=["pos32"], key=("pos32", half), allow_slow_non_contiguous=True)
                kb.op("pool", [], ["wc"], lambda e: e.memset(wc[:], 0.0))
                for kv in range(2):
                    for half in range(2):
                        kb.dma("sp", wc32[64 * half:64 * half + 64], cw_d[layer, kv].rearrange("l d e -> d l e"),
                               writes=["wc32"], key=("wc32", half))
                    for half in range(2):
                        kb.op("dve", ["wc32"], ["wc"], lambda e, kv=kv, half=half: e.tensor_copy(
                            out=wc[64 * half:64 * half + 64, half, kv], in_=wc32[64 * half:64 * half + 64]))
                kb.op("dve", ["pos32"], ["posB"], lambda e: e.tensor_copy(
                    out=posB[:].rearrange("p k l n -> p (k l) n"),
                    in_=pos32[:].rearrange("p k l -> p (k l)").unsqueeze(2).to_broadcast([128, 64, 128])))
                kb.op("pool", [], ["ov"], lambda e: e.memset(ov[:], 1.0))
                for c in range(NCC):
                    kb.op("pool", ["ov"], ["ov"], lambda e, c=c: e.affine_select(
                        out=ov[:, c, :], in_=ov[:, c, :], pattern=[[64, 64]], compare_op=ALU.is_gt, fill=reg_zero,
                        base=64 - 16 * 128 * c, channel_multiplier=-16))
                    kb.op("pool", ["ov"], ["ov"], lambda e, c=c: e.affine_select(
                        out=ov[:, c, :], in_=ov[:, c, :], pattern=[[-64, 64]], compare_op=ALU.is_gt, fill=reg_zero,
                        base=16 * 128 * c + 32, channel_multiplier=16))
                kb.op("dve", [], ["CVX"], lambda e: e.memset(CVX[:], 1.0))
                for g in range(2):
                    kb.op("dve", ["ov", "CVX"], ["CVX"], lambda e, g=g: e.tensor_copy(out=CVX[:, g, :, 65:129], in_=ov[:]))
                kb.op("dve", [], ["kcT"], lambda e: e.memset(kcT[:], 0.0))
                n_it = 0
                for kv in range(2):
                    for g in range(2):
                        rows = slice(64 * g, 64 * g + 64)
                        for c in range(NCC):
                            nv = min(128, NCMP - 128 * c)
                            s = n_it % 2
                            n_it += 1
                            for l in range(32):
                                t0 = 16 * 128 * c + l
                                kb.op("pe", ["KCVC", "wc"], [("pc", s)], lambda e, l=l, t0=t0, nv=nv, s=s, g=g, kv=kv: e.matmul(
                                    pc[0:nv, s, 0:64], lhsT=KCVC[:, kv, t0:t0 + 16 * (nv - 1) + 1:16], rhs=wc[:, g, kv, l, :],
                                    start=(l == 0), stop=False), inc=False)
                            for l in range(32):
                                kb.op("pe", ["posB", "wc"], [("pc", s)], lambda e, l=l, nv=nv, s=s, g=g, kv=kv: e.matmul(
                                    pc[0:nv, s, 0:64], lhsT=posB[:, kv, l, 0:nv], rhs=wc[:, g, kv, l, :],
                                    start=False, stop=(l == 31)), inc=(l == 31))
                            if kv == 1:
                                kb.op("act", [("pc", s)], ["CVX"], lambda e, nv=nv, s=s, g=g, c=c: e.copy(
                                    out=CVX[0:nv, g, c, 0:64], in_=pc[0:nv, s, 0:64]))
                            else:
                                kb.op("act", [("pc", s)], ["cj", ("cs", s)], lambda e, nv=nv, s=s: e.activation(
                                    out=cj[0:nv, :], in_=pc[0:nv, s, 0:64], func=AF.Square, accum_out=cs[0:nv, s, 0:1]))
                                kb.op("act", [("cs", s)], [("cs", s)], lambda e, nv=nv, s=s: e.activation(
                                    out=cs[0:nv, s, 1:2], in_=cs[0:nv, s, 0:1], func=AF.Sqrt, bias=epsc[0:nv, 0:1], scale=1.0 / 64))
                                kb.op("dve", [("cs", s)], [("cs", s)], lambda e, nv=nv, s=s: e.reciprocal(
                                    out=cs[0:nv, s, 2:3], in_=cs[0:nv, s, 1:2]))
                                kb.op("dve", [], [("ktm", s)], lambda e, s=s: e.memset(ktm[:, s, :], 0.0))
                                for dup in range(2):
                                    kb.op("dve", [("pc", s), ("cs", s), "Gk"], [("ktm", s)], lambda e, nv=nv, s=s, dup=dup: e.scalar_tensor_tensor(
                                        out=ktm[0:nv, s, 64 * dup:64 * dup + 64], in0=pc[0:nv, s, 0:64], scalar=cs[0:nv, s, 2:3],
                                        in1=Gk[0:nv, :], op0=ALU.mult, op1=ALU.mult))
                                kb.op("pe", [("ktm", s), "identb"], [("ptr", s)], lambda e, s=s: e.transpose(
                                    out=ptr[:, s, 0:128], in_=ktm[:, s, :], identity=identb[:]))
                                for v in range(2):
                                    kb.op("act", [("ptr", s)], ["kcT"], lambda e, s=s, g=g, c=c, v=v: e.copy(
                                        out=kcT[64 * v:64 * v + 64, g, v, 128 * c:128 * c + 128], in_=ptr[64 * v:64 * v + 64, s, 0:128]))
                kb.barrier()

            S_ps = st.enter_context(pst_("S_psB", [128, 2, 512], F32))
            OCU = st.enter_context(pst_("OCU", [128, 2, 512], F32))
            OS = st.enter_context(pst_("OS", [128, 512], F32))
            OW = st.enter_context(pst_("OW", [128, 512], F32))
            cnt = [0]

            def slots():
                cnt[0] += 1
                return cnt[0] % 2, cnt[0] % 3

            for g in range(2):
                kb.dma("sp", QB[:], QB_d[2 * g:2 * g + 2].rearrange("c p s -> p c s"), writes=["QB"])
                for v in range(2):
                    kb.dma("sp", KS[64 * v:64 * v + 64, v, :], KS_d[g, 64 * v:64 * v + 64, :], writes=["KS"], key=("KSl", v))
                    kb.dma("sp", KW[64 * v:64 * v + 64, v, :], KW_d[g, 64 * v:64 * v + 64, :], writes=["KW"], key=("KWl", v))
                kb.dma("sp", VS[:], VS_d[:, g, :].rearrange("(t p) c -> p t c", p=128), writes=["VS"])
                kb.dma("sp", VW[:], VW_d[:, g, :].rearrange("(t p) c -> p t c", p=128), writes=["VW"])
                kb.dma("sp", bBs[:], bBs_d[:, :, 4 * g:4 * g + 4, :], writes=["bBs"])
                kb.dma("sp", bBw[:], bBw_d[:, :, 4 * g:4 * g + 4, :], writes=["bBw"])
                for i in range(NT):
                    L = 128 * (i + 1)
                    qs = slice(i * 128, (i + 1) * 128)
                    so = i % 2
                    sg = i % 2
                    def kq_for(Ksrc, cols, nk=128, g=g):
                        out = []
                        for v in range(2):
                            out.append((Ksrc(v, cols), QB[:, 0:2, qs], ["QB", "KS", "KW", "kcT"]))
                        return out
                    cmax = min(NCC - 1, (128 * i + 96) // 2048)
                    for c in range(cmax + 1):
                        nv = min(128, NCMP - 128 * c)
                        o = 128 * i - 2048 * c - 31
                        s2, s3 = slots()
                        bias = None
                        if o < 16 * 127:
                            kb.op("pool", ["zt"], [("mct", s2)], lambda e, s2=s2, o=o: e.affine_select(
                                out=mct[:, s2], in_=zt[:], pattern=[[0, 4], [1, 128]], compare_op=ALU.is_ge, fill=reg_negbig,
                                base=o, channel_multiplier=-16))
                            bias = (mct[0:nv, s2].rearrange("p h f -> p (h f)"), [("mct", s2)])
                        attn_tile(S_ps, s2, PT, s3, None, bias, None,
                                  kq_for(lambda v, cols, g=g: kcT[:, g, v, cols], slice(128 * c, 128 * c + nv)),
                                  lambda hh, g=g, c=c, nv=nv: (CVX[0:nv, g, c, :], OCU[:, hh // 2, (hh % 2) * 129:(hh % 2) * 129 + 129],
                                                               ["CVX"], ("OCU", hh // 2), hh % 2 == 0),
                                  first=(c == 0), nk=nv)
                    attn_flush()
                    ock = [("OCU", 0), ("OCU", 1)]
                    OCv = OCU[:, :, 0:258].rearrange("p b (h c) -> p b h c", c=129)
                    smk = ("sm", sg)
                    kb.op("dve", ock, [smk], lambda e, sg=sg, OCv=OCv: e.tensor_scalar(
                        out=sm[:, sg, 0:4].rearrange("p (b h) -> p b h", b=2), in0=OCv[:, :, :, 64], scalar1=1e-30, scalar2=None, op0=ALU.max))
                    kb.op("dve", [smk], [smk], lambda e, sg=sg: e.reciprocal(out=sm[:, sg, 0:4], in_=sm[:, sg, 0:4]))
                    ik_ = ("imp", sg)
                    for hh in range(4):
                        Uh = OCU[:, hh // 2, (hh % 2) * 129 + 65:(hh % 2) * 129 + 65 + NSEL]
                        if hh == 0:
                            kb.op("dve", ock + [smk], [ik_], lambda e, sg=sg, Uh=Uh: e.tensor_scalar(
                                out=imp[:, sg, 0:NSEL], in0=Uh, scalar1=sm[:, sg, 0:1], scalar2=None, op0=ALU.mult))
                        else:
                            kb.op("dve", ock + [smk, ik_], [ik_], lambda e, sg=sg, Uh=Uh, hh=hh: e.scalar_tensor_tensor(
                                out=imp[:, sg, 0:NSEL], in0=Uh, scalar=sm[:, sg, hh:hh + 1], in1=imp[:, sg, 0:NSEL],
                                op0=ALU.mult, op1=ALU.add))
                    x0 = NSEL - 2 * i
                    kb.op("dve", [ik_, "Vw"], [ik_], lambda e, sg=sg, x0=x0: e.tensor_tensor(
                        out=imp[:, sg, 64:64 + NSEL], in0=imp[:, sg, 0:NSEL], in1=Vw[:, x0:x0 + NSEL], op=ALU.mult))
                    kb.op("dve", [ik_, "Fw"], [ik_], lambda e, sg=sg, x0=x0: e.tensor_tensor(
                        out=imp[:, sg, 64:64 + NSEL], in0=imp[:, sg, 64:64 + NSEL], in1=Fw[:, x0:x0 + NSEL], op=ALU.add))
                    kb.op("dve", [ik_], [ik_], lambda e, sg=sg: e.memset(imp[:, sg, 64:65], BIGF))
                    mk = ("m8", sg)
                    kb.op("dve", [ik_], [mk], lambda e, sg=sg: e.max(out=m8[:, sg, 0:8], in_=imp[:, sg, 64:64 + NSEL]))
                    kb.op("dve", [ik_, mk], [ik_], lambda e, sg=sg: e.match_replace(
                        out=imp[:, sg, 128:128 + NSEL], in_to_replace=m8[:, sg, 0:8], in_values=imp[:, sg, 64:64 + NSEL], imm_value=-3.0e38))
                    kb.op("dve", [ik_], [mk], lambda e, sg=sg: e.max(out=m8[:, sg, 8:16], in_=imp[:, sg, 128:128 + NSEL]))
                    kb.op("dve", [ik_, mk], [ik_], lambda e, sg=sg: e.tensor_scalar(
                        out=imp[:, sg, 192:192 + NSEL], in0=imp[:, sg, 64:64 + NSEL], scalar1=m8[:, sg, 15:16], scalar2=None, op0=ALU.is_lt))
                    nbk = ("negmB", sg)
                    nb_ = 2 * (i + 1)
                    kb.op("dve", [ik_], [nbk], lambda e, sg=sg, nb_=nb_, L=L: e.tensor_copy(
                        out=negmB[:, sg, 0:L].rearrange("p (m k) -> p m k", k=64),
                        in_=imp[:, sg, 192:192 + nb_].unsqueeze(2).to_broadcast([128, nb_, 64])))
                    for j in range(i + 1):
                        d = min(i - j, ND - 1)
                        ks_ = slice(j * 128, (j + 1) * 128)
                        s2, s3 = slots()
                        attn_tile(S_ps, s2, PT, s3, None,
                                  (bBs[:, d].rearrange("p (c v) f -> p v c f", v=2), ["bBs"]),
                                  (negmB[:, sg, ks_], [nbk]),
                                  kq_for(lambda v, cols: KS[:, v, cols], ks_),
                                  lambda hh, j=j: (VS[:, j, :], OS[:, hh * 65:(hh + 1) * 65], ["VS"], "OS", hh == 0),
                                  first=(j == 0))
                    j0 = max(0, i - 4)
                    for j in range(j0, i + 1):
                        ks_ = slice(j * 128, (j + 1) * 128)
                        s2, s3 = slots()
                        attn_tile(S_ps, s2, PT, s3, None,
                                  (bBw[:, i - j].rearrange("p (c v) f -> p v c f", v=2), ["bBw"]),
                                  None,
                                  kq_for(lambda v, cols: KW[:, v, cols], ks_),
                                  lambda hh, j=j: (VW[:, j, :], OW[:, hh * 65:(hh + 1) * 65], ["VW"], "OW", hh == 0),
                                  first=(j == j0))
                    attn_flush()
                    OSv = OS[:, 0:260].rearrange("p (h c) -> p h c", c=65)
                    OWv = OW[:, 0:260].rearrange("p (h c) -> p h c", c=65)
                    kb.op("dve", ["OS"], [smk], lambda e, sg=sg, OSv=OSv: e.reciprocal(out=sm[:, sg, 4:8], in_=OSv[:, :, 64]))
                    kb.op("dve", ["OW"], [smk], lambda e, sg=sg, OWv=OWv: e.reciprocal(out=sm[:, sg, 8:12], in_=OWv[:, :, 64]))
                    for br in range(3):
                        kb.op("dve", [smk, "BGs"], [smk], lambda e, sg=sg, br=br, g=g, i=i: e.tensor_tensor(
                            out=sm[:, sg, 12 + 4 * br:16 + 4 * br], in0=sm[:, sg, 4 * br:4 * br + 4], in1=BGs[:, i, 4 * g:4 * g + 4, br], op=ALU.mult))
                    def cf(br, sg=sg):
                        return sm[:, sg, 12 + 4 * br:16 + 4 * br].unsqueeze(2).to_broadcast([128, 4, 64])
                    t1v = t1[:, sg, :].rearrange("p (h c) -> p h c", c=64)
                    t2v = t2[:, sg, :].rearrange("p (h c) -> p h c", c=64)
                    kb.op("dve", ock + [smk], [("t1", sg)], lambda e, t1v=t1v, OCv=OCv, cf=cf: e.tensor_tensor(
                        out=t1v.rearrange("p (b h) c -> p b h c", b=2), in0=OCv[:, :, :, 0:64],
                        in1=cf(0).rearrange("p (b h) c -> p b h c", b=2), op=ALU.mult))
                    kb.op("dve", ["OS", smk], [("t2", sg)], lambda e, t2v=t2v, OSv=OSv, cf=cf: e.tensor_tensor(
                        out=t2v, in0=OSv[:, :, 0:64], in1=cf(1), op=ALU.mult))
                    kb.op("dve", [("t1", sg), ("t2", sg)], [("t1", sg)], lambda e, sg=sg: e.tensor_tensor(
                        out=t1[:, sg, :], in0=t1[:, sg, :], in1=t2[:, sg, :], op=ALU.add))
                    kb.op("dve", ["OW", smk], [("t2", sg)], lambda e, t2v=t2v, OWv=OWv, cf=cf: e.tensor_tensor(
                        out=t2v, in0=OWv[:, :, 0:64], in1=cf(2), op=ALU.mult))
                    kb.op("dve", [("t1", sg), ("t2", sg)], [("ob", so)], lambda e, sg=sg, so=so, g=g: e.tensor_tensor(
                        out=ob[:, so, :], in0=t1[:, sg, :], in1=t2[:, sg, :], op=ALU.add))
                    kb.dma("sp", AT_d[i * 128:(i + 1) * 128, 512 + 256 * g:768 + 256 * g], ob[:, so, :], reads=[("ob", so)])
            kb.barrier()

    def phase2C(layer):
        with ExitStack() as st:
            def sbl(name, shape, dt):
                return st.enter_context(sbt(name, list(shape), dt))
            QC = sbl("QC_s", [128, 2, S], BF16); KC = sbl("KC_s", [128, 2, 2, S], BF16)
            VC = sbl("VC_s", [128, NT, 4, 65], BF16)
            bC = sbl("bC_s", [128, 3, 2, 4, 128], BF16)
            PT = sbl("PTc", [128, 3, 512], BF16)
            osb = sbl("osb", [128, 2, 260], F32)
            S_ps = st.enter_context(pst_("S_psC", [128, 2, 512], F32))
            O_ps = st.enter_context(pst_("O_psC", [128, 2, 512], F32))
            kb.dma("sp", bC[:], bC_d, writes=["bC"])
            kb.op("pool", [], ["KC"], lambda e: e.memset(KC[:], 0.0))
            n = 0
            for gi, dil in enumerate((1, 4, 16)):
                U = S // dil
                UT = U // 128
                kb.dma("sp", QC[:], QC_d[2 * gi:2 * gi + 2].rearrange("c p s -> p c s"), writes=["QC"])
                for v in range(2):
                    kb.dma("sp", KC[64 * v:64 * v + 64, :, v, :], KCc_d[2 * gi:2 * gi + 2, 64 * v:64 * v + 64, :].rearrange("c p s -> p c s"),
                           writes=["KC"], key=("KCl", v))
                kb.dma("sp", VC[:], VC_d[gi].rearrange("(t p) h c -> p t h c", p=128), writes=["VC"])
                ocv = OC_d[gi].rearrange("(u r) h c -> r u (h c)", r=dil)
                for pt in range(NT):
                    r, ui = pt // UT, pt % UT
                    so = pt % 2
                    qs = slice(pt * 128, (pt + 1) * 128)
                    first = True
                    for dj in (1, 0):
                        if ui - dj < 0:
                            continue
                        kt = pt - dj
                        ks_ = slice(kt * 128, (kt + 1) * 128)
                        n += 1
                        kq = []
                        for hh in range(4):
                            kq.append((KC[:, hh // 2, hh % 2, ks_], QC[:, hh // 2, qs], ["KC", "QC"]))
                        attn_tile(S_ps, n % 2, PT, n % 3, None,
                                  (bC[:, gi, dj].rearrange("p h f -> p (h f)"), ["bC"]), None, kq,
                                  lambda hh, kt=kt, so=so: (VC[:, kt, hh, :], O_ps[:, so, hh * 65:(hh + 1) * 65], ["VC"], ("O_psC", so), hh == 0),
                                  first=first)
                        first = False
                    attn_flush()
                    kb.op("act", [("O_psC", so)], [("osb", so)], lambda e, so=so: e.copy(out=osb[:, so, :], in_=O_ps[:, so, 0:260]))
                    kb.dma("sp", ocv[r, ui * 128:(ui + 1) * 128, :], osb[:, so, :], reads=[("osb", so)])
            kb.barrier()
            oc3 = sbl("oc3", [128, 2, 3, 260], F32)
            rd = sbl("rdC", [128, 2, 4], F32)
            oc = sbl("ocC", [128, 2, 256], BF16)
            for tt in range(NT):
                s = tt % 2
                kb.dma("sp", oc3[:, s], OC_d[:, tt * 128:(tt + 1) * 128].rearrange("g p h c -> p g (h c)"), writes=[("oc3", s)])
                kb.op("dve", [("oc3", s)], [("oc3", s)], lambda e, s=s: e.tensor_tensor(
                    out=oc3[:, s, 0], in0=oc3[:, s, 0], in1=oc3[:, s, 1], op=ALU.add))
                kb.op("dve", [("oc3", s)], [("oc3", s)], lambda e, s=s: e.tensor_tensor(
                    out=oc3[:, s, 0], in0=oc3[:, s, 0], in1=oc3[:, s, 2], op=ALU.add))
                v0 = oc3[:, s, 0].rearrange("p (h c) -> p h c", c=65)
                kb.op("dve", [("oc3", s)], [("rdC", s)], lambda e, s=s, v0=v0: e.reciprocal(out=rd[:, s, :], in_=v0[:, :, 64]))
                kb.op("dve", [("oc3", s), ("rdC", s)], [("ocC", s)], lambda e, s=s, v0=v0: e.tensor_tensor(
                    out=oc[:, s, :].rearrange("p (h c) -> p h c", c=64), in0=v0[:, :, 0:64],
                    in1=rd[:, s, :].unsqueeze(2).to_broadcast([128, 4, 64]), op=ALU.mult))
                kb.dma("sp", AT_d[tt * 128:(tt + 1) * 128, 1024:1280], oc[:, s, :], reads=[("ocC", s)])
            kb.barrier()


    def load_cast(st_pool, dst, src_ap, key, nparts=128):
        stg, = st_pool
        shp = dst.shape
        A, Bn = shp[1], shp[2]
        per = max(1, 2048 // Bn)
        for a0 in range(0, A, per):
            a1 = min(A, a0 + per)
            sl = getattr(load_cast, "n", 0) % 2
            load_cast.n = getattr(load_cast, "n", 0) + 1
            kb.dma("sp", stg[0:nparts, sl, 0:(a1 - a0) * Bn].rearrange("p (a b) -> p a b", b=Bn), src_ap[:, a0:a1, :],
                   writes=[("stg", sl)])
            kb.op("pool", [("stg", sl)], [key], lambda e, a0=a0, a1=a1, sl=sl: e.tensor_copy(
                out=dst[0:nparts, a0:a1, :], in_=stg[0:nparts, sl, 0:(a1 - a0) * Bn].rearrange("p (a b) -> p a b", b=Bn)))

    def phase3(layer, xin_d):
        with ExitStack() as st:
            def sbl(name, shape, dt):
                return st.enter_context(sbt(name, list(shape), dt))
            stg = sbl("stg3", [128, 2, 2048], F32)
            Wb = sbl("Wb", [128, 10, D], BF16)
            Wo = sbl("Wo", [128, DC, D], BF16)
            att = sbl("att", [128, 2, 1280], BF16)
            attT = sbl("attT", [128, 10, 512], BF16)
            gt = sbl("gt", [128, GC, 512], BF16)
            m1 = sbl("m1", [128, 2, 512], F32); m2 = sbl("m2", [128, 2, 512], F32)
            mg = sbl("mg", [128, DC, 512], BF16)
            xt = sbl("xt3", [128, 2, D], F32)
            tpa = st.enter_context(pst_("tpa", [128, 2, 8, 128], BF16))
            yp = st.enter_context(pst_("yp", [128, 3, 512], F32))
            op_ = st.enter_context(pst_("op3", [128, 2, 512], F32))
            load_cast((stg,), Wb[:, 0:4, :], wba_d[layer].rearrange("(c p) n -> p c n", p=128), "Wb")
            load_cast((stg,), Wb[:, 4:8, :], wbb_d[layer].rearrange("(c p) n -> p c n", p=128), "Wb")
            load_cast((stg,), Wb[:, 8:10, :], wbc_d[layer].rearrange("(c p) n -> p c n", p=128), "Wb")
            load_cast((stg,), Wo[:], wout_d[layer].rearrange("(c p) n -> p c n", p=128), "Wo")
            nt4 = 0
            for tb in range(S // 512):
                ts_ = slice(tb * 512, (tb + 1) * 512)
                kb.dma("act", gt[:], GT_d[:, :, ts_].rearrange("c p s -> p c s"), writes=["gt"])
                for t4 in range(4):
                    tt = tb * 4 + t4
                    s = tt % 2
                    kb.dma("act", att[:, s, :], AT_d[tt * 128:(tt + 1) * 128, :], writes=[("att", s)])
                    for hf in range(2):
                        for c in range(5):
                            kb.op("pe", [("att", s), "identb"], [("tpa", hf)], lambda e, s=s, c=c, hf=hf: e.transpose(
                                out=tpa[:, hf, c, :], in_=att[:, s, (hf * 5 + c) * 128:(hf * 5 + c + 1) * 128], identity=identb[:]),
                                inc=(c == 4))
                        kb.op("act", [("tpa", hf)], ["attT"], lambda e, hf=hf, t4=t4: e.copy(
                            out=attT[:, hf * 5:hf * 5 + 5, t4 * 128:(t4 + 1) * 128], in_=tpa[:, hf, 0:5, :]))
                for fo in range(DC):
                    fs = slice(fo * 128, (fo + 1) * 128)
                    s = fo % 2
                    for bi, (c0, c1) in enumerate(((0, 4), (4, 8), (8, 10))):
                        for c in range(c0, c1):
                            kb.op("pe", ["Wb", "attT"], [("yp", bi)], lambda e, c=c, bi=bi, c0=c0, c1=c1, fs=fs: e.matmul(
                                yp[:, bi, :], lhsT=Wb[:, c, fs], rhs=attT[:, c, :], start=(c == c0), stop=(c == c1 - 1)),
                                inc=(c == c1 - 1))
                    kb.op("dve", [("yp", 0), "gt"], [("m1", s)], lambda e, s=s, fo=fo: e.tensor_tensor(
                        out=m1[:, s, :], in0=yp[:, 0, :], in1=gt[:, fo, :], op=ALU.mult))
                    kb.op("dve", [("yp", 1), "gt"], [("m2", s)], lambda e, s=s, fo=fo: e.tensor_tensor(
                        out=m2[:, s, :], in0=yp[:, 1, :], in1=gt[:, DC + fo, :], op=ALU.mult))
                    kb.op("dve", [("m1", s), ("m2", s)], [("m1", s)], lambda e, s=s: e.tensor_tensor(
                        out=m1[:, s, :], in0=m1[:, s, :], in1=m2[:, s, :], op=ALU.add))
                    kb.op("dve", [("yp", 2), "gt"], [("m2", s)], lambda e, s=s, fo=fo: e.tensor_tensor(
                        out=m2[:, s, :], in0=yp[:, 2, :], in1=gt[:, 2 * DC + fo, :], op=ALU.mult))
                    kb.op("dve", [("m1", s), ("m2", s)], ["mg"], lambda e, s=s, fo=fo: e.tensor_tensor(
                        out=mg[:, fo, :], in0=m1[:, s, :], in1=m2[:, s, :], op=ALU.add))
                for t4 in range(4):
                    tt = tb * 4 + t4
                    s = tt % 2
                    kb.dma("act", xt[:, s, :], xin_d[tt * 128:(tt + 1) * 128, :], writes=[("xt3", s)])
                    for cb in range(0, D, 512):
                        cw_ = min(512, D - cb)
                        nt4 += 1
                        so = nt4 % 2
                        for fo in range(DC):
                            kb.op("pe", ["mg", "Wo"], [("op3", so)], lambda e, fo=fo, so=so, t4=t4, cb=cb, cw_=cw_: e.matmul(
                                op_[:, so, 0:cw_], lhsT=mg[:, fo, t4 * 128:(t4 + 1) * 128], rhs=Wo[:, fo, cb:cb + cw_],
                                start=(fo == 0), stop=(fo == DC - 1)), inc=(fo == DC - 1))
                        kb.op("dve", [("op3", so), ("xt3", s)], [("xt3", s)], lambda e, so=so, s=s, cb=cb, cw_=cw_: e.tensor_tensor(
                            out=xt[:, s, cb:cb + cw_], in0=op_[:, so, 0:cw_], in1=xt[:, s, cb:cb + cw_], op=ALU.add))
                    kb.dma("sp", x1_d[tt * 128:(tt + 1) * 128, :], xt[:, s, :], reads=[("xt3", s)], key=("x1st", s))
            kb.barrier()

    def phase4(layer, xout_d):
        with ExitStack() as st:
            def sbl(name, shape, dt):
                return st.enter_context(sbt(name, list(shape), dt))
            TB = 256
            stg = sbl("stg4", [128, 2, 2048], F32)
            Wfi = sbl("Wfi", [128, DC, 2 * DFF], BF16)
            Wfo = sbl("Wfo", [128, FC, D], BF16)
            G2 = sbl("G2", [128, D], F32)
            xt = sbl("xt4", [128, 2, D], F32)
            junk = sbl("junk4", [128, D], BF16)
            hb_ = sbl("hb4", [128, 2, D], BF16)
            ss = sbl("ss4", [128, 2, 4], F32)
            h2T = sbl("h2T", [128, DC, TB], BF16)
            sg = sbl("sg", [128, 2, TB], F32)
            actT = sbl("actT", [128, FC, TB], BF16)
            tp = st.enter_context(pst_("tp4", [128, 2, DC, 128], BF16))
            gp = st.enter_context(pst_("gp", [128, 2, 512], F32))
            up = st.enter_context(pst_("up", [128, 2, 512], F32))
            op_ = st.enter_context(pst_("op4", [128, 2, 512], F32))
            kb.dma("sp", G2[:], n2_d[layer:layer + 1, :].to_broadcast([128, D]), writes=["G2"])
            wfi_v = wfi_d[layer].rearrange("(c p) n -> p c n", p=128)
            for n0 in range(0, DFF, 256):
                n1 = min(DFF, n0 + 256)
                for half in range(2):
                    o_ = half * DFF
                    load_cast((stg,), Wfi[:, :, o_ + n0:o_ + n1], wfi_v[:, :, o_ + n0:o_ + n1], ("Wfi", half, n0 // 256))
            wfo_v = wfo_d[layer].rearrange("(c p) n -> p c n", p=128)
            for c0 in range(0, FC, 2):
                c1 = min(FC, c0 + 2)
                load_cast((stg,), Wfo[:, c0:c1, :], wfo_v[:, c0:c1, :], ("Wfo", c0 // 2))
            nt4 = 0
            for tb in range(S // TB):
                for t4 in range(TB // 128):
                    tt = tb * (TB // 128) + t4
                    s = t4 % 2
                    kb.dma("act", xt[:, s], x1_d[tt * 128:(tt + 1) * 128, :], writes=[("xt4", s)])
                    kb.op("act", [("xt4", s)], ["junk4", ("ss4", s)], lambda e, s=s: e.activation(
                        out=junk[:], in_=xt[:, s], func=AF.Square, accum_out=ss[:, s, 0:1]))
                    kb.op("act", [("ss4", s)], [("ss4", s)], lambda e, s=s: e.activation(
                        out=ss[:, s, 1:2], in_=ss[:, s, 0:1], func=AF.Sqrt, bias=epsc[:, 0:1], scale=1.0 / D))
                    kb.op("dve", [("ss4", s)], [("ss4", s)], lambda e, s=s: e.reciprocal(out=ss[:, s, 2:3], in_=ss[:, s, 1:2]))
                    kb.op("dve", [("xt4", s), ("ss4", s), "G2"], [("hb4", s)], lambda e, s=s: e.scalar_tensor_tensor(
                        out=hb_[:, s], in0=xt[:, s], scalar=ss[:, s, 2:3], in1=G2[:], op0=ALU.mult, op1=ALU.mult))
                    for c in range(DC):
                        kb.op("pe", [("hb4", s), "identb"], [("tp4", s)], lambda e, s=s, c=c: e.transpose(
                            out=tp[:, s, c, :], in_=hb_[:, s, c * 128:(c + 1) * 128], identity=identb[:]), inc=(c == DC - 1))
                    kb.op("act", [("tp4", s)], ["h2T"], lambda e, s=s, t4=t4: e.copy(
                        out=h2T[:, :, t4 * 128:(t4 + 1) * 128], in_=tp[:, s]))
                for fc in range(FC):
                    s = fc % 2
                    for c in range(DC):
                        kb.op("pe", [("Wfi", 0, fc // 2), "h2T"], [("gp", s)], lambda e, c=c, s=s, fc=fc: e.matmul(
                            gp[:, s, 0:TB], lhsT=Wfi[:, c, fc * 128:(fc + 1) * 128], rhs=h2T[:, c, :], start=(c == 0), stop=(c == DC - 1)),
                            inc=(c == DC - 1))
                    for c in range(DC):
                        kb.op("pe", [("Wfi", 1, fc // 2), "h2T"], [("up", s)], lambda e, c=c, s=s, fc=fc: e.matmul(
                            up[:, s, 0:TB], lhsT=Wfi[:, c, DFF + fc * 128:DFF + (fc + 1) * 128], rhs=h2T[:, c, :], start=(c == 0),
                            stop=(c == DC - 1)), inc=(c == DC - 1))
                    kb.op("act", [("gp", s)], [("sg", s)], lambda e, s=s: e.activation(
                        out=sg[:, s, :], in_=gp[:, s, 0:TB], func=AF.Silu))
                    kb.op("dve", [("up", s), ("sg", s)], ["actT"], lambda e, s=s, fc=fc: e.tensor_tensor(
                        out=actT[:, fc, :], in0=up[:, s, 0:TB], in1=sg[:, s, :], op=ALU.mult))
                for t4 in range(TB // 128):
                    tt = tb * (TB // 128) + t4
                    s = t4 % 2
                    for cb in range(0, D, 512):
                        cw_ = min(512, D - cb)
                        nt4 += 1
                        so = nt4 % 2
                        for fc in range(FC):
                            kb.op("pe", ["actT", ("Wfo", fc // 2)], [("op4", so)], lambda e, fc=fc, so=so, t4=t4, cb=cb, cw_=cw_: e.matmul(
                                op_[:, so, 0:cw_], lhsT=actT[:, fc, t4 * 128:(t4 + 1) * 128], rhs=Wfo[:, fc, cb:cb + cw_],
                                start=(fc == 0), stop=(fc == FC - 1)), inc=(fc == FC - 1))
                        kb.op("dve", [("op4", so), ("xt4", s)], [("xt4", s)], lambda e, so=so, s=s, cb=cb, cw_=cw_: e.tensor_tensor(
                            out=xt[:, s, cb:cb + cw_], in0=op_[:, so, 0:cw_], in1=xt[:, s, cb:cb + cw_], op=ALU.add))
                    kb.dma("sp", xout_d[tt * 128:(tt + 1) * 128, :], xt[:, s, :], reads=[("xt4", s)], key=("x2st", s))
            kb.barrier()

    LIMIT = getattr(build_nc, "limit", "all")
    NL = getattr(build_nc, "nlayers", 2)
    x2_d = dscr("x2", [S, D], F32)
    if LIMIT == "all":
        for layer in range(NL):
            xin = x_d if layer == 0 else x2_d
            xout = y_d if layer == NL - 1 else x2_d
            phase1(layer, xin)
            phase2A(layer)
            phase2B(layer)
            phase2C(layer)
            phase3(layer, xin)
            phase4(layer, xout)
    else:
        if LIMIT not in ("none", "setup"):
            phase1(0, x_d)
        if LIMIT == "A":
            phase2A(0)
        if LIMIT == "B":
            phase2B(0)
        if LIMIT == "C":
            phase2C(0)
        with ExitStack() as st:
            yt = st.enter_context(sbt("yt", [128, D], F32))
            for tt in range(NT):
                kb.dma("sp", yt[:], x_d[tt * 128:(tt + 1) * 128, :], writes=["yt"])
                kb.dma("sp", y_d[tt * 128:(tt + 1) * 128, :], yt[:], reads=["yt"], key="ystore")
            kb.barrier()
    stack.close()
    return nc


_NC_CACHE = {}


def kernel(**inputs):
    x = np.asarray(inputs["x"], dtype=np.float32)
    B, S, D = x.shape
    DFF = int(np.asarray(inputs["w_ffn_out"]).shape[1])
    key = (S, D, DFF)
    if key not in _NC_CACHE:
        _NC_CACHE[key] = build_nc(S, D, DFF)
    nc = _NC_CACHE[key]
    consts = make_consts()
    shared = {k: np.ascontiguousarray(np.asarray(v, dtype=np.float32)) for k, v in inputs.items() if k != "x"}
    shared.update(consts)
    in_maps = []
    for b in range(B):
        m = dict(shared)
        m["x"] = np.ascontiguousarray(x[b])
        in_maps.append(m)
    res = run_bass_kernel_spmd(nc, in_maps, core_ids=list(range(B)))
    return np.stack([np.asarray(r["y"], dtype=np.float32) for r in res.results], axis=0)
```
